# Optimizing a Trainium2 kernel written in Bass

```python
import jax, jax.numpy as jnp
from jax import lax
import numpy as np

D_MODEL = 2048
BATCH = 16
SEQ = 2048
DEPTH = 2

GRID_W = 64
CTX_LEN = 256
HEAD_DIM = 64
BRANCH_WIDTH = D_MODEL // 2
N_BRANCHES = 3
RWKV_WIDTH = BRANCH_WIDTH
RWKV_HEADS = RWKV_WIDTH // HEAD_DIM
DECAY_RANK = 96
ICLR_RANK = 96
VRES_RANK = 64
GATE_RANK = 256
GN_EPS = 64e-5
CONV_WIDTH = BRANCH_WIDTH
CONV_K = 3
ATTN_HEADS = BRANCH_WIDTH // HEAD_DIM
ATTN_KV_HEADS = ATTN_HEADS // 4
ATTN_WIDTH = ATTN_HEADS * HEAD_DIM
KV_WIDTH = ATTN_KV_HEADS * HEAD_DIM
WINDOW = 128
BLOCK = 128
ROPE_BASE = 10000.0
NEG_INF = -1e30
N_EXPERTS = 16
EXPERT_FF = 2048
CAPACITY_FACTOR = 2
NORM_EPS = 1e-6

RWKV_COLS = 3 * RWKV_WIDTH + 2 * DECAY_RANK + 2 * ICLR_RANK + GATE_RANK
RWKV_SPLITS = (RWKV_WIDTH, 2 * RWKV_WIDTH, 3 * RWKV_WIDTH,
               3 * RWKV_WIDTH + 2 * DECAY_RANK, 3 * RWKV_WIDTH + 2 * DECAY_RANK + 2 * ICLR_RANK)
CTX_COLS = RWKV_COLS + 2 * KV_WIDTH
Q_END = CTX_COLS + ATTN_WIDTH
CONV_END = Q_END + 3 * CONV_WIDTH
IN_COLS = CONV_END + N_BRANCHES * D_MODEL

kernel_name = "hybrid_rwkv7_shortconv_swa_ec_dit"


def rms_norm(x, gain):
    xf = x.astype(jnp.float32)
    y = xf * lax.rsqrt(jnp.mean(xf * xf, axis=-1, keepdims=True) + NORM_EPS)
    return y.astype(x.dtype) * gain


def modulate(h, shift, scale):
    return h * (1 + scale) + shift


def neighbours(u):
    up = jnp.pad(u, ((0, 0), (1, 1), (0, 0)))
    return up[:, :-2], up[:, 2:]


def depthwise_conv3(u, w):
    prev, nxt = neighbours(u)
    return w[0] * prev + w[1] * u + w[2] * nxt


def rope_2d(x, rows, cols):
    half = HEAD_DIM // 2
    quarter = half // 2
    inv = ROPE_BASE ** (-jnp.arange(quarter, dtype=jnp.float32) / quarter)

    def rot(xp, pos):
        ang = pos.astype(jnp.float32)[:, None] * inv[None, :]
        cos = jnp.cos(ang)[:, None, :].astype(x.dtype)
        sin = jnp.sin(ang)[:, None, :].astype(x.dtype)
        x1, x2 = xp[..., :quarter], xp[..., quarter:]
        return jnp.concatenate([x1 * cos - x2 * sin, x2 * cos + x1 * sin], axis=-1)

    return jnp.concatenate([rot(x[..., :half], rows), rot(x[..., half:], cols)], axis=-1)


def rwkv_streams(p, mu, decay_up, decay_bias, iclr_up, iclr_bias, k_k, k_a, v_first, vres):
    B, T, _ = p.shape
    prev, nxt = neighbours(p)
    p = p + mu * (0.5 * (prev + nxt) - p)
    r, k, v, wd, ad, gd = jnp.split(p, RWKV_SPLITS, axis=-1)
    wd = wd.reshape(B, T, 2, DECAY_RANK)
    ad = ad.reshape(B, T, 2, ICLR_RANK)
    w_logit = decay_bias + jnp.einsum('btdr,drc->btdc', jnp.tanh(wd), decay_up)
    decay = jnp.exp(-jnp.exp(-jax.nn.softplus(-w_logit) - 0.5))
    a = jax.nn.sigmoid(iclr_bias + jnp.einsum('btdr,drc->btdc', ad, iclr_up))
    if vres is None:
        v_first = v
    else:
        vd, vu, vb = vres
        v = v + (v_first - v) * jax.nn.sigmoid(vb + (v @ vd) @ vu)
    kh = (k * k_k).reshape(B, T, RWKV_HEADS, HEAD_DIM).astype(jnp.float32)
    kk = kh * lax.rsqrt(jnp.maximum(jnp.sum(kh * kh, axis=-1, keepdims=True), 1e-24))
    kk = kk.reshape(B, T, RWKV_WIDTH).astype(k.dtype)
    kd = k[:, :, None, :] * (1 + (a - 1) * k_a)
    return (r, kd, v, kk, decay, a, gd), v_first


def to_scan_layout(z):
    if z.ndim == 3:
        z = jnp.broadcast_to(z[:, :, None, :], z.shape[:2] + (2,) + z.shape[2:])
    z = jnp.stack([z[:, :, 0], jnp.flip(z[:, :, 1], axis=1)], axis=0)
    d, b, t, _ = z.shape
    return jnp.moveaxis(z.reshape(d, b, t, RWKV_HEADS, HEAD_DIM), 2, 0).astype(jnp.float32)


def rwkv_update(S, w, k, v, kk, a):
    sa = jnp.einsum('dbhij,dbhj->dbhi', S, -kk)
    return S * w[..., None, :] + sa[..., :, None] * (kk * a)[..., None, :] + v[..., :, None] * k[..., None, :]


def rwkv_scan(S0, r, decay, kd, v, kk, a, with_output):
    w_s, k_s, v_s, kk_s, a_s = (to_scan_layout(decay), to_scan_layout(kd), to_scan_layout(v),
                                to_scan_layout(kk), to_scan_layout(a))
    if with_output:
        def step(S, inp):
            rr, w, k, vv, kq, aa = inp
            S = rwkv_update(S, w, k, vv, kq, aa)
            return S, jnp.einsum('dbhij,dbhj->dbhi', S, rr)
        S, y = lax.scan(step, S0, (to_scan_layout(r), w_s, k_s, v_s, kk_s, a_s))
        y = y[:, 0] + jnp.flip(y[:, 1], axis=0)
        return S, jnp.moveaxis(y, 0, 1)

    def step_state(S, inp):
        w, k, vv, kq, aa = inp
        return rwkv_update(S, w, k, vv, kq, aa), None
    S, _ = lax.scan(step_state, S0, (w_s, k_s, v_s, kk_s, a_s))
    return S, None


def rwkv_output(y, r, kd, v, gd, gate_up, r_k, gn_w, gn_b):
    B, T, _ = r.shape
    mean = jnp.mean(y, axis=-1, keepdims=True)
    var = jnp.mean(jnp.square(y - mean), axis=-1, keepdims=True)
    yn = ((y - mean) * lax.rsqrt(var + GN_EPS)).reshape(B, T, RWKV_WIDTH).astype(r.dtype) * gn_w + gn_b
    rh = r.reshape(B, T, RWKV_HEADS, HEAD_DIM)
    kh = kd.reshape(B, T, 2, RWKV_HEADS, HEAD_DIM)
    vh = v.reshape(B, T, RWKV_HEADS, HEAD_DIM)
    bonus = jnp.einsum('bthn,btdhn,hn->bth', rh, kh, r_k)[..., None] * vh
    g = jax.nn.sigmoid(gd) @ gate_up
    return (yn + bonus.reshape(B, T, RWKV_WIDTH)) * g


def short_conv_mixer(p, conv_w):
    b_gate, c_gate, u = jnp.split(p, 3, axis=-1)
    return b_gate * depthwise_conv3(c_gate * u, conv_w)


def latent_attention(q, k, v, kc, vc, sink):
    B, S, H, Dh = q.shape
    nb = S // BLOCK
    G = H // ATTN_KV_HEADS
    scale = Dh ** -0.5
    qb = q.reshape(B, nb, BLOCK, ATTN_KV_HEADS, G, Dh)

    def band(z):
        zp = jnp.pad(z, ((0, 0), (BLOCK, BLOCK), (0, 0), (0, 0))).reshape(B, nb + 2, BLOCK, ATTN_KV_HEADS, Dh)
        return jnp.concatenate([zp[:, :-2], zp[:, 1:-1], zp[:, 2:]], axis=2)

    kb, vb = band(k), band(v)
    qpos = jnp.arange(BLOCK)[:, None]
    kpos = jnp.arange(3 * BLOCK)[None, :] - BLOCK
    kabs = jnp.arange(nb)[:, None, None] * BLOCK + kpos[None]
    mask = (jnp.abs(qpos - kpos) <= WINDOW)[None] & (kabs >= 0) & (kabs < S)
    s_band = jnp.einsum('bnqkgd,bnskd->bnkgqs', qb, kb).astype(jnp.float32) * scale
    s_band = jnp.where(mask[None, :, None, None], s_band, NEG_INF)
    s_ctx = jnp.einsum('bnqkgd,bckd->bnkgqc', qb, kc).astype(jnp.float32) * scale
    s_sink = jnp.broadcast_to(sink.astype(jnp.float32).reshape(1, 1, ATTN_KV_HEADS, G, 1, 1),
                              s_ctx.shape[:-1] + (1,))
    probs = jax.nn.softmax(jnp.concatenate([s_band, s_ctx, s_sink], axis=-1), axis=-1).astype(v.dtype)
    nband = 3 * BLOCK
    L = kc.shape[1]
    out = (jnp.einsum('bnkgqs,bnskd->bnqkgd', probs[..., :nband], vb)
           + jnp.einsum('bnkgqc,bckd->bnqkgd', probs[..., nband:nband + L], vc))
    return out.reshape(B, S, H * Dh)


def context_attention(q, kc, vc, sink):
    B, L, H, Dh = q.shape
    G = H // ATTN_KV_HEADS
    qg = q.reshape(B, L, ATTN_KV_HEADS, G, Dh)
    s = jnp.einsum('bqkgd,bckd->bkgqc', qg, kc).astype(jnp.float32) * Dh ** -0.5
    s_sink = jnp.broadcast_to(sink.astype(jnp.float32).reshape(1, ATTN_KV_HEADS, G, 1, 1), s.shape[:-1] + (1,))
    probs = jax.nn.softmax(jnp.concatenate([s, s_sink], axis=-1), axis=-1)[..., :L].astype(vc.dtype)
    return jnp.einsum('bkgqc,bckd->bqkgd', probs, vc).reshape(B, L, H * Dh)


def merge_branches(branches, gate_cols, w_branch, w_out):
    B, T, _ = gate_cols.shape
    gates = jax.nn.sigmoid(gate_cols.reshape(B, T, N_BRANCHES, D_MODEL))
    proj = jnp.einsum('btiw,iwd->btid', branches, w_branch)
    return jnp.einsum('btd,de->bte', jnp.sum(gates * proj, axis=2), w_out)


def expert_choice_ffn(h, w_router, w_gate, w_up, w_down):
    B, n, _ = h.shape
    cap = CAPACITY_FACTOR * n // N_EXPERTS
    aff = jax.nn.softmax(jnp.einsum('bnd,de->bne', h, w_router).astype(jnp.float32), axis=-1)
    g, idx = lax.top_k(jnp.swapaxes(aff, 1, 2), cap)
    bidx = jnp.arange(B)[:, None, None]
    xs = h[bidx, idx]
    hid = jax.nn.silu(jnp.einsum('becd,edf->becf', xs, w_gate)) * jnp.einsum('becd,edf->becf', xs, w_up)
    ys = jnp.einsum('becf,efd->becd', hid, w_down) * g[..., None].astype(h.dtype)
    return jnp.zeros_like(h).at[bidx, idx].add(ys)


def setup_inputs(seed: int = 0) -> dict:
    key = jax.random.key(seed)
    ks = jax.random.split(key, 40)
    f32 = jnp.float32

    def nrm(k, shape, scale):
        return jax.random.normal(k, shape, f32) * scale

    D, L1 = D_MODEL, DEPTH - 1
    return {
        "x": nrm(ks[0], (BATCH, SEQ, D), 1.0),
        "c": nrm(ks[1], (BATCH, D), 1.0),
        "ctx": nrm(ks[2], (BATCH, CTX_LEN, D), 1.0),
        "c_ctx": nrm(ks[3], (D,), 1.0),
        "w_mod": nrm(ks[4], (DEPTH, D, 6 * D), 0.5 * D ** -0.5),
        "b_mod": nrm(ks[5], (DEPTH, 6 * D), 0.02),
        "norm_mix": 1.0 + nrm(ks[6], (DEPTH, D), 0.02),
        "norm_ffn": 1.0 + nrm(ks[7], (DEPTH, D), 0.02),
        "w_in": nrm(ks[8], (DEPTH, D, IN_COLS), D ** -0.5),
        "shift_mu": jax.random.uniform(ks[9], (DEPTH, RWKV_COLS), f32),
        "decay_up": nrm(ks[10], (DEPTH, 2, DECAY_RANK, RWKV_WIDTH), 0.5 * DECAY_RANK ** -0.5),
        "decay_bias": nrm(ks[11], (DEPTH, 2, RWKV_WIDTH), 0.5),
        "iclr_up": nrm(ks[12], (DEPTH, 2, ICLR_RANK, RWKV_WIDTH), 0.5 * ICLR_RANK ** -0.5),
        "iclr_bias": nrm(ks[13], (DEPTH, 2, RWKV_WIDTH), 0.5),
        "gate_up": nrm(ks[14], (DEPTH, GATE_RANK, RWKV_WIDTH), GATE_RANK ** -0.5),
        "vres_down": nrm(ks[15], (L1, RWKV_WIDTH, VRES_RANK), RWKV_WIDTH ** -0.5),
        "vres_up": nrm(ks[16], (L1, VRES_RANK, RWKV_WIDTH), 0.5 * VRES_RANK ** -0.5),
        "vres_bias": nrm(ks[17], (L1, RWKV_WIDTH), 0.5),
        "k_k": 0.85 + nrm(ks[18], (DEPTH, RWKV_WIDTH), 0.05),
        "k_a": 1.0 + nrm(ks[19], (DEPTH, RWKV_WIDTH), 0.05),
        "r_k": nrm(ks[20], (DEPTH, RWKV_HEADS, HEAD_DIM), 0.1),
        "gn_w": 1.0 + nrm(ks[21], (DEPTH, RWKV_WIDTH), 0.02),
        "gn_b": nrm(ks[22], (DEPTH, RWKV_WIDTH), 0.02),
        "conv_w": nrm(ks[23], (DEPTH, CONV_K, CONV_WIDTH), CONV_K ** -0.5),
        "attn_sink": nrm(ks[24], (DEPTH, ATTN_HEADS), 1.0),
        "w_branch": nrm(ks[25], (DEPTH, N_BRANCHES, BRANCH_WIDTH, D), BRANCH_WIDTH ** -0.5),
        "w_out": nrm(ks[26], (DEPTH, D, D), D ** -0.5),
        "w_router": nrm(ks[27], (DEPTH, D, N_EXPERTS), D ** -0.5),
        "w_exp_gate": nrm(ks[28], (DEPTH, N_EXPERTS, D, EXPERT_FF), D ** -0.5),
        "w_exp_up": nrm(ks[29], (DEPTH, N_EXPERTS, D, EXPERT_FF), D ** -0.5),
        "w_exp_down": nrm(ks[30], (DEPTH, N_EXPERTS, EXPERT_FF, D), EXPERT_FF ** -0.5),
        "norm_final": 1.0 + nrm(ks[31], (D,), 0.02),
    }


def reference(x, c, ctx, c_ctx, w_mod, b_mod, norm_mix, norm_ffn, w_in, shift_mu, decay_up, decay_bias,
              iclr_up, iclr_bias, gate_up, vres_down, vres_up, vres_bias, k_k, k_a, r_k, gn_w, gn_b,
              conv_w, attn_sink, w_branch, w_out, w_router, w_exp_gate, w_exp_up, w_exp_down, norm_final):
    B, S, _ = x.shape
    L = ctx.shape[1]
    n_rows = S // GRID_W
    rows = jnp.repeat(jnp.arange(n_rows), GRID_W)
    cols = jnp.tile(jnp.arange(GRID_W), n_rows)
    silu_c = jax.nn.silu(c)
    silu_cc = jax.nn.silu(c_ctx)
    x_l, x_c = x, ctx
    v_first_l = v_first_c = None

    for l in range(DEPTH):
        last = l == DEPTH - 1
        mod_l = (silu_c @ w_mod[l] + b_mod[l]).reshape(B, 6, 1, D_MODEL)
        mod_c = (silu_cc @ w_mod[l] + b_mod[l]).reshape(6, 1, D_MODEL)
        h_l = modulate(rms_norm(x_l, norm_mix[l]), mod_l[:, 0], mod_l[:, 1])
        h_c = modulate(rms_norm(x_c, norm_mix[l]), mod_c[0], mod_c[1])
        p_l = h_l @ w_in[l]
        p_c = h_c @ (w_in[l][:, :CTX_COLS] if last else w_in[l])

        vres = None if l == 0 else (vres_down[l - 1], vres_up[l - 1], vres_bias[l - 1])
        (r_c, kd_c, v_c, kk_c, dec_c, a_c, gd_c), v_first_c = rwkv_streams(
            p_c[..., :RWKV_COLS], shift_mu[l], decay_up[l], decay_bias[l], iclr_up[l], iclr_bias[l],
            k_k[l], k_a[l], v_first_c, vres)
        (r_l, kd_l, v_l, kk_l, dec_l, a_l, gd_l), v_first_l = rwkv_streams(
            p_l[..., :RWKV_COLS], shift_mu[l], decay_up[l], decay_bias[l], iclr_up[l], iclr_bias[l],
            k_k[l], k_a[l], v_first_l, vres)
        s0 = jnp.zeros((2, B, RWKV_HEADS, HEAD_DIM, HEAD_DIM), jnp.float32)
        state_c, y_c = rwkv_scan(s0, r_c, dec_c, kd_c, v_c, kk_c, a_c, not last)
        _, y_l = rwkv_scan(state_c, r_l, dec_l, kd_l, v_l, kk_l, a_l, True)
        br_rwkv_l = rwkv_output(y_l, r_l, kd_l, v_l, gd_l, gate_up[l], r_k[l], gn_w[l], gn_b[l])

        k_ctx = p_c[..., RWKV_COLS:RWKV_COLS + KV_WIDTH].reshape(B, L, ATTN_KV_HEADS, HEAD_DIM)
        v_ctx = p_c[..., RWKV_COLS + KV_WIDTH:CTX_COLS].reshape(B, L, ATTN_KV_HEADS, HEAD_DIM)
        q_lat = rope_2d(p_l[..., CTX_COLS:Q_END].reshape(B, S, ATTN_HEADS, HEAD_DIM), rows, cols)
        k_lat = rope_2d(p_l[..., RWKV_COLS:RWKV_COLS + KV_WIDTH].reshape(B, S, ATTN_KV_HEADS, HEAD_DIM), rows, cols)
        v_lat = p_l[..., RWKV_COLS + KV_WIDTH:CTX_COLS].reshape(B, S, ATTN_KV_HEADS, HEAD_DIM)
        br_attn_l = latent_attention(q_lat, k_lat, v_lat, k_ctx, v_ctx, attn_sink[l])

        br_conv_l = short_conv_mixer(p_l[..., Q_END:CONV_END], conv_w[l])

        out_l = merge_branches(jnp.stack([br_rwkv_l, br_conv_l, br_attn_l], axis=2),
                               p_l[..., CONV_END:], w_branch[l], w_out[l])
        x_l = x_l + mod_l[:, 2] * out_l
        f_l = expert_choice_ffn(modulate(rms_norm(x_l, norm_ffn[l]), mod_l[:, 3], mod_l[:, 4]),
                                w_router[l], w_exp_gate[l], w_exp_up[l], w_exp_down[l])
        x_l = x_l + mod_l[:, 5] * f_l

        if not last:
            br_rwkv_c = rwkv_output(y_c, r_c, kd_c, v_c, gd_c, gate_up[l], r_k[l], gn_w[l], gn_b[l])
            q_ctx = p_c[..., CTX_COLS:Q_END].reshape(B, L, ATTN_HEADS, HEAD_DIM)
            br_attn_c = context_attention(q_ctx, k_ctx, v_ctx, attn_sink[l])
            br_conv_c = short_conv_mixer(p_c[..., Q_END:CONV_END], conv_w[l])
            out_c = merge_branches(jnp.stack([br_rwkv_c, br_conv_c, br_attn_c], axis=2),
                                   p_c[..., CONV_END:], w_branch[l], w_out[l])
            x_c = x_c + mod_c[2] * out_c
            f_c = expert_choice_ffn(modulate(rms_norm(x_c, norm_ffn[l]), mod_c[3], mod_c[4]),
                                    w_router[l], w_exp_gate[l], w_exp_up[l], w_exp_down[l])
            x_c = x_c + mod_c[5] * f_c

    return rms_norm(x_l, norm_final)
```

```python
import numpy as np
from contextlib import ExitStack
import concourse.bass as bass
import concourse.mybir as mybir
from concourse.bass_utils import run_bass_kernel_spmd

F32 = mybir.dt.float32
BF16 = mybir.dt.bfloat16
U32 = mybir.dt.uint32
I32 = mybir.dt.int32
ALU = mybir.AluOpType
AF = mybir.ActivationFunctionType
AX = mybir.AxisListType


class Cfg:
    def __init__(s, D=2048, SEQ=2048, CTX=256, GRID_W=64, DEPTH=2, NE=16, FF=2048, NCORES=8, BATCH=16, GATHER=True):
        s.GATHER = GATHER
        s.D, s.SEQ, s.CTX, s.GRID_W, s.DEPTH, s.NE, s.FF = D, SEQ, CTX, GRID_W, DEPTH, NE, FF
        s.NCORES, s.BATCH = NCORES, BATCH
        s.NS = BATCH // NCORES
        s.BW = D // 2
        s.H = s.BW // 64
        s.KVH = s.H // 4
        s.DR, s.IR, s.VR, s.GR = 96, 96, 64, 256
        s.RC = 3 * s.BW + 2 * s.DR + 2 * s.IR + s.GR
        s.KVW = s.KVH * 64
        s.CTXC = s.RC + 2 * s.KVW
        s.QEND = s.CTXC + s.BW
        s.CONVEND = s.QEND + 3 * s.BW
        s.INC = s.CONVEND + 3 * D
        s.NT = s.NS * (s.CTX + s.SEQ)
        s.TALL = s.CTX + s.SEQ
        s.KC = D // 128
        s.BC = s.BW // 128
        s.CAPL = 2 * s.SEQ // NE
        s.CAPC = 2 * s.CTX // NE
        s.IQ = 128 // (s.NS * s.H)
        s.IP = 64 // s.IQ
        s.seqs = [('c', b, b * s.CTX, s.CTX, s.NS) for b in range(s.NS)] + \
                 [('l', b, s.NS * s.CTX + b * s.SEQ, s.SEQ, b) for b in range(s.NS)]


BIGW = ['w_mod', 'w_in', 'w_branch', 'w_out', 'w_exp_gate', 'w_exp_up', 'w_exp_down']
SMALLW = ['b_mod', 'norm_mix', 'norm_ffn', 'shift_mu', 'decay_up', 'decay_bias', 'iclr_up', 'iclr_bias',
          'gate_up', 'vres_down', 'vres_up', 'vres_bias', 'k_k', 'k_a', 'r_k', 'gn_w', 'gn_b', 'conv_w',
          'attn_sink', 'w_router', 'norm_final', 'c_ctx']


def big_shapes(c):
    L = c.DEPTH
    return {'w_mod': (L * c.D, 6 * c.D), 'w_in': (L * c.D, c.INC), 'w_branch': (L * 3 * c.BW, c.D),
            'w_out': (L * c.D, c.D), 'w_exp_gate': (L * c.NE * c.D, c.FF), 'w_exp_up': (L * c.NE * c.D, c.FF),
            'w_exp_down': (L * c.NE * c.FF, c.D)}


def small_shapes(c):
    L = c.DEPTH
    return {'b_mod': (L, 6 * c.D), 'norm_mix': (L, c.D), 'norm_ffn': (L, c.D), 'shift_mu': (L, c.RC),
            'decay_up': (L * 2 * c.DR, c.BW), 'decay_bias': (L * 2, c.BW), 'iclr_up': (L * 2 * c.IR, c.BW),
            'iclr_bias': (L * 2, c.BW), 'gate_up': (L * c.GR, c.BW), 'vres_down': ((L - 1) * c.BW, c.VR),
            'vres_up': ((L - 1) * c.VR, c.BW), 'vres_bias': (L - 1, c.BW), 'k_k': (L, c.BW), 'k_a': (L, c.BW),
            'r_k': (L, c.BW), 'gn_w': (L, c.BW), 'gn_b': (L, c.BW), 'conv_w': (L * 3, c.BW),
            'attn_sink': (L, c.H), 'w_router': (L * c.D, c.NE), 'norm_final': (1, c.D), 'c_ctx': (1, c.D)}


def host_consts(c):
    ident = np.eye(128, dtype=np.float32)
    blk = np.zeros((128, 128), np.float32)
    blk[:64, :64] = 1
    blk[64:, 64:] = 1
    swp = np.zeros((64, 64), np.float32)
    for m in range(64):
        q = m % 32
        swp[m + 16 if q < 16 else m - 16, m] = 1
    k = np.arange(128)[:, None]
    q = np.arange(128)[None, :]
    masks = np.stack([(k >= q), (k <= q)]).astype(np.float32)
    t = np.arange(c.SEQ)
    rows = (t // c.GRID_W).astype(np.float32)
    cols = (t % c.GRID_W).astype(np.float32)
    inv = (10000.0 ** (-np.arange(16, dtype=np.float32) / 16)).astype(np.float32)
    cs = np.zeros((2, 64, c.SEQ), np.float32)
    for n in range(64):
        pos = rows if n < 32 else cols
        m = n % 32
        ang = (pos * inv[m % 16]).astype(np.float32)
        cs[0, n] = np.cos(ang)
        cs[1, n] = -np.sin(ang) if m < 16 else np.sin(ang)
    return {'k_ident': ident, 'k_blk': blk, 'k_swp': swp, 'k_masks': masks.reshape(256, 128),
            'k_rope': cs.reshape(128, c.SEQ)}


class Prog:
    ENG = ('sp', 'act', 'dve', 'pool', 'pe')
    NDS = 8

    def __init__(s, nc):
        s.nc = nc
        s.sems = {}
        s.esem = {e: s._sem('e_' + e) for e in s.ENG}
        s.ecnt = {e: 0 for e in s.ENG}
        s.dsem = {e: [s._sem('d_%s%d' % (e, i)) for i in range(s.NDS)] for e in ('sp', 'act', 'pool')}
        s.dcnt = {e: 0 for e in ('sp', 'act', 'pool')}
        s.waited = {e: {} for e in s.ENG}
        s.last = {}
        s.res = {}
        s.q = {e: [] for e in s.ENG}

    def _sem(s, name):
        s.sems[name] = s.nc.alloc_semaphore(name=name)
        return name

    @staticmethod
    def _key(r):
        if isinstance(r, tuple):
            return (Prog._key(r[0]),) + tuple(r[1:])
        if isinstance(r, str):
            return r
        return id(r)

    def _waits(s, e, reads, writes, extra=()):
        toks = list(extra)
        for r in reads:
            st = s.res.get(s._key(r))
            if st and st['w']:
                toks.append(st['w'])
        for w in writes:
            st = s.res.get(s._key(w))
            if st:
                if st['w']:
                    toks.append(st['w'])
                toks.extend(st['r'].items())
        out = {}
        for sk, v in toks:
            if e == 'pe' and sk == s.esem['pe']:
                continue
            if s.waited[e].get(sk, 0) < v:
                out[sk] = max(out.get(sk, 0), v)
        for sk, v in out.items():
            s.waited[e][sk] = v
        return list(out.items())

    def _commit(s, tok, reads, writes):
        s.last[tok[0]] = tok[1]
        wk = [s._key(w) for w in writes]
        for k in wk:
            s.res[k] = {'w': tok, 'r': {}}
        for r in reads:
            k = s._key(r)
            if k in wk:
                continue
            st = s.res.setdefault(k, {'w': None, 'r': {}})
            st['r'][tok[0]] = max(st['r'].get(tok[0], 0), tok[1])

    def op(s, e, fn, reads=(), writes=()):
        waits = s._waits(e, reads, writes)
        s.ecnt[e] += 1
        tok = (s.esem[e], s.ecnt[e])
        s.q[e].append((waits, fn, tok[0], 1))
        s._commit(tok, reads, writes)

    def dma(s, e, fn, reads=(), writes=(), inc=16):
        n = s.dcnt[e]
        s.dcnt[e] += 1
        slot = s.dsem[e][n % s.NDS]
        prev = s.last.get(slot, 0)
        waits = s._waits(e, reads, writes, extra=[(slot, prev)] if prev else [])
        tok = (slot, prev + inc)
        s.q[e].append((waits, fn, slot, inc))
        s._commit(tok, reads, writes)

    MAGIC = 1000

    def prologue(s):
        nc = s.nc
        s.gate = {e: nc.alloc_semaphore(name='gate_' + e) for e in s.ENG}
        s.done = nc.alloc_semaphore(name='done')
        gate, sems, done, MAGIC = s.gate, s.sems, s.done, s.MAGIC
        with nc.Block() as block:
            def mk(e):
                def f(eng):
                    if e == 'pool':
                        for h in sems.values():
                            eng.sem_clear(h)
                        eng.sem_clear(done)
                        for g in gate.values():
                            eng.sem_clear(g)
                        for g in gate.values():
                            eng.sem_inc(g, MAGIC)
                    eng.wait_op(gate[e], MAGIC, 'sem-eq')
                    eng.sem_inc(gate[e], 1)
                return f
            block.sync(mk('sp'))
            block.scalar(mk('act'))
            block.vector(mk('dve'))
            block.gpsimd(mk('pool'))
            block.tensor(mk('pe'))

    def epilogue(s):
        nc = s.nc
        gate, sems, done = s.gate, s.sems, s.done
        with nc.Block() as block:
            def mk(e):
                def f(eng):
                    if e == 'pool':
                        eng.wait_ge(done, 4)
                        for h in sems.values():
                            eng.sem_clear(h)
                        for g in gate.values():
                            eng.sem_clear(g)
                        eng.sem_clear(done)
                    else:
                        eng.sem_inc(done, 1)
                return f
            block.sync(mk('sp'))
            block.scalar(mk('act'))
            block.vector(mk('dve'))
            block.gpsimd(mk('pool'))
            block.tensor(mk('pe'))

    def flush(s):
        nc = s.nc
        for e in s.ENG:
            waits = []
            for sk, v in s.last.items():
                if s.waited[e].get(sk, 0) < v:
                    s.waited[e][sk] = v
                    waits.append((sk, v))
            s.q[e].append((waits, None, None, 0))
        q = s.q
        sems = s.sems
        with nc.Block() as block:
            def mk(e):
                def f(eng):
                    for waits, fn, sem, inc in q[e]:
                        for sk, v in waits:
                            eng.wait_ge(sems[sk], v)
                        if fn is not None:
                            ins = fn(eng)
                            ins.then_inc(sems[sem], inc)
                return f
            block.sync(mk('sp'))
            block.scalar(mk('act'))
            block.vector(mk('dve'))
            block.gpsimd(mk('pool'))
            block.tensor(mk('pe'))
        s.q = {e: [] for e in s.ENG}
        s.res = {}


def AP(t, off, dims):
    return bass.AP(t.tensor, off, [list(d) for d in dims])


class Builder:
    def __init__(s, c, debug_out=None, stop=None):
        s.c = c
        s.stop = stop
        s.uid = 0
        s.nc = bass.Bass("TRN2", target_bir_lowering=False)
        s.P = Prog(s.nc)
        s.debug_out = debug_out or []

    def dram(s, name, shape, dt=F32, kind="Internal"):
        if kind == "Internal":
            return s.nc.dram_tensor(name, list(shape), dt).ap()
        return s.nc.dram_tensor(name, list(shape), dt, kind=kind).ap()

    def sb(s, es, name, shape, dt=F32):
        s.uid += 1
        return es.enter_context(s.nc.sbuf_tensor('%s_%d' % (name, s.uid), list(shape), dt))

    def ps(s, es, name, shape, dt=F32):
        s.uid += 1
        return es.enter_context(s.nc.psum_tensor('%s_%d' % (name, s.uid), list(shape), dt))

    def ld(s, out, in_, reads, writes, q='sp'):
        s.P.dma(q, lambda e: e.dma_start(out=out, in_=in_), reads, writes)

    def ldnc(s, out, in_, reads, writes, q='sp'):
        s.P.dma(q, lambda e: e.dma_start(out=out, in_=in_, allow_slow_non_contiguous=True), reads, writes)

    def tt(s, e, out, a, b, op, reads, writes):
        s.P.op(e, lambda g: g.tensor_tensor(out=out, in0=a, in1=b, op=op), reads, writes)

    def ts(s, e, out, a, s1, s2, op0, op1, reads, writes):
        if s2 is None:
            s.P.op(e, lambda g: g.tensor_scalar(out=out, in0=a, scalar1=s1, scalar2=None, op0=op0), reads, writes)
        else:
            s.P.op(e, lambda g: g.tensor_scalar(out=out, in0=a, scalar1=s1, scalar2=s2, op0=op0, op1=op1),
                   reads, writes)

    def stt(s, e, out, a, sc, b, op0, op1, reads, writes):
        s.P.op(e, lambda g: g.scalar_tensor_tensor(out=out, in0=a, scalar=sc, in1=b, op0=op0, op1=op1),
               reads, writes)

    def act(s, out, in_, func, reads, writes, bias=None, scale=None, accum=None):
        kw = {}
        if bias is not None:
            kw['bias'] = bias
        if scale is not None:
            kw['scale'] = scale
        if accum is not None:
            kw['accum_out'] = accum
        s.P.op('act', lambda g: g.activation(out=out, in_=in_, func=func, **kw), reads, writes)

    def rsqrt(s, out, in_, mult, add, reads, writes):
        s.act(out, in_, AF.Sqrt, reads, writes, bias=s.cbias(add), scale=mult)
        s.P.op('dve', lambda g: g.reciprocal(out=out, in_=out), writes, writes)

    def cbias(s, val):
        key = float(val)
        if key not in s.cb:
            i = len(s.cb)
            s.cb[key] = i
            s.P.op('pool', (lambda i, key: lambda g: g.memset(s.cbt[:, i:i + 1], key))(i, key), (), [(s.cbt, i)])
        i = s.cb[key]
        return s.cbt[:, i:i + 1]

    def cp(s, e, out, in_, reads, writes):
        if e == 'act':
            s.act(out, in_, AF.Copy, reads, writes)
        else:
            s.P.op(e, lambda g: g.tensor_copy(out=out, in_=in_), reads, writes)

    def red(s, out, in_, reads, writes, negate=False):
        s.P.op('dve', lambda g: g.tensor_reduce(out=out, in_=in_, axis=AX.X, op=ALU.add, negate=negate),
               reads, writes)

    def mm(s, out, pairs, reads, writes, start=True, stop=True):
        def fn(g):
            ins = None
            n = len(pairs)
            for i, (l, r) in enumerate(pairs):
                ins = g.matmul(out, l, r, start=(start and i == 0), stop=(stop and i == n - 1))
            return ins
        s.P.op('pe', fn, reads, writes)

    def tr(s, outs_ins, ident, reads, writes):
        def fn(g):
            ins = None
            for o, i in outs_ins:
                ins = g.transpose(o, i, ident)
            return ins
        s.P.op('pe', fn, reads, writes)

    def memset(s, e, ap, val, writes):
        s.P.op(e, lambda g: g.memset(ap, val), (), writes)

    def build(s):
        c, nc, P = s.c, s.nc, s.P
        L = c.DEPTH
        s.I = {}
        s.I['x'] = s.dram('x', (c.NS * c.SEQ, c.D), kind="ExternalInput")
        s.I['ctx'] = s.dram('ctx', (c.NS * c.CTX, c.D), kind="ExternalInput")
        s.I['c'] = s.dram('c', (c.NS, c.D), kind="ExternalInput")
        bs = big_shapes(c)
        for n in BIGW:
            r, w = bs[n]
            s.I[n + '_sh'] = s.dram(n, (r // c.NCORES if c.GATHER else r, w), kind="ExternalInput")
        for n, shp in small_shapes(c).items():
            s.I[n] = s.dram(n, (max(shp[0], 1), shp[1]), kind="ExternalInput")
        for n, a in host_consts(c).items():
            s.I[n] = s.dram(n, a.shape, kind="ExternalInput")
        s.out = s.dram('y', (c.NS * c.SEQ, c.D), kind="ExternalOutput")
        s.W = {}
        s.Wsh = {}
        for n in BIGW:
            r, w = bs[n]
            s.Wsh[n] = s.dram(n + '_b16s', (r // c.NCORES, w), BF16)
            s.W[n] = s.dram(n + '_b16', (r, w), BF16)
        s.xres = s.dram('xres', (c.NT, c.D))
        s.hT = s.dram('hT', (c.D, c.NT), BF16)
        s.pT = s.dram('pT', (c.INC, c.NT))
        s.strm = {n: s.dram('st_' + n, (c.NS, c.H, c.TALL, 64)) for n in
                  ['w0', 'w1', 'kd0', 'kd1', 'ka0', 'ka1', 'nkk', 'r', 'v']}
        s.ysc = [s.dram('ysc%d' % d, (c.NS, c.H, c.TALL, 64)) for d in range(2)]
        s.sgdT = s.dram('sgdT', (c.GR, c.NT))
        s.vfT = s.dram('vfT', (c.BW, c.NT))
        s.brT = [s.dram('brT%d' % i, (c.BW, c.NT), BF16) for i in range(3)]
        s.xn = s.dram('xn', (c.NT, c.D))
        s.modrow = s.dram('modrow', (L * (c.NS + 1), 6 * c.D))
        s.gselk = {'l': s.dram('gsel_l', (c.NS, c.CAPL, c.NE)), 'c': s.dram('gsel_c', (c.NS, c.CAPC, c.NE))}
        s.iselk = {'l': s.dram('isel_l', (c.NS, c.CAPL, c.NE), I32), 'c': s.dram('isel_c', (c.NS, c.CAPC, c.NE), I32)}

        with ExitStack() as g:
            g.enter_context(nc.allow_low_precision("bf16 matmul operands, fp32 accumulation"))
            s.ident = s.sb(g, 'ident', (128, 128))
            s.blk = s.sb(g, 'blk', (128, 128))
            s.swp = s.sb(g, 'swp', (64, 64))
            s.masks = s.sb(g, 'masks', (128, 2, 128), BF16)
            s.masks_f = s.sb(g, 'masks_f', (128, 2, 128))
            s.identb = s.sb(g, 'identb', (128, 128), BF16)
            s.onesb = s.sb(g, 'onesb', (128, 64), BF16)
            s.modcol = s.sb(g, 'modcol', (128, 6 * c.KC, c.NS + 1))
            s.ncol = s.sb(g, 'ncol', (128, 2, c.KC))
            s.gcol = s.sb(g, 'gcol', (128, 2, c.KC, c.NS + 1))
            s.cbt = s.sb(g, 'cbt', (128, 8))
            s.cb = {}
            s.dbg = {}
            s.scr = {'pT': s.pT, 'hT': s.hT, 'xres': s.xres, 'modrow': s.modrow, 'sgdT': s.sgdT, 'vfT': s.vfT,
                     'xn': s.xn, 'ysc0': s.ysc[0], 'ysc1': s.ysc[1], 'brT0': s.brT[0], 'brT1': s.brT[1],
                     'brT2': s.brT[2]}
            for n_ in s.strm:
                s.scr['st_' + n_] = s.strm[n_]
            for k_ in 'lc':
                s.scr['gsel_' + k_] = s.gselk[k_]
                s.scr['isel_' + k_] = s.iselk[k_]
            for n_ in s.debug_out:
                a_ = s.scr[n_]
                s.dbg[n_] = s.dram('dbg_' + n_, a_.shape, a_.dtype, kind="ExternalOutput")
            s.P.prologue()
            s.phase_init()
            done = False
            for l in range(L):
                s.l = l
                s.last = (l == L - 1)
                phases = [('mod', s.phase_mod), ('norm1', s.phase_norm1), ('inproj', s.phase_inproj),
                          ('rwkv_pre', s.phase_rwkv_pre), ('scan', s.phase_scan), ('rwkv_post', s.phase_rwkv_post),
                          ('attn', s.phase_attn), ('conv', s.phase_conv), ('merge', s.phase_merge)]
                for kind in (['l'] if s.last else ['l', 'c']):
                    phases.append(('router_' + kind, (lambda k: lambda: s.phase_router(k))(kind)))
                    phases.append(('moe_' + kind, (lambda k: lambda: s.phase_moe(k))(kind)))
                for name, fn in phases:
                    fn()
                    if s.stop == (l, name):
                        done = True
                        break
                if done:
                    break
            if not done:
                s.phase_final()
            for n_ in s.dbg:
                s.ld(s.dbg[n_], s.scr[n_], ['dbgsrc'], [('dbg', n_)])
            s.P.flush()
            s.P.epilogue()
        return nc

    def phase_init(s):
        c, P = s.c, s.P
        I = s.I
        s.ld(s.ident[:, :], I['k_ident'][:, :], (), [s.ident])
        s.ld(s.blk[:, :], I['k_blk'][:, :], (), [s.blk])
        s.ld(s.swp[:, :], I['k_swp'][:, :], (), [s.swp])
        s.ld(s.masks_f[:, :, :], I['k_masks'].rearrange("(m k) q -> k m q", m=2), (), [s.masks_f])
        s.cp('dve', s.masks[:, :, :], s.masks_f[:, :, :], [s.masks_f], [s.masks])
        s.cp('dve', s.identb[:, :], s.ident[:, :], [s.ident], [s.identb])
        s.memset('dve', s.onesb[:, :], 1.0, [s.onesb])
        nct = c.NS * c.CTX
        s.ld(s.xres[0:nct, :], I['ctx'][:, :], (), ['xres'])
        R = c.NS * c.SEQ
        step = max(R // 4, 128)
        for r0 in range(0, R, step):
            s.ld(s.xres[nct + r0:nct + r0 + step, :], I['x'][r0:r0 + step, :], (), [('xres', r0)])
        bs = big_shapes(c)
        for n in BIGW:
            r = bs[n][0] // c.NCORES if c.GATHER else bs[n][0]
            step = max(r // (4 if c.GATHER else 32), 1)
            dstw = s.Wsh[n] if c.GATHER else s.W[n]
            for r0 in range(0, r, step):
                s.P.dma('pool', (lambda dstw, n, r0, step: lambda e: e.dma_start(
                    out=dstw[r0:r0 + step, :], in_=I[n + '_sh'][r0:r0 + step, :]))(dstw, n, r0, step),
                    (), [('wsh', n, r0)])
        P.flush()
        for n in (BIGW if c.GATHER else []):
            s.P.dma('pool', (lambda n: lambda e: e.collective_compute(
                "AllGather", ALU.bypass, replica_groups=[list(range(c.NCORES))],
                ins=[s.Wsh[n].opt()], outs=[s.W[n].opt()]))(n), (), [('wfull', n)], inc=1)
        P.flush()

    def phase_mod(s):
        c, P, l = s.c, s.P, s.l
        NS1 = c.NS + 1
        M6 = 6 * c.D
        with ExitStack() as es:
            cT = s.sb(es, 'cT', (128, c.KC, NS1))
            cTb = s.sb(es, 'cTb', (128, c.KC, NS1), BF16)
            for sc in range(c.NS):
                s.ldnc(cT[:, :, sc], s.I['c'][sc:sc + 1, :].rearrange("s (k p) -> p (s k)", p=128), [cT], [cT])
            s.ldnc(cT[:, :, c.NS], s.I['c_ctx'].rearrange("s (k p) -> p (s k)", p=128), [cT], [cT])
            s.act(cTb[:, :, :], cT[:, :, :], AF.Silu, [cT], [cTb])
            brow = s.sb(es, 'brow', (NS1, M6))
            s.ld(brow[:, :], AP(s.I['b_mod'], l * M6, [[0, NS1], [1, M6]]), (), [brow])
            mrow = s.sb(es, 'mrow', (NS1, M6))
            wb = [s.sb(es, 'wmod%d' % i, (128, c.KC, 512), BF16) for i in range(2)]
            pm = [s.ps(es, 'pmod%d' % i, (NS1, 512)) for i in range(2)]
            wsrc = s.W['w_mod'][l * c.D:(l + 1) * c.D, :].rearrange("(k p) n -> p k n", p=128)
            ng = M6 // 512
            for gi in range(ng):
                w = wb[gi % 2]
                p_ = pm[gi % 2]
                s.ld(w[:, :, :], wsrc[:, :, gi * 512:(gi + 1) * 512], (), [w])
                s.mm(p_[:, :], [(cTb[:, k, :], w[:, k, :]) for k in range(c.KC)], [cTb, w], [p_])
                s.tt('dve', mrow[:, gi * 512:(gi + 1) * 512], p_[:, :], brow[:, gi * 512:(gi + 1) * 512], ALU.add,
                     [p_, brow], [(mrow, gi)])
            allm = [(mrow, gi) for gi in range(ng)]
            s.ld(s.modrow[l * NS1:(l + 1) * NS1, :], mrow[:, :], allm, [('modrow', l)])
            pc = s.ps(es, 'pcol', (128, 6 * c.KC, NS1))
            nch = 6 * c.KC
            s.tr([(pc[:, j, :], mrow[:, j * 128:(j + 1) * 128]) for j in range(nch)], s.ident[0:NS1, 0:NS1],
                 allm + [s.ident], [pc])
            s.cp('dve', s.modcol[:, :, :], pc[:, :, :], [pc], [s.modcol])
            s.ldnc(s.ncol[:, 0, :], s.I['norm_mix'][l:l + 1, :].rearrange("o (k p) -> p (o k)", p=128), (), [s.ncol])
            s.ldnc(s.ncol[:, 1, :], s.I['norm_ffn'][l:l + 1, :].rearrange("o (k p) -> p (o k)", p=128), [s.ncol],
                   [s.ncol])
            KC = c.KC
            for j, mi in ((0, 1), (1, 4)):
                for sc in range(NS1):
                    s.stt('dve', s.gcol[:, j, :, sc], s.modcol[:, mi * KC:(mi + 1) * KC, sc], 1.0, s.ncol[:, j, :],
                          ALU.add, ALU.mult, [s.modcol, s.ncol], [s.gcol])
            P.flush()

    def norm_tile(s, es_t, x_ap, tag):
        pass

    def phase_norm1(s):
        c, P, l = s.c, s.P, s.l
        KC = c.KC
        with ExitStack() as es:
            xt = [s.sb(es, 'n1x%d' % i, (128, c.D)) for i in range(2)]
            junk = s.sb(es, 'n1j', (128, c.D))
            ss = [s.sb(es, 'n1s%d' % i, (128, 2)) for i in range(2)]
            hst = [s.sb(es, 'n1h%d' % i, (128, KC, 128), BF16) for i in range(2)]
            tmp = [s.sb(es, 'n1t%d' % i, (128, 4, 128)) for i in range(2)]
            pt = [s.ps(es, 'n1p%d' % i, (128, 4, 128)) for i in range(4)]
            it = 0
            pi = 0
            for (kind, b, t0s, ln, mc) in c.seqs:
                for t0 in range(t0s, t0s + ln, 128):
                    x, sq, h = xt[it % 2], ss[it % 2], hst[it % 2]
                    s.ld(x[:, :], s.xres[t0:t0 + 128, :], ['xres'], [x])
                    s.act(junk[:, :], x[:, :], AF.Square, [x], [junk, sq], accum=sq[:, 0:1])
                    s.rsqrt(sq[:, 1:2], sq[:, 0:1], 1.0 / c.D, 1e-6, [sq], [sq])
                    s.ts('dve', x[:, :], x[:, :], sq[:, 1:2], None, ALU.mult, None, [x, sq], [x])
                    for q4 in range(KC // 4):
                        p_ = pt[pi % 4]
                        tm = tmp[pi % 2]
                        pi += 1
                        s.tr([(p_[:, j, :], x[:, (q4 * 4 + j) * 128:(q4 * 4 + j + 1) * 128]) for j in range(4)],
                             s.ident[:, :], [x, s.ident], [p_])
                        gb = s.gcol[:, 0, q4 * 4:q4 * 4 + 4, mc]
                        gb = AP(gb, gb.offset, [gb.ap[0], gb.ap[1], [0, 128]])
                        sb_ = s.modcol[:, q4 * 4:q4 * 4 + 4, mc]
                        sb_ = AP(sb_, sb_.offset, [sb_.ap[0], sb_.ap[1], [0, 128]])
                        s.tt('dve', tm[:, :, :], p_[:, :, :], gb, ALU.mult, [p_, s.gcol], [tm])
                        s.tt('pool', h[:, q4 * 4:q4 * 4 + 4, :], tm[:, :, :], sb_, ALU.add, [tm, s.modcol], [h])
                    s.ld(s.hT[:, t0:t0 + 128].rearrange("(k p) t -> p k t", p=128), h[:, :, :], [h], [('hT', t0)])
                    it += 1
            P.flush()

    def phase_inproj(s):
        c, P, l = s.c, s.P, s.l
        KC = c.KC
        G = 512
        nchunk = c.INC // 128
        sig0 = c.CONVEND // 128
        with ExitStack() as es:
            hb = [s.sb(es, 'iph%d' % i, (128, KC, G), BF16) for i in range(2)]
            wb = [s.sb(es, 'ipw%d' % i, (128, KC, 512), BF16) for i in range(3)]
            ob = [s.sb(es, 'ipo%d' % i, (128, 4, G)) for i in range(2)]
            pp = [s.ps(es, 'ipp%d' % i, (128, G)) for i in range(4)]
            wsrc = s.W['w_in'][l * c.D:(l + 1) * c.D, :].rearrange("(k p) n -> p k n", p=128)
            wi = 0
            pi = 0
            oi = 0
            for gi, t0 in enumerate(range(0, c.NT, G)):
                h = hb[gi % 2]
                n = min(G, c.NT - t0)
                s.ld(h[:, :, 0:n], s.hT[:, t0:t0 + n].rearrange("(k p) t -> p k t", p=128), ['hT_all'], [h])
                nch = nchunk
                if s.last and t0 + n <= c.NS * c.CTX:
                    nch = c.CTXC // 128
                for c0 in range(0, nch, 4):
                    n4 = min(4, nch - c0)
                    w = wb[wi % 3]
                    wi += 1
                    s.ld(w[:, :, 0:n4 * 128], wsrc[:, :, c0 * 128:(c0 + n4) * 128], ['wfull_in'], [w])
                    o = ob[oi % 2]
                    oi += 1
                    for j in range(n4):
                        p_ = pp[pi % 4]
                        pi += 1
                        s.mm(p_[:, 0:n], [(w[:, k, j * 128:(j + 1) * 128], h[:, k, 0:n]) for k in range(KC)], [w, h], [p_])
                        if c0 + j >= sig0:
                            s.act(o[:, j, 0:n], p_[:, 0:n], AF.Sigmoid, [p_], [(o, j)])
                        elif (c0 + j) % 2 == 0:
                            s.cp('act', o[:, j, 0:n], p_[:, 0:n], [p_], [(o, j)])
                        else:
                            s.cp('dve', o[:, j, 0:n], p_[:, 0:n], [p_], [(o, j)])
                    s.ld(s.pT[c0 * 128:(c0 + n4) * 128, t0:t0 + n].rearrange("(j p) t -> p j t", p=128),
                         o[:, 0:n4, 0:n], [(o, j) for j in range(n4)], [('pT', c0, t0)] + [(o, j) for j in range(n4)])
            P.flush()

    def load_halo(s, tile, n, row0, t0, seg, s0, s1):
        lo = max(t0 - 1, s0)
        hi = min(t0 + seg + 1, s1)
        a = lo - (t0 - 1)
        if a > 0:
            s.memset('pool', tile[0:n, 0:1], 0.0, [tile])
        if hi < t0 + seg + 1:
            s.memset('pool', tile[0:n, seg + 1:seg + 2], 0.0, [tile])
        s.ld(tile[0:n, a:a + (hi - lo)], s.pT[row0:row0 + n, lo:hi], ['pT_all'], [tile])

    def colvec(s, tile_col, vec_ap_1d_len_n):
        pass

    def phase_rwkv_pre(s):
        c, P, l = s.c, s.P, s.l
        I = s.I
        BC = c.BC
        BW = c.BW
        with ExitStack() as es:
            blocks = []
            for j in range(3 * BC):
                blocks.append((j * 128, 128))
            base = 3 * BW
            for j in range(2):
                blocks.append((base + j * c.DR, c.DR))
            base += 2 * c.DR
            for j in range(2):
                blocks.append((base + j * c.IR, c.IR))
            base += 2 * c.IR
            for j in range(c.GR // 128):
                blocks.append((base + j * 128, 128))
            NB = len(blocks)
            mu = s.sb(es, 'mu', (128, NB))
            omm = s.sb(es, 'omm', (128, NB))
            hmu = s.sb(es, 'hmu', (128, NB))
            s.memset('dve', mu[:, :], 0.0, [mu])
            for j, (r0, n) in enumerate(blocks):
                s.ldnc(mu[0:n, j:j + 1], I['shift_mu'][l:l + 1, r0:r0 + n].rearrange("o n -> n o"), [mu], [mu])
            s.ts('dve', omm[:, :], mu[:, :], -1.0, 1.0, ALU.mult, ALU.add, [mu], [omm])
            s.ts('dve', hmu[:, :], mu[:, :], 0.5, None, ALU.mult, None, [mu], [hmu])
            pc = s.sb(es, 'pcols', (128, 8, BC))
            def colload(idx, src2d_row):
                s.ldnc(pc[:, idx, :], src2d_row.rearrange("o (k p) -> p (o k)", p=128), [pc], [pc])
            s.memset('dve', pc[:, :, :], 0.0, [pc])
            colload(0, I['k_k'][l:l + 1, :])
            colload(1, I['k_a'][l:l + 1, :])
            for d in range(2):
                colload(3 + d, I['decay_bias'][2 * l + d:2 * l + d + 1, :])
                colload(5 + d, I['iclr_bias'][2 * l + d:2 * l + d + 1, :])
            if l > 0:
                colload(7, I['vres_bias'][l - 1:l, :])
            s.ts('dve', pc[:, 2, :], pc[:, 1, :], -1.0, 1.0, ALU.mult, ALU.add, [pc], [pc])
            dup = [s.sb(es, 'dup%d' % d, (c.DR, BW)) for d in range(2)]
            iup = [s.sb(es, 'iup%d' % d, (c.IR, BW)) for d in range(2)]
            for d in range(2):
                s.ld(dup[d][:, :], I['decay_up'][(2 * l + d) * c.DR:(2 * l + d + 1) * c.DR, :], (), [dup[d]])
                s.ld(iup[d][:, :], I['iclr_up'][(2 * l + d) * c.IR:(2 * l + d + 1) * c.IR, :], (), [iup[d]])
            if l > 0:
                vdn = s.sb(es, 'vdn', (128, BC, c.VR))
                vup = s.sb(es, 'vup', (c.VR, BW))
                s.ld(vdn[:, :, :], I['vres_down'][(l - 1) * BW:l * BW, :].rearrange("(k p) r -> p k r", p=128), (), [vdn])
                s.ld(vup[:, :], I['vres_up'][(l - 1) * c.VR:l * c.VR, :], (), [vup])
            SEG = 512
            NTB = 4
            Pin = [s.sb(es, 'rpP%d' % i, (128, SEG + 2)) for i in range(3)]
            tsum = [s.sb(es, 'rpT%d' % i, (128, SEG)) for i in range(2)]
            wdT = [s.sb(es, 'wdT%d' % d, (c.DR, SEG)) for d in range(2)]
            adT = [s.sb(es, 'adT%d' % d, (c.IR, SEG)) for d in range(2)]
            sg = [s.sb(es, 'sg%d' % j, (128, SEG)) for j in range(c.GR // 128)]
            vall = s.sb(es, 'vall', (128, BC, SEG))
            rt = s.sb(es, 'rt', (128, SEG))
            kt = s.sb(es, 'kt', (128, SEG))
            kh = s.sb(es, 'kh', (128, SEG))
            sq = s.sb(es, 'sqk', (128, SEG))
            kk = s.sb(es, 'kk', (128, SEG))
            nkk = s.sb(es, 'nkk', (128, SEG))
            wt = [s.sb(es, 'wt%d' % d, (128, SEG)) for d in range(2)]
            at = [s.sb(es, 'at%d' % d, (128, SEG)) for d in range(2)]
            kdt = [s.sb(es, 'kdt%d' % d, (128, SEG)) for d in range(2)]
            kat = [s.sb(es, 'kat%d' % d, (128, SEG)) for d in range(2)]
            vf = s.sb(es, 'vf', (128, SEG))
            vg = s.sb(es, 'vg', (128, SEG))
            vdT = s.sb(es, 'vdT', (c.VR, SEG))
            stg = [s.sb(es, 'stg%d' % i, (128, NTB, 128)) for i in range(3)]
            pA = [s.ps(es, 'rpA%d' % i, (128, SEG)) for i in range(3)]
            pTr = [s.ps(es, 'rpTr%d' % i, (128, NTB, 128)) for i in range(3)]
            pV = s.ps(es, 'rpV', (c.VR, SEG))
            cnt = {'p': 0, 't': 0, 'a': 0, 'tr': 0}

            def shift(dst, blk, t0, seg, s0, s1, res=None):
                res = res if res is not None else dst
                r0, n = blocks[blk]
                Pt = Pin[cnt['p'] % 3]
                cnt['p'] += 1
                ts_ = tsum[cnt['t'] % 2]
                cnt['t'] += 1
                s.load_halo(Pt, n, r0, t0, seg, s0, s1)
                s.tt('pool', ts_[0:n, 0:seg], Pt[0:n, 0:seg], Pt[0:n, 2:seg + 2], ALU.add, [Pt], [ts_])
                s.act(dst[0:n, 0:seg], Pt[0:n, 1:seg + 1], AF.Copy, [Pt, omm], [res], scale=omm[0:n, blk:blk + 1])
                s.stt('dve', dst[0:n, 0:seg], ts_[0:n, 0:seg], hmu[0:n, blk:blk + 1], dst[0:n, 0:seg],
                      ALU.mult, ALU.add, [ts_, res, hmu], [res])

            def emit_stream(name, tile, b, ch, tall0, seg, res=None):
                res = res if res is not None else tile
                ntb = seg // 128
                p_ = pTr[cnt['tr'] % 3]
                st = stg[cnt['tr'] % 3]
                cnt['tr'] += 1
                s.tr([(p_[:, j, :], tile[:, j * 128:(j + 1) * 128]) for j in range(ntb)], s.ident[:, :],
                     [res, s.ident], [p_])
                s.cp('act' if cnt['tr'] % 2 else 'dve', st[:, 0:ntb, :], p_[:, 0:ntb, :], [p_], [st])
                for tb in range(ntb):
                    dst = s.strm[name][b, 2 * ch:2 * ch + 2, tall0 + tb * 128:tall0 + (tb + 1) * 128, :].rearrange(
                        "h p j -> p h j")
                    src = st[:, tb, :].rearrange("p (h j) -> p h j", j=64)
                    s.ld(dst, src, [st], [('strm', name, b, ch, tall0, tb)])

            for (kind, b, t0s, ln, mc) in c.seqs:
                tallb = 0 if kind == 'c' else c.CTX
                for t0 in range(t0s, t0s + ln, SEG):
                    seg = min(SEG, t0s + ln - t0)
                    tall0 = tallb + (t0 - t0s)
                    a = (t0, seg, t0s, t0s + ln)
                    for d in range(2):
                        shift(wdT[d], 3 * BC + d, *a)
                        s.act(wdT[d][:, 0:seg], wdT[d][:, 0:seg], AF.Tanh, [wdT[d]], [wdT[d]])
                        shift(adT[d], 3 * BC + 2 + d, *a)
                    for j in range(c.GR // 128):
                        shift(sg[j], 3 * BC + 4 + j, *a)
                        s.act(sg[j][:, 0:seg], sg[j][:, 0:seg], AF.Sigmoid, [sg[j]], [sg[j]])
                        s.ld(s.sgdT[j * 128:(j + 1) * 128, t0:t0 + seg], sg[j][:, 0:seg], [sg[j]], [('sgdT', j, t0)])
                    for ch in range(BC):
                        shift(vall[:, ch, :], 2 * BC + ch, *a, res=vall)
                    if l == 0:
                        for ch in range(BC):
                            s.ld(s.vfT[ch * 128:(ch + 1) * 128, t0:t0 + seg], vall[:, ch, 0:seg], [vall],
                                 [('vfT', ch, t0)])
                    else:
                        s.mm(pV[:, 0:seg], [(vdn[:, ch, :], vall[:, ch, 0:seg]) for ch in range(BC)], [vdn, vall], [pV])
                        s.cp('dve', vdT[:, 0:seg], pV[:, 0:seg], [pV], [vdT])
                        for ch in range(BC):
                            p_ = pA[cnt['a'] % 3]
                            cnt['a'] += 1
                            s.mm(p_[:, 0:seg], [(vup[:, ch * 128:(ch + 1) * 128], vdT[:, 0:seg])], [vup, vdT], [p_])
                            s.act(vg[:, 0:seg], p_[:, 0:seg], AF.Sigmoid, [p_, pc], [vg], bias=pc[:, 7, ch:ch + 1])
                            s.ld(vf[:, 0:seg], s.vfT[ch * 128:(ch + 1) * 128, t0:t0 + seg], ['vfT_all'], [vf])
                            s.tt('dve', vf[:, 0:seg], vf[:, 0:seg], vall[:, ch, 0:seg], ALU.subtract, [vf, vall], [vf])
                            s.tt('dve', vf[:, 0:seg], vf[:, 0:seg], vg[:, 0:seg], ALU.mult, [vf, vg], [vf])
                            s.tt('dve', vall[:, ch, 0:seg], vall[:, ch, 0:seg], vf[:, 0:seg], ALU.add, [vf, vall], [vall])
                    for ch in range(BC):
                        shift(rt, ch, *a)
                        shift(kt, BC + ch, *a)
                        s.ts('dve', kh[:, 0:seg], kt[:, 0:seg], pc[:, 0, ch:ch + 1], None, ALU.mult, None, [kt, pc], [kh])
                        s.tt('pool', sq[:, 0:seg], kh[:, 0:seg], kh[:, 0:seg], ALU.mult, [kh], [sq])
                        p_ = pA[cnt['a'] % 3]
                        cnt['a'] += 1
                        s.mm(p_[:, 0:seg], [(s.blk[:, :], sq[:, 0:seg])], [s.blk, sq], [p_])
                        s.ts('dve', sq[:, 0:seg], p_[:, 0:seg], 1e-24, None, ALU.max, None, [p_], [sq])
                        s.rsqrt(sq[:, 0:seg], sq[:, 0:seg], 1.0, 0.0, [sq], [sq])
                        s.tt('dve', kk[:, 0:seg], kh[:, 0:seg], sq[:, 0:seg], ALU.mult, [kh, sq], [kk])
                        s.ts('pool', nkk[:, 0:seg], kk[:, 0:seg], -1.0, None, ALU.mult, None, [kk], [nkk])
                        for d in range(2):
                            p_ = pA[cnt['a'] % 3]
                            cnt['a'] += 1
                            s.mm(p_[:, 0:seg], [(dup[d][:, ch * 128:(ch + 1) * 128], wdT[d][:, 0:seg])],
                                 [dup[d], wdT[d]], [p_])
                            s.act(wt[d][:, 0:seg], p_[:, 0:seg], AF.Sigmoid, [p_, pc], [wt[d]],
                                  bias=pc[:, 3 + d, ch:ch + 1])
                            s.act(wt[d][:, 0:seg], wt[d][:, 0:seg], AF.Exp, [wt[d]], [wt[d]],
                                  scale=-0.6065306597126334)
                            p_ = pA[cnt['a'] % 3]
                            cnt['a'] += 1
                            s.mm(p_[:, 0:seg], [(iup[d][:, ch * 128:(ch + 1) * 128], adT[d][:, 0:seg])],
                                 [iup[d], adT[d]], [p_])
                            s.act(at[d][:, 0:seg], p_[:, 0:seg], AF.Sigmoid, [p_, pc], [at[d]],
                                  bias=pc[:, 5 + d, ch:ch + 1])
                            s.ts('dve', kdt[d][:, 0:seg], at[d][:, 0:seg], pc[:, 1, ch:ch + 1], pc[:, 2, ch:ch + 1],
                                 ALU.mult, ALU.add, [at[d], pc], [kdt[d]])
                            s.tt('dve', kdt[d][:, 0:seg], kdt[d][:, 0:seg], kt[:, 0:seg], ALU.mult, [kdt[d], kt],
                                 [kdt[d]])
                            s.tt('pool', kat[d][:, 0:seg], kk[:, 0:seg], at[d][:, 0:seg], ALU.mult, [kk, at[d]],
                                 [kat[d]])
                            emit_stream('w%d' % d, wt[d], b, ch, tall0, seg)
                            emit_stream('kd%d' % d, kdt[d], b, ch, tall0, seg)
                            emit_stream('ka%d' % d, kat[d], b, ch, tall0, seg)
                        emit_stream('nkk', nkk, b, ch, tall0, seg)
                        emit_stream('r', rt, b, ch, tall0, seg)
                        emit_stream('v', vall[:, ch, :], b, ch, tall0, seg, res=vall)
            P.flush()

    def phase_scan(s):
        c, P = s.c, s.P
        TC = 16
        IQ, IP, H, NS = c.IQ, c.IP, c.H, c.NS
        NBH = NS * H
        names = ['nkk', 'ka', 'w', 'kd', 'r']
        with ExitStack() as es:
            S = [s.sb(es, 'S%d' % i, (128, 2, IP, 64)) for i in range(2)]
            t1 = s.sb(es, 'sc_t1', (128, 2, IP, 64))
            t2 = s.sb(es, 'sc_t2', (128, 2, IP, 64))
            Sw = s.sb(es, 'sc_sw', (128, 2, IP, 64))
            t3 = [s.sb(es, 'sc_t3%d' % i, (128, 2, IP, 64)) for i in range(2)]
            t4 = [s.sb(es, 'sc_t4%d' % i, (128, 2, IP, 64)) for i in range(2)]
            sa = s.sb(es, 'sc_sa', (128, 2, IP))
            st = {n: [s.sb(es, 'sc_%s%d' % (n, i), (128, 2, TC, 64)) for i in range(2)] for n in names}
            vt = [s.sb(es, 'sc_v%d' % i, (128, 2, TC, IP)) for i in range(2)]
            yb = [s.sb(es, 'sc_y%d' % i, (128, 2, TC, IP)) for i in range(2)]
            for hf in range(2):
                s.memset('dve', S[0][:, hf, :, :], 0.0, [(S[0], hf)])
            cur = 0
            step = 0
            ci = 0
            pending = []
            stores = []

            def flush_pending(which):
                for fn in pending:
                    fn(which)

            for (tb0, T) in ((0, c.CTX), (c.CTX, c.SEQ)):
                for ck in range(T // TC):
                    k = ci % 2
                    ci += 1
                    tf = tb0 + ck * TC
                    tr_ = tb0 + T - (ck + 1) * TC
                    for n in names:
                        for d in range(2):
                            tt0 = tf if d == 0 else tr_
                            nm = n if n in ('nkk', 'r') else '%s%d' % (n, d)
                            src = s.strm[nm][:, :, tt0:tt0 + TC, :].rearrange("b h t j -> (b h) t j")
                            for iq in range(IQ):
                                s.ld(st[n][k][iq * NBH:(iq + 1) * NBH, d, :, :], src, ['strm_all'], [(st[n][k], d)])
                    for d in range(2):
                        tt0 = tf if d == 0 else tr_
                        for iq in range(IQ):
                            src = s.strm['v'][:, :, tt0:tt0 + TC, iq * IP:(iq + 1) * IP].rearrange(
                                "b h t j -> (b h) t j")
                            s.ldnc(vt[k][iq * NBH:(iq + 1) * NBH, d, :, :], src, ['strm_all'], [(vt[k], d)])
                    y = yb[k]
                    for sp in range(TC):
                        So, Sn = S[cur], S[1 - cur]
                        T3 = t3[step % 2]
                        T4 = t4[step % 2]

                        def jb(tile, hf):
                            a = tile[:, hf, sp if hf == 0 else TC - 1 - sp, :]
                            return AP(a, a.offset, [a.ap[0], [0, IP], [1, 64]])

                        def ib(tile, hf):
                            a = tile[:, hf, sp if hf == 0 else TC - 1 - sp, :]
                            return AP(a, a.offset, [a.ap[0], [1, IP], [0, 64]])

                        for hf in range(2):
                            s.tt('pool', T3[:, hf, :, :], ib(vt[k], hf), jb(st['kd'][k], hf), ALU.mult,
                                 [(vt[k], hf), (st['kd'][k], hf)], [(T3, hf)])
                        for hf in range(2):
                            s.tt('pool', Sw[:, hf, :, :], So[:, hf, :, :], jb(st['w'][k], hf), ALU.mult,
                                 [(So, hf), (st['w'][k], hf)], [(Sw, hf)])
                        for hf in range(2):
                            s.tt('dve', t1[:, hf, :, :], So[:, hf, :, :], jb(st['nkk'][k], hf), ALU.mult,
                                 [(So, hf), (st['nkk'][k], hf)], [(t1, hf)])
                        for hf in range(2):
                            s.red(sa[:, hf, :], t1[:, hf, :, :], [(t1, hf)], [(sa, hf)])
                        flush_pending('pool')
                        for hf in range(2):
                            a = sa[:, hf, :]
                            sab = AP(a, a.offset, [a.ap[0], [1, IP], [0, 64]])
                            s.tt('dve', t2[:, hf, :, :], sab, jb(st['ka'][k], hf), ALU.mult,
                                 [(sa, hf), (st['ka'][k], hf)], [(t2, hf)])
                        for hf in range(2):
                            s.tt('dve', Sn[:, hf, :, :], Sw[:, hf, :, :], t2[:, hf, :, :], ALU.add,
                                 [(Sw, hf), (t2, hf)], [(Sn, hf)])
                        for hf in range(2):
                            s.tt('dve', Sn[:, hf, :, :], Sn[:, hf, :, :], T3[:, hf, :, :], ALU.add,
                                 [(Sn, hf), (T3, hf)], [(Sn, hf)])
                        flush_pending('dve')
                        pending.clear()
                        for fn in stores:
                            fn()
                        stores.clear()

                        def mk(Sn, T4, y, k, sp):
                            def fn(which):
                                for hf in range(2):
                                    if which == 'pool':
                                        a = st['r'][k][:, hf, sp if hf == 0 else TC - 1 - sp, :]
                                        rb = AP(a, a.offset, [a.ap[0], [0, IP], [1, 64]])
                                        s.tt('pool', T4[:, hf, :, :], Sn[:, hf, :, :], rb, ALU.mult,
                                             [(Sn, hf), (st['r'][k], hf)], [(T4, hf)])
                                    else:
                                        s.red(y[:, hf, sp if hf == 0 else TC - 1 - sp, :], T4[:, hf, :, :], [(T4, hf)],
                                              [(y, hf)])
                            return fn
                        pending.append(mk(Sn, T4, y, k, sp))
                        cur = 1 - cur
                        step += 1

                    def mkstore(y, tf, tr_):
                        def fn():
                            for d in range(2):
                                tt0 = tf if d == 0 else tr_
                                for iq in range(IQ):
                                    dst = s.ysc[d][:, :, tt0:tt0 + TC, iq * IP:(iq + 1) * IP].rearrange(
                                        "b h t j -> (b h) t j")
                                    s.ldnc(dst, y[iq * NBH:(iq + 1) * NBH, d, :, :], [(y, d)], [('ysc', d, tt0, iq)])
                        return fn
                    stores.append(mkstore(y, tf, tr_))
            flush_pending('pool')
            flush_pending('dve')
            for fn in stores:
                fn()
            P.flush()

    def phase_rwkv_post(s):
        c, P, l = s.c, s.P, s.l
        I = s.I
        BW, H, BC = c.BW, c.H, c.BC
        with ExitStack() as es:
            def rowb(name, src_row):
                t = s.sb(es, name, (128, BW))
                s.ld(t[:, :], AP(src_row, src_row.offset, [[0, 128], [1, BW]]), (), [t])
                return t
            gnw = rowb('gnw', I['gn_w'][l:l + 1, :])
            gnb = rowb('gnb', I['gn_b'][l:l + 1, :])
            rkr = rowb('rkr', I['r_k'][l:l + 1, :])
            gup = s.sb(es, 'gup', (128, c.GR // 128, BW))
            s.ld(gup[:, :, :], I['gate_up'][l * c.GR:(l + 1) * c.GR, :].rearrange("(k p) n -> p k n", p=128), (), [gup])
            nb = 2
            y0 = [s.sb(es, 'po_y0%d' % i, (128, H, 64)) for i in range(nb)]
            y1 = [s.sb(es, 'po_y1%d' % i, (128, H, 64)) for i in range(nb)]
            rr = [s.sb(es, 'po_r%d' % i, (128, H, 64)) for i in range(nb)]
            k0 = [s.sb(es, 'po_k0%d' % i, (128, H, 64)) for i in range(nb)]
            k1 = [s.sb(es, 'po_k1%d' % i, (128, H, 64)) for i in range(nb)]
            vv = [s.sb(es, 'po_v%d' % i, (128, H, 64)) for i in range(nb)]
            sgt = [s.sb(es, 'po_sg%d' % i, (128, c.GR // 128, 128)) for i in range(nb)]
            sq = s.sb(es, 'po_sq', (128, H, 64))
            st = s.sb(es, 'po_st', (128, 4, H))
            ob = [s.sb(es, 'po_o%d' % i, (128, BC, 128), BF16) for i in range(2)]
            pg = [s.ps(es, 'po_pg%d' % i, (128, 512)) for i in range(max(BW // 512, 1))]
            ptr = [s.ps(es, 'po_pt%d' % i, (128, 4, 128)) for i in range(2)]
            it = 0
            for (kind, b, t0s, ln, mc) in c.seqs:
                if s.last and kind == 'c':
                    continue
                tallb = 0 if kind == 'c' else c.CTX
                for t0 in range(t0s, t0s + ln, 128):
                    k = it % nb
                    it += 1
                    ta = tallb + (t0 - t0s)
                    def tok(dr):
                        return dr[b, :, ta:ta + 128, :].rearrange("h t j -> t h j")
                    s.ld(y0[k][:, :, :], tok(s.ysc[0]), ['ysc_all'], [y0[k]])
                    s.ld(y1[k][:, :, :], tok(s.ysc[1]), ['ysc_all'], [y1[k]])
                    s.ld(rr[k][:, :, :], tok(s.strm['r']), ['strm_all'], [rr[k]])
                    s.ld(k0[k][:, :, :], tok(s.strm['kd0']), ['strm_all'], [k0[k]])
                    s.ld(k1[k][:, :, :], tok(s.strm['kd1']), ['strm_all'], [k1[k]])
                    s.ld(vv[k][:, :, :], tok(s.strm['v']), ['strm_all'], [vv[k]])
                    s.ld(sgt[k][:, :, :], s.sgdT[:, t0:t0 + 128].rearrange("(k p) t -> p k t", p=128), ['sgdT_all'],
                         [sgt[k]])
                    Y, Y1, R, K0, K1, V = y0[k], y1[k], rr[k], k0[k], k1[k], vv[k]
                    def hb(ap2):
                        return AP(ap2, ap2.offset, [ap2.ap[0], ap2.ap[1], [0, 64]])
                    s.tt('dve', Y[:, :, :], Y[:, :, :], Y1[:, :, :], ALU.add, [Y, Y1], [Y])
                    s.red(st[:, 0, :], Y[:, :, :], [Y], [st])
                    s.ts('dve', st[:, 0, :], st[:, 0, :], 1.0 / 64, None, ALU.mult, None, [st], [st])
                    s.tt('dve', Y[:, :, :], Y[:, :, :], hb(st[:, 0, :]), ALU.subtract, [Y, st], [Y])
                    s.tt('pool', sq[:, :, :], Y[:, :, :], Y[:, :, :], ALU.mult, [Y], [sq])
                    s.red(st[:, 1, :], sq[:, :, :], [sq], [st])
                    s.rsqrt(st[:, 1, :], st[:, 1, :], 1.0 / 64, 64e-5, [st], [st])
                    s.tt('dve', Y[:, :, :], Y[:, :, :], hb(st[:, 1, :]), ALU.mult, [Y, st], [Y])
                    Yf = Y[:, :, :].rearrange("p h j -> p (h j)")
                    s.tt('pool', Yf, Yf, gnw[:, :], ALU.mult, [Y, gnw], [Y])
                    s.tt('pool', Yf, Yf, gnb[:, :], ALU.add, [Y, gnb], [Y])
                    s.tt('pool', K0[:, :, :], K0[:, :, :], K1[:, :, :], ALU.add, [K0, K1], [K0])
                    s.tt('pool', K0[:, :, :], K0[:, :, :], R[:, :, :], ALU.mult, [K0, R], [K0])
                    K0f = K0[:, :, :].rearrange("p h j -> p (h j)")
                    s.tt('pool', K0f, K0f, rkr[:, :], ALU.mult, [K0, rkr], [K0])
                    s.red(st[:, 2, :], K0[:, :, :], [K0], [st])
                    s.tt('dve', V[:, :, :], V[:, :, :], hb(st[:, 2, :]), ALU.mult, [V, st], [V])
                    s.tt('dve', Y[:, :, :], Y[:, :, :], V[:, :, :], ALU.add, [Y, V], [Y])
                    for gi in range(len(pg)):
                        n0 = gi * 512
                        n1 = min(BW, n0 + 512)
                        s.mm(pg[gi][:, 0:n1 - n0], [(sgt[k][:, kk_, :], gup[:, kk_, n0:n1]) for kk_ in range(c.GR // 128)],
                             [sgt[k], gup], [pg[gi]])
                        s.tt('dve', Yf[:, n0:n1], Yf[:, n0:n1], pg[gi][:, 0:n1 - n0], ALU.mult, [Y, pg[gi]], [Y])
                    o = ob[it % 2]
                    for q4 in range(0, BC, 4):
                        n4 = min(4, BC - q4)
                        p_ = ptr[(q4 // 4) % 2]
                        s.tr([(p_[:, j, :], Yf[:, (q4 + j) * 128:(q4 + j + 1) * 128]) for j in range(n4)], s.ident[:, :],
                             [Y, s.ident], [p_])
                        s.cp('act', o[:, q4:q4 + n4, :], p_[:, 0:n4, :], [p_], [o])
                    s.ld(s.brT[0][:, t0:t0 + 128].rearrange("(k p) t -> p k t", p=128), o[:, :, :], [o], [('br0', t0)])
            P.flush()

    def phase_attn(s):
        c, P, l = s.c, s.P, s.l
        I = s.I
        KVH, H = c.KVH, c.H
        TA = c.TALL
        NBK = TA // 128
        NCB = c.CTX // 128
        G = 4
        with ExitStack() as es:
            rope = s.sb(es, 'rope', (64, 2, c.SEQ))
            s.ld(rope[:, :, :], I['k_rope'].rearrange("(a n) t -> n a t", a=2), (), [rope])
            esk = s.sb(es, 'esk', (64, H))
            s.ld(esk[:, :], AP(I['attn_sink'], l * H, [[0, 64], [1, H]]), (), [esk])
            s.act(esk[:, :], esk[:, :], AF.Exp, [esk], [esk])
            kT = s.sb(es, 'kT', (64, KVH, TA), BF16)
            qT = s.sb(es, 'qT', (64, G, TA), BF16)
            vtk = s.sb(es, 'vtk', (128, NBK, KVH, 64), BF16)
            xin = [s.sb(es, 'at_x%d' % i, (64, 512)) for i in range(3)]
            t1 = [s.sb(es, 'at_t%d' % i, (64, 512)) for i in range(2)]
            eb = [s.sb(es, 'at_e%d' % i, (128, G, 128), BF16) for i in range(3)]
            den = s.sb(es, 'at_den', (64, G, 128))
            ot = [s.sb(es, 'at_o%d' % i, (64, G, 128), BF16) for i in range(2)]
            psw = [s.ps(es, 'at_ps%d' % i, (64, 512)) for i in range(2)]
            pss = [s.ps(es, 'at_s%d' % i, (128, G, 128)) for i in range(2)]
            pso = s.ps(es, 'at_po', (64, G, 128))
            psd = s.ps(es, 'at_pd', (64, G, 128))
            psv = s.ps(es, 'at_pv', (128, 4, 64))
            cnt = {'x': 0, 'e': 0, 'o': 0}

            def load_feat(dst, row0, b, with_q_ctx, res):
                segs = []
                tc0 = b * c.CTX
                for t in range(0, c.CTX, 512):
                    n = min(512, c.CTX - t)
                    segs.append((tc0 + t, t, n, None))
                tl0 = c.NS * c.CTX + b * c.SEQ
                for t in range(0, c.SEQ, 512):
                    n = min(512, c.SEQ - t)
                    segs.append((tl0 + t, c.CTX + t, n, t))
                for (tg, td, n, tp) in segs:
                    if tp is None and not with_q_ctx:
                        continue
                    x = xin[cnt['x'] % 3]
                    cnt['x'] += 1
                    s.ld(x[:, 0:n], s.pT[row0:row0 + 64, tg:tg + n], ['pT_all'], [x])
                    if tp is None:
                        s.cp('act', dst[:, td:td + n], x[:, 0:n], [x], [res])
                    else:
                        p_ = psw[cnt['x'] % 2]
                        ta_, tb_ = t1[0], t1[1]
                        s.mm(p_[:, 0:n], [(s.swp[:, :], x[:, 0:n])], [s.swp, x], [p_])
                        s.tt('pool', ta_[:, 0:n], x[:, 0:n], rope[:, 0, tp:tp + n], ALU.mult, [x, rope], [ta_])
                        s.tt('dve', tb_[:, 0:n], p_[:, 0:n], rope[:, 1, tp:tp + n], ALU.mult, [p_, rope], [tb_])
                        s.tt('dve', dst[:, td:td + n], ta_[:, 0:n], tb_[:, 0:n], ALU.add, [ta_, tb_], [res])

            for b in range(c.NS):
                for kh in range(KVH):
                    load_feat(kT[:, kh, :], c.RC + kh * 64, b, True, kT)
                    tc0 = b * c.CTX
                    tl0 = c.NS * c.CTX + b * c.SEQ
                    for blk0 in range(0, NBK, 4):
                        nb_ = min(4, NBK - blk0)
                        x = xin[cnt['x'] % 3]
                        cnt['x'] += 1
                        for j in range(nb_):
                            kb = blk0 + j
                            tg = tc0 + kb * 128 if kb < NCB else tl0 + (kb - NCB) * 128
                            s.ld(x[:, j * 128:(j + 1) * 128], s.pT[c.RC + c.KVW + kh * 64:c.RC + c.KVW + kh * 64 + 64,
                                                                  tg:tg + 128], ['pT_all'], [x])
                        s.tr([(psv[:, j, :], x[:, j * 128:(j + 1) * 128]) for j in range(nb_)], s.ident[0:64, 0:64],
                             [x, s.ident], [psv])
                        s.cp('act', vtk[:, blk0:blk0 + nb_, kh, :], psv[:, 0:nb_, :], [psv], [vtk])
                for kh in range(KVH):
                    for g in range(G):
                        load_feat(qT[:, g, :], c.CTXC + (kh * G + g) * 64, b, not s.last, qT)
                    qblocks = []
                    if not s.last:
                        for n in range(NCB):
                            qblocks.append(('c', n))
                    for n in range(c.SEQ // 128):
                        qblocks.append(('l', n))
                    for (qk, n) in qblocks:
                        if qk == 'c':
                            q0 = n * 128
                            kbs = [(kb, None) for kb in range(NCB)]
                            tok0 = b * c.CTX + n * 128
                        else:
                            q0 = c.CTX + n * 128
                            kbs = [(kb, None) for kb in range(NCB)]
                            nl = c.SEQ // 128
                            if n > 0:
                                kbs.append((NCB + n - 1, 0))
                            kbs.append((NCB + n, None))
                            if n < nl - 1:
                                kbs.append((NCB + n + 1, 1))
                            tok0 = c.NS * c.CTX + b * c.SEQ + n * 128
                        for i, (kb, mk) in enumerate(kbs):
                            ps_ = pss[cnt['e'] % 2]
                            e = eb[cnt['e'] % 3]
                            cnt['e'] += 1
                            s.mm(ps_[:, :, :], [(kT[:, kh, kb * 128:(kb + 1) * 128], qT[:, :, q0:q0 + 128])],
                                 [kT, qT], [ps_])
                            s.act(e[:, :, :], ps_[:, :, :], AF.Exp, [ps_], [e], scale=0.125)
                            if mk is not None:
                                m = s.masks[:, mk, :]
                                mb = AP(m, m.offset, [m.ap[0], [0, G], m.ap[1]])
                                s.tt('dve', e[:, :, :], e[:, :, :], mb, ALU.mult, [e, s.masks], [e])
                            s.mm(pso[:, :, :], [(vtk[:, kb, kh, :], e[:, :, :])], [vtk, e], [pso],
                                 start=(i == 0), stop=(i == len(kbs) - 1))
                            s.mm(psd[:, :, :], [(s.onesb[:, :], e[:, :, :])], [s.onesb, e], [psd],
                                 start=(i == 0), stop=(i == len(kbs) - 1))
                        a = esk[:, kh * G:(kh + 1) * G]
                        eskb = AP(a, a.offset, [a.ap[0], a.ap[1], [0, 128]])
                        s.tt('dve', den[:, :, :], psd[:, :, :], eskb, ALU.add, [psd, esk], [den])
                        s.P.op('dve', lambda g_: g_.reciprocal(out=den[:, :, :], in_=den[:, :, :]), [den], [den])
                        o = ot[cnt['o'] % 2]
                        cnt['o'] += 1
                        s.tt('dve', o[:, :, :], pso[:, :, :], den[:, :, :], ALU.mult, [pso, den], [o])
                        s.ld(s.brT[2][kh * G * 64:(kh + 1) * G * 64, tok0:tok0 + 128].rearrange("(g p) t -> p g t", p=64),
                             o[:, :, :], [o], [('br2', kh, tok0)])
            P.flush()

    def phase_conv(s):
        c, P, l = s.c, s.P, s.l
        BC, BW = c.BC, c.BW
        SEG = 512
        with ExitStack() as es:
            cw = s.sb(es, 'cw', (128, 3, BC))
            for j in range(3):
                s.ldnc(cw[:, j, :], s.I['conv_w'][3 * l + j:3 * l + j + 1, :].rearrange("o (k p) -> p (o k)", p=128),
                       [cw], [cw])
            bt = [s.sb(es, 'cv_b%d' % i, (128, SEG)) for i in range(2)]
            ct = [s.sb(es, 'cv_c%d' % i, (128, SEG + 2)) for i in range(2)]
            ut = [s.sb(es, 'cv_u%d' % i, (128, SEG + 2)) for i in range(2)]
            o = [s.sb(es, 'cv_o%d' % i, (128, SEG)) for i in range(2)]
            ob = [s.sb(es, 'cv_ob%d' % i, (128, SEG), BF16) for i in range(2)]
            it = 0
            for (kind, b, t0s, ln, mc) in c.seqs:
                if s.last and kind == 'c':
                    continue
                for t0 in range(t0s, t0s + ln, SEG):
                    seg = min(SEG, t0s + ln - t0)
                    for ch in range(BC):
                        k = it % 2
                        it += 1
                        B_, C_, U_, O_, OB = bt[k], ct[k], ut[k], o[k], ob[k]
                        s.ld(B_[:, 0:seg], s.pT[c.QEND + ch * 128:c.QEND + (ch + 1) * 128, t0:t0 + seg], ['pT_all'], [B_])
                        s.load_halo(C_, 128, c.QEND + BW + ch * 128, t0, seg, t0s, t0s + ln)
                        s.load_halo(U_, 128, c.QEND + 2 * BW + ch * 128, t0, seg, t0s, t0s + ln)
                        s.tt('pool', C_[:, 0:seg + 2], C_[:, 0:seg + 2], U_[:, 0:seg + 2], ALU.mult, [C_, U_], [C_])
                        s.ts('dve', O_[:, 0:seg], C_[:, 1:seg + 1], cw[:, 1, ch:ch + 1], None, ALU.mult, None, [C_, cw], [O_])
                        s.stt('dve', O_[:, 0:seg], C_[:, 0:seg], cw[:, 0, ch:ch + 1], O_[:, 0:seg], ALU.mult, ALU.add,
                              [C_, cw, O_], [O_])
                        s.stt('dve', O_[:, 0:seg], C_[:, 2:seg + 2], cw[:, 2, ch:ch + 1], O_[:, 0:seg], ALU.mult, ALU.add,
                              [C_, cw, O_], [O_])
                        s.tt('pool', OB[:, 0:seg], O_[:, 0:seg], B_[:, 0:seg], ALU.mult, [O_, B_], [OB])
                        s.ld(s.brT[1][ch * 128:(ch + 1) * 128, t0:t0 + seg], OB[:, 0:seg], [OB], [('br1', ch, t0)])
            P.flush()

    def phase_merge(s):
        c, P, l = s.c, s.P, s.l
        KC, BC, D, BW = c.KC, c.BC, c.D, c.BW
        G = 512
        NS1 = c.NS + 1
        with ExitStack() as es:
            br = [[s.sb(es, 'mg_br%d_%d' % (i, k), (128, BC, G), BF16) for k in range(1)] for i in range(3)]
            wb = [s.sb(es, 'mg_w%d' % i, (128, BC, 512), BF16) for i in range(2)]
            wo = [s.sb(es, 'mg_wo%d' % i, (128, KC, 512), BF16) for i in range(2)]
            gt = [s.sb(es, 'mg_g%d' % i, (128, 4, G)) for i in range(2)]
            mT = s.sb(es, 'mg_m', (128, KC, G), BF16)
            acc = s.sb(es, 'mg_acc', (128, 4, G))
            tmp = s.sb(es, 'mg_tmp', (128, G))
            xt = [s.sb(es, 'mg_x%d' % i, (128, D)) for i in range(4)]
            g2t = s.sb(es, 'mg_gate', (128, D))
            pp = [s.ps(es, 'mg_p%d' % i, (128, 512)) for i in range(4)]
            M6 = 6 * D
            wbs = s.W['w_branch']
            wos = s.W['w_out'][l * D:(l + 1) * D, :].rearrange("(k p) n -> p k n", p=128)
            wi = 0
            pi = 0
            gi_ = 0
            xi = 0
            groups = []
            for (kind, b, t0s, ln, mc) in c.seqs:
                if s.last and kind == 'c':
                    continue
                for t0 in range(t0s, t0s + ln, G):
                    groups.append((t0, min(G, t0s + ln - t0), mc))
            for gidx, (t0, n, mc) in enumerate(groups):
                k = 0
                s.ld(g2t[:, :], AP(s.modrow, (l * NS1 + mc) * M6 + 2 * D, [[0, 128], [1, D]]), ['modrow_all'], [g2t])
                for i in range(3):
                    s.ld(br[i][k][:, :, 0:n], s.brT[i][:, t0:t0 + n].rearrange("(k p) t -> p k t", p=128), ['br_all'],
                         [br[i][k]])
                for oq in range(D // 512):
                    for i in range(3):
                        w = wb[wi % 2]
                        wi += 1
                        s.ld(w[:, :, :], wbs[(l * 3 + i) * BW:(l * 3 + i + 1) * BW, oq * 512:(oq + 1) * 512].rearrange(
                            "(k p) n -> p k n", p=128), ['wfull_b'], [w])
                        gts = gt[gi_ % 2]
                        gi_ += 1
                        r0 = c.CONVEND + i * D + oq * 512
                        s.ld(gts[:, :, 0:n], s.pT[r0:r0 + 512, t0:t0 + n].rearrange("(j p) t -> p j t", p=128), ['pT_all'],
                             [gts])
                        for j in range(4):
                            p_ = pp[pi % 4]
                            pi += 1
                            s.mm(p_[:, 0:n], [(w[:, kk_, j * 128:(j + 1) * 128], br[i][k][:, kk_, 0:n]) for kk_ in range(BC)],
                                 [w, br[i][k]], [p_])
                            if i == 0:
                                s.tt('dve', acc[:, j, 0:n], p_[:, 0:n], gts[:, j, 0:n], ALU.mult, [p_, gts], [(acc, j)])
                            else:
                                s.tt('dve', tmp[:, 0:n], p_[:, 0:n], gts[:, j, 0:n], ALU.mult, [p_, gts], [tmp])
                                if i == 1:
                                    s.tt('pool', acc[:, j, 0:n], acc[:, j, 0:n], tmp[:, 0:n], ALU.add, [(acc, j), tmp],
                                         [(acc, j)])
                                else:
                                    s.tt('pool', mT[:, oq * 4 + j, 0:n], acc[:, j, 0:n], tmp[:, 0:n], ALU.add,
                                         [(acc, j), tmp], [(mT, oq * 4 + j)])
                allm = [(mT, j) for j in range(KC)]
                wts = []
                for oq in range(D // 512):
                    w = wo[oq % 2]
                    s.ld(w[:, :, :], wos[:, :, oq * 512:(oq + 1) * 512], ['wfull_o'], [w])
                    for tb in range(n // 128):
                        x = xt[tb]
                        if oq == 0:
                            s.ld(x[:, :], s.xres[t0 + tb * 128:t0 + (tb + 1) * 128, :], ['xres'], [x])
                        p_ = pp[pi % 4]
                        pi += 1
                        s.mm(p_[:, :], [(mT[:, kk_, tb * 128:(tb + 1) * 128], w[:, kk_, :]) for kk_ in range(KC)],
                             allm + [w], [p_])
                        s.tt('dve', tmp[:, 0:512], p_[:, :], g2t[:, oq * 512:(oq + 1) * 512], ALU.mult, [p_, g2t], [tmp])
                        s.tt('pool', x[:, oq * 512:(oq + 1) * 512], x[:, oq * 512:(oq + 1) * 512], tmp[:, 0:512], ALU.add,
                             [x, tmp], [x])
                        if oq == D // 512 - 1:
                            s.ld(s.xres[t0 + tb * 128:t0 + (tb + 1) * 128, :], x[:, :], [x], ['xres'])
                xi += n // 128
            P.flush()

    def phase_router(s, kind):
        c, P, l = s.c, s.P, s.l
        KC, D, NE = c.KC, c.D, c.NE
        n_tok = c.SEQ if kind == 'l' else c.CTX
        cap = c.CAPL if kind == 'l' else c.CAPC
        s.cap = cap
        with ExitStack() as es:
            wr = s.sb(es, 'rt_w', (128, KC, NE))
            s.ld(wr[:, :, :], s.I['w_router'][l * D:(l + 1) * D, :].rearrange("(k p) e -> p k e", p=128), (), [wr])
            xt = [s.sb(es, 'rt_x%d' % i, (128, D)) for i in range(2)]
            junk = s.sb(es, 'rt_j', (128, D))
            ss = [s.sb(es, 'rt_s%d' % i, (128, 2)) for i in range(2)]
            hT = [s.sb(es, 'rt_h%d' % i, (128, KC, 128)) for i in range(2)]
            tmp = [s.sb(es, 'rt_t%d' % i, (128, 4, 128)) for i in range(2)]
            lg = [s.sb(es, 'rt_lg%d' % i, (128, NE)) for i in range(2)]
            sm = [s.sb(es, 'rt_sm%d' % i, (128, 2)) for i in range(2)]
            affT = [s.sb(es, 'rt_aff%d' % b, (NE, n_tok)) for b in range(c.NS)]
            work = [s.sb(es, 'rt_wk%d' % b, (NE, n_tok)) for b in range(c.NS)]
            pt = [s.ps(es, 'rt_p%d' % i, (128, 4, 128)) for i in range(2)]
            pl = [s.ps(es, 'rt_pl%d' % i, (128, NE)) for i in range(2)]
            pa = [s.ps(es, 'rt_pa%d' % i, (NE, 128)) for i in range(2)]
            it = 0
            pi = 0
            for (kd, b, t0s, ln, mc) in c.seqs:
                if kd != kind:
                    continue
                for t0 in range(t0s, t0s + ln, 128):
                    k = it % 2
                    it += 1
                    x, sq, h = xt[k], ss[k], hT[k]
                    s.ld(x[:, :], s.xres[t0:t0 + 128, :], ['xres'], [x])
                    s.act(junk[:, :], x[:, :], AF.Square, [x], [junk, sq], accum=sq[:, 0:1])
                    s.rsqrt(sq[:, 1:2], sq[:, 0:1], 1.0 / D, 1e-6, [sq], [sq])
                    s.ts('dve', x[:, :], x[:, :], sq[:, 1:2], None, ALU.mult, None, [x, sq], [x])
                    s.ld(s.xn[t0:t0 + 128, :], x[:, :], [x], [('xn', t0)])
                    for q4 in range(KC // 4):
                        p_ = pt[pi % 2]
                        tm = tmp[pi % 2]
                        pi += 1
                        s.tr([(p_[:, j, :], x[:, (q4 * 4 + j) * 128:(q4 * 4 + j + 1) * 128]) for j in range(4)],
                             s.ident[:, :], [x, s.ident], [p_])
                        gb = s.gcol[:, 1, q4 * 4:q4 * 4 + 4, mc]
                        gb = AP(gb, gb.offset, [gb.ap[0], gb.ap[1], [0, 128]])
                        sb_ = s.modcol[:, 3 * KC + q4 * 4:3 * KC + q4 * 4 + 4, mc]
                        sb_ = AP(sb_, sb_.offset, [sb_.ap[0], sb_.ap[1], [0, 128]])
                        s.tt('dve', tm[:, :, :], p_[:, :, :], gb, ALU.mult, [p_, s.gcol], [tm])
                        s.tt('pool', h[:, q4 * 4:q4 * 4 + 4, :], tm[:, :, :], sb_, ALU.add, [tm, s.modcol], [h])
                    pl_ = pl[k]
                    s.mm(pl_[:, :], [(h[:, kk_, :], wr[:, kk_, :]) for kk_ in range(KC)], [h, wr], [pl_])
                    L_, sm_ = lg[k], sm[k]
                    s.P.op('dve', lambda g_, sm_=sm_, pl_=pl_: g_.tensor_reduce(out=sm_[:, 0:1], in_=pl_[:, :], axis=AX.X,
                                                                              op=ALU.max, negate=True), [pl_], [sm_])
                    s.act(L_[:, :], pl_[:, :], AF.Exp, [pl_, sm_], [L_, sm_], bias=sm_[:, 0:1], accum=sm_[:, 1:2])
                    s.P.op('dve', lambda g_, sm_=sm_: g_.reciprocal(out=sm_[:, 1:2], in_=sm_[:, 1:2]), [sm_], [sm_])
                    s.ts('dve', L_[:, :], L_[:, :], sm_[:, 1:2], None, ALU.mult, None, [L_, sm_], [L_])
                    pa_ = pa[k]
                    s.tr([(pa_[:, :], L_[:, :])], s.ident[:, :], [L_, s.ident], [pa_])
                    s.cp('act', affT[b][:, t0 - t0s:t0 - t0s + 128], pa_[:, :], [pa_], [(affT[b], t0)])
            P.flush()
            s.topk = es
            gk = [s.sb(es, 'rt_gk%d' % b, (NE, cap)) for b in range(c.NS)]
            ik = [s.sb(es, 'rt_ik%d' % b, (NE, cap), U32) for b in range(c.NS)]
            ikf = [s.sb(es, 'rt_ikf%d' % b, (NE, cap)) for b in range(c.NS)]
            for b in range(c.NS):
                cur = affT[b]
                for r in range(cap // 8):
                    g8 = gk[b][:, r * 8:(r + 1) * 8]
                    s.P.op('dve', (lambda g8, cur: lambda g_: g_.max(out=g8, in_=cur[:, :]))(g8, cur), [cur], [gk[b]])
                    s.P.op('dve', (lambda g8, cur, b, r: lambda g_: g_.max_index(out=ik[b][:, r * 8:(r + 1) * 8], in_max=g8,
                                                                           in_values=cur[:, :]))(g8, cur, b, r),
                           [cur, gk[b]], [ik[b]])
                    if r < cap // 8 - 1:
                        s.P.op('dve', (lambda g8, cur, b: lambda g_: g_.match_replace(
                            out=work[b][:, :], in_to_replace=g8, in_values=cur[:, :], imm_value=-1.0))(g8, cur, b),
                            [cur, gk[b]], [work[b]])
                        cur = work[b]
                s.cp('dve', ikf[b][:, :], ik[b][:, :], [ik[b]], [ikf[b]])
                t0s_b = [q for q in c.seqs if q[0] == kind and q[1] == b][0][2]
                s.ts('dve', ikf[b][:, :], ikf[b][:, :], float(t0s_b), None, ALU.add, None, [ikf[b]], [ikf[b]])
            nck = (cap + 127) // 128
            s.gsel = s.gselk[kind]
            s.isel = s.iselk[kind]
            ptk = [s.ps(es, 'rt_ptk%d' % i, (128, NE)) for i in range(2)]
            tg = [s.sb(es, 'rt_tg%d' % i, (128, NE)) for i in range(2)]
            ti = [s.sb(es, 'rt_ti%d' % i, (128, NE), I32) for i in range(2)]
            j = 0
            for b in range(c.NS):
                for ck in range(nck):
                    n = min(128, cap - ck * 128)
                    p_ = ptk[j % 2]
                    s.tr([(p_[0:n, :], gk[b][:, ck * 128:ck * 128 + n])], s.ident[0:NE, 0:NE], [gk[b], s.ident], [p_])
                    s.cp('dve', tg[j % 2][0:n, :], p_[0:n, :], [p_], [tg[j % 2]])
                    s.ld(s.gsel[b, ck * 128:ck * 128 + n, :], tg[j % 2][0:n, :], [tg[j % 2]], [('gsel', b, ck)])
                    j += 1
                    p_ = ptk[j % 2]
                    s.tr([(p_[0:n, :], ikf[b][:, ck * 128:ck * 128 + n])], s.ident[0:NE, 0:NE], [ikf[b], s.ident], [p_])
                    s.cp('dve', ti[j % 2][0:n, :], p_[0:n, :], [p_], [ti[j % 2]])
                    s.ld(s.isel[b, ck * 128:ck * 128 + n, :], ti[j % 2][0:n, :], [ti[j % 2]], [('isel', b, ck)])
                    j += 1
            P.flush()

    def phase_moe(s, kind):
        c, P, l = s.c, s.P, s.l
        KC, D, NE, FF = c.KC, c.D, c.NE, c.FF
        FC = FF // 128
        cap = s.cap
        NS1 = c.NS + 1
        nck = (cap + 127) // 128
        cw = min(cap, 128)
        NTOK = c.NS * cap
        M6 = 6 * D
        seqs = [q for q in c.seqs if q[0] == kind]
        with ExitStack() as es:
            gsel = s.sb(es, 'mo_g', (128, c.NS, nck, NE))
            isel = s.sb(es, 'mo_i', (128, c.NS, nck, NE), I32)
            for b in range(c.NS):
                for ck in range(nck):
                    n = min(128, cap - ck * 128)
                    s.ld(gsel[0:n, b, ck, :], s.gsel[b, ck * 128:ck * 128 + n, :], ['gsel_all'], [gsel])
                    s.ld(isel[0:n, b, ck, :], s.isel[b, ck * 128:ck * 128 + n, :], ['isel_all'], [isel])
            g5 = []
            for (kd, b, t0s, ln, mc) in seqs:
                t = s.sb(es, 'mo_g5%d' % b, (128, D))
                s.ld(t[:, :], AP(s.modrow, (l * NS1 + mc) * M6 + 5 * D, [[0, 128], [1, D]]), ['modrow_all'], [t])
                g5.append(t)
            xs = [s.sb(es, 'mo_xs%d' % i, (128, D)) for i in range(2)]
            xsT = [s.sb(es, 'mo_xT%d' % i, (128, KC, NTOK), BF16) for i in range(2)]
            hid = s.sb(es, 'mo_hid', (128, FC, NTOK), BF16)
            sg = [s.sb(es, 'mo_sg%d' % i, (128, NTOK)) for i in range(2)]
            ys = [s.sb(es, 'mo_ys%d' % i, (128, D)) for i in range(c.NS * nck)]
            tmp = [s.sb(es, 'mo_t%d' % i, (128, 4, 128)) for i in range(2)]
            wb = [s.sb(es, 'mo_w%d' % i, (128, max(KC, FC), 512), BF16) for i in range(3)]
            pt = [s.ps(es, 'mo_pt%d' % i, (128, 4, 128)) for i in range(2)]
            pg = [s.ps(es, 'mo_pg%d' % i, (128, NTOK)) for i in range(2)]
            pu = [s.ps(es, 'mo_pu%d' % i, (128, NTOK)) for i in range(2)]
            pd = [s.ps(es, 'mo_pd%d' % i, (128, 512)) for i in range(2)]
            cnt = {'x': 0, 'p': 0, 'w': 0, 'h': 0, 'y': 0, 'd': 0}
            for e_ in range(NE):
                xT = xsT[e_ % 2]
                for bi, (kd, b, t0s, ln, mc) in enumerate(seqs):
                    for ck in range(nck):
                        n = min(128, cap - ck * 128)
                        x = xs[cnt['x'] % 2]
                        cnt['x'] += 1
                        s.P.dma('pool', (lambda x, n, b, ck, e_, t0s, ln: lambda g_: g_.indirect_dma_start(
                            out=x[0:n, :], out_offset=None, in_=s.xn[:, :],
                            in_offset=bass.IndirectOffsetOnAxis(ap=isel[0:n, b, ck, e_:e_ + 1], axis=0)))(
                            x, n, b, ck, e_, t0s, ln), ['xn_all', isel], [x])
                        c0 = bi * cap + ck * 128
                        for q4 in range(KC // 4):
                            p_ = pt[cnt['p'] % 2]
                            tm = tmp[cnt['p'] % 2]
                            cnt['p'] += 1
                            s.tr([(p_[:, j, 0:n], x[0:n, (q4 * 4 + j) * 128:(q4 * 4 + j + 1) * 128]) for j in range(4)],
                                 s.ident[0:n, 0:n], [x, s.ident], [p_])
                            gb = s.gcol[:, 1, q4 * 4:q4 * 4 + 4, mc]
                            gb = AP(gb, gb.offset, [gb.ap[0], gb.ap[1], [0, n]])
                            sb_ = s.modcol[:, 3 * KC + q4 * 4:3 * KC + q4 * 4 + 4, mc]
                            sb_ = AP(sb_, sb_.offset, [sb_.ap[0], sb_.ap[1], [0, n]])
                            s.tt('dve', tm[:, :, 0:n], p_[:, :, 0:n], gb, ALU.mult, [p_, s.gcol], [tm])
                            s.tt('pool', xT[:, q4 * 4:q4 * 4 + 4, c0:c0 + n], tm[:, :, 0:n], sb_, ALU.add,
                                 [tm, s.modcol], [xT])
                wg = s.W['w_exp_gate'][(l * NE + e_) * D:(l * NE + e_ + 1) * D, :].rearrange("(k p) f -> p k f", p=128)
                wu = s.W['w_exp_up'][(l * NE + e_) * D:(l * NE + e_ + 1) * D, :].rearrange("(k p) f -> p k f", p=128)
                wd = s.W['w_exp_down'][(l * NE + e_) * FF:(l * NE + e_ + 1) * FF, :].rearrange("(k p) d -> p k d", p=128)
                for fq in range(FF // 512):
                    w1 = wb[cnt['w'] % 3]
                    cnt['w'] += 1
                    w2 = wb[cnt['w'] % 3]
                    cnt['w'] += 1
                    s.ld(w1[:, 0:KC, :], wg[:, :, fq * 512:(fq + 1) * 512], ['wfull_e'], [w1])
                    s.ld(w2[:, 0:KC, :], wu[:, :, fq * 512:(fq + 1) * 512], ['wfull_e'], [w2])
                    for j in range(4):
                        pg_ = pg[cnt['h'] % 2]
                        pu_ = pu[cnt['h'] % 2]
                        sg_ = sg[cnt['h'] % 2]
                        cnt['h'] += 1
                        s.mm(pg_[:, :], [(w1[:, kk_, j * 128:(j + 1) * 128], xT[:, kk_, :]) for kk_ in range(KC)], [w1, xT], [pg_])
                        s.mm(pu_[:, :], [(w2[:, kk_, j * 128:(j + 1) * 128], xT[:, kk_, :]) for kk_ in range(KC)], [w2, xT], [pu_])
                        s.act(sg_[:, :], pg_[:, :], AF.Silu, [pg_], [sg_])
                        s.tt('dve', hid[:, fq * 4 + j, :], sg_[:, :], pu_[:, :], ALU.mult, [sg_, pu_], [hid])
                tbs = []
                for bi, (kd, b, t0s, ln, mc) in enumerate(seqs):
                    for ck in range(nck):
                        tbs.append((bi, b, ck, min(128, cap - ck * 128), bi * cap + ck * 128))
                for dq in range(D // 512):
                    w = wb[cnt['w'] % 3]
                    cnt['w'] += 1
                    s.ld(w[:, 0:FC, :], wd[:, :, dq * 512:(dq + 1) * 512], ['wfull_e'], [w])
                    for ti_, (bi, b, ck, n, c0) in enumerate(tbs):
                        y = ys[ti_]
                        p_ = pd[cnt['d'] % 2]
                        cnt['d'] += 1
                        s.mm(p_[0:n, :], [(hid[:, kk_, c0:c0 + n], w[:, kk_, :]) for kk_ in range(FC)], [hid, w], [p_])
                        s.stt('dve', y[0:n, dq * 512:(dq + 1) * 512], p_[0:n, :], gsel[0:n, b, ck, e_:e_ + 1],
                              g5[bi][0:n, dq * 512:(dq + 1) * 512], ALU.mult, ALU.mult, [p_, gsel, g5[bi]], [y])
                for ti_, (bi, b, ck, n, c0) in enumerate(tbs):
                    y = ys[ti_]
                    s.P.dma('pool', (lambda y, n, b, ck, e_: lambda g_: g_.indirect_dma_start(
                        out=s.xres[:, :],
                        out_offset=bass.IndirectOffsetOnAxis(ap=isel[0:n, b, ck, e_:e_ + 1], axis=0),
                        in_=y[0:n, :], in_offset=None, compute_op=ALU.add))(y, n, b, ck, e_),
                        [y, isel], ['xres'])
            P.flush()

    def phase_final(s):
        c, P = s.c, s.P
        D = c.D
        with ExitStack() as es:
            nf = s.sb(es, 'fn_w', (128, D))
            s.ld(nf[:, :], AP(s.I['norm_final'], 0, [[0, 128], [1, D]]), (), [nf])
            xt = [s.sb(es, 'fn_x%d' % i, (128, D)) for i in range(3)]
            junk = s.sb(es, 'fn_j', (128, D))
            ss = [s.sb(es, 'fn_s%d' % i, (128, 2)) for i in range(3)]
            it = 0
            for (kind, b, t0s, ln, mc) in c.seqs:
                if kind != 'l':
                    continue
                for t0 in range(t0s, t0s + ln, 128):
                    x, sq = xt[it % 3], ss[it % 3]
                    it += 1
                    s.ld(x[:, :], s.xres[t0:t0 + 128, :], ['xres'], [x])
                    s.act(junk[:, :], x[:, :], AF.Square, [x], [junk, sq], accum=sq[:, 0:1])
                    s.rsqrt(sq[:, 1:2], sq[:, 0:1], 1.0 / D, 1e-6, [sq], [sq])
                    s.stt('dve', x[:, :], x[:, :], sq[:, 1:2], nf[:, :], ALU.mult, ALU.mult, [x, sq, nf], [x])
                    o0 = t0 - c.NS * c.CTX
                    s.ld(s.out[o0:o0 + 128, :], x[:, :], [x], [('out', o0)])
            P.flush()


def make_in_maps(c, inputs):
    bs = big_shapes(c)
    ss = small_shapes(c)
    consts = host_consts(c)
    maps = []
    big = {n: np.ascontiguousarray(np.asarray(inputs[n], np.float32)).reshape(bs[n]) for n in BIGW}
    small = {}
    for n, shp in ss.items():
        a = np.asarray(inputs[n], np.float32)
        if shp[0] == 0:
            a = np.zeros((1, shp[1]), np.float32)
        small[n] = np.ascontiguousarray(a.reshape(max(shp[0], 1), shp[1]))
    x = np.asarray(inputs['x'], np.float32)
    ctx = np.asarray(inputs['ctx'], np.float32)
    cc = np.asarray(inputs['c'], np.float32)
    for i in range(c.NCORES):
        m = {}
        m['x'] = np.ascontiguousarray(x[i * c.NS:(i + 1) * c.NS]).reshape(c.NS * c.SEQ, c.D)
        m['ctx'] = np.ascontiguousarray(ctx[i * c.NS:(i + 1) * c.NS]).reshape(c.NS * c.CTX, c.D)
        m['c'] = np.ascontiguousarray(cc[i * c.NS:(i + 1) * c.NS])
        for n in BIGW:
            r = bs[n][0] // c.NCORES
            m[n] = big[n][i * r:(i + 1) * r] if c.GATHER else big[n]
        m.update(small)
        m.update(consts)
        maps.append(m)
    return maps


def run(c, inputs, debug_out=None, stop=None):
    b = Builder(c, debug_out, stop)
    nc = b.build()
    maps = make_in_maps(c, inputs)
    res = run_bass_kernel_spmd(nc, maps, core_ids=list(range(c.NCORES)))
    return res


def kernel(**inputs):
    c = Cfg(GATHER=False)
    res = run(c, inputs)
    out = np.stack([r['y'] for r in res.results]).reshape(c.BATCH, c.SEQ, c.D)
    return out.astype(np.float32)
```

```python
import numpy as np
from contextlib import ExitStack
import concourse.bass as bass
import concourse.mybir as mybir
from concourse.bass_utils import run_bass_kernel_spmd

F32 = mybir.dt.float32
BF16 = mybir.dt.bfloat16
U32 = mybir.dt.uint32
I32 = mybir.dt.int32
ALU = mybir.AluOpType
AF = mybir.ActivationFunctionType
AX = mybir.AxisListType


class Cfg:
    def __init__(s, D=2048, SEQ=2048, CTX=256, GRID_W=64, DEPTH=2, NE=16, FF=2048, NCORES=8, BATCH=16, GATHER=True):
        s.GATHER = GATHER
        s.D, s.SEQ, s.CTX, s.GRID_W, s.DEPTH, s.NE, s.FF = D, SEQ, CTX, GRID_W, DEPTH, NE, FF
        s.NCORES, s.BATCH = NCORES, BATCH
        s.NS = BATCH // NCORES
        s.BW = D // 2
        s.H = s.BW // 64
        s.KVH = s.H // 4
        s.DR, s.IR, s.VR, s.GR = 96, 96, 64, 256
        s.RC = 3 * s.BW + 2 * s.DR + 2 * s.IR + s.GR
        s.KVW = s.KVH * 64
        s.CTXC = s.RC + 2 * s.KVW
        s.QEND = s.CTXC + s.BW
        s.CONVEND = s.QEND + 3 * s.BW
        s.INC = s.CONVEND + 3 * D
        s.NT = s.NS * (s.CTX + s.SEQ)
        s.TALL = s.CTX + s.SEQ
        s.KC = D // 128
        s.BC = s.BW // 128
        s.CAPL = 2 * s.SEQ // NE
        s.CAPC = 2 * s.CTX // NE
        s.IQ = 128 // (s.NS * s.H)
        s.IP = 64 // s.IQ
        s.seqs = [('c', b, b * s.CTX, s.CTX, s.NS) for b in range(s.NS)] + \
                 [('l', b, s.NS * s.CTX + b * s.SEQ, s.SEQ, b) for b in range(s.NS)]


BIGW = ['w_mod', 'w_in', 'w_branch', 'w_out', 'w_exp_gate', 'w_exp_up', 'w_exp_down']
SMALLW = ['b_mod', 'norm_mix', 'norm_ffn', 'shift_mu', 'decay_up', 'decay_bias', 'iclr_up', 'iclr_bias',
          'gate_up', 'vres_down', 'vres_up', 'vres_bias', 'k_k', 'k_a', 'r_k', 'gn_w', 'gn_b', 'conv_w',
          'attn_sink', 'w_router', 'norm_final', 'c_ctx']


def big_shapes(c):
    L = c.DEPTH
    return {'w_mod': (L * c.D, 6 * c.D), 'w_in': (L * c.D, c.INC), 'w_branch': (L * 3 * c.BW, c.D),
            'w_out': (L * c.D, c.D), 'w_exp_gate': (L * c.NE * c.D, c.FF), 'w_exp_up': (L * c.NE * c.D, c.FF),
            'w_exp_down': (L * c.NE * c.FF, c.D)}


def small_shapes(c):
    L = c.DEPTH
    return {'b_mod': (L, 6 * c.D), 'norm_mix': (L, c.D), 'norm_ffn': (L, c.D), 'shift_mu': (L, c.RC),
            'decay_up': (L * 2 * c.DR, c.BW), 'decay_bias': (L * 2, c.BW), 'iclr_up': (L * 2 * c.IR, c.BW),
            'iclr_bias': (L * 2, c.BW), 'gate_up': (L * c.GR, c.BW), 'vres_down': ((L - 1) * c.BW, c.VR),
            'vres_up': ((L - 1) * c.VR, c.BW), 'vres_bias': (L - 1, c.BW), 'k_k': (L, c.BW), 'k_a': (L, c.BW),
            'r_k': (L, c.BW), 'gn_w': (L, c.BW), 'gn_b': (L, c.BW), 'conv_w': (L * 3, c.BW),
            'attn_sink': (L, c.H), 'w_router': (L * c.D, c.NE), 'norm_final': (1, c.D), 'c_ctx': (1, c.D)}


def host_consts(c):
    ident = np.eye(128, dtype=np.float32)
    blk = np.zeros((128, 128), np.float32)
    blk[:64, :64] = 1
    blk[64:, 64:] = 1
    swp = np.zeros((64, 64), np.float32)
    for m in range(64):
        q = m % 32
        swp[m + 16 if q < 16 else m - 16, m] = 1
    k = np.arange(128)[:, None]
    q = np.arange(128)[None, :]
    masks = np.stack([(k >= q), (k <= q)]).astype(np.float32)
    t = np.arange(c.SEQ)
    rows = (t // c.GRID_W).astype(np.float32)
    cols = (t % c.GRID_W).astype(np.float32)
    inv = (10000.0 ** (-np.arange(16, dtype=np.float32) / 16)).astype(np.float32)
    cs = np.zeros((2, 64, c.SEQ), np.float32)
    for n in range(64):
        pos = rows if n < 32 else cols
        m = n % 32
        ang = (pos * inv[m % 16]).astype(np.float32)
        cs[0, n] = np.cos(ang)
        cs[1, n] = -np.sin(ang) if m < 16 else np.sin(ang)
    return {'k_ident': ident, 'k_blk': blk, 'k_swp': swp, 'k_masks': masks.reshape(256, 128),
            'k_rope': cs.reshape(128, c.SEQ)}


class Prog:
    ENG = ('sp', 'act', 'dve', 'pool', 'pe')
    NDS = 8

    def __init__(s, nc):
        s.nc = nc
        s.sems = {}
        s.esem = {e: s._sem('e_' + e) for e in s.ENG}
        s.ecnt = {e: 0 for e in s.ENG}
        s.dsem = {e: [s._sem('d_%s%d' % (e, i)) for i in range(s.NDS)] for e in ('sp', 'act', 'pool')}
        s.dcnt = {e: 0 for e in ('sp', 'act', 'pool')}
        s.waited = {e: {} for e in s.ENG}
        s.last = {}
        s.res = {}
        s.q = {e: [] for e in s.ENG}

    def _sem(s, name):
        s.sems[name] = s.nc.alloc_semaphore(name=name)
        return name

    @staticmethod
    def _key(r):
        if isinstance(r, tuple):
            return (Prog._key(r[0]),) + tuple(r[1:])
        if isinstance(r, str):
            return r
        return id(r)

    def _waits(s, e, reads, writes, extra=()):
        toks = list(extra)
        for r in reads:
            st = s.res.get(s._key(r))
            if st and st['w']:
                toks.append(st['w'])
        for w in writes:
            st = s.res.get(s._key(w))
            if st:
                if st['w']:
                    toks.append(st['w'])
                toks.extend(st['r'].items())
        out = {}
        for sk, v in toks:
            if e == 'pe' and sk == s.esem['pe']:
                continue
            if s.waited[e].get(sk, 0) < v:
                out[sk] = max(out.get(sk, 0), v)
        for sk, v in out.items():
            s.waited[e][sk] = v
        return list(out.items())

    def _commit(s, tok, reads, writes):
        s.last[tok[0]] = tok[1]
        wk = [s._key(w) for w in writes]
        for k in wk:
            s.res[k] = {'w': tok, 'r': {}}
        for r in reads:
            k = s._key(r)
            if k in wk:
                continue
            st = s.res.setdefault(k, {'w': None, 'r': {}})
            st['r'][tok[0]] = max(st['r'].get(tok[0], 0), tok[1])

    def op(s, e, fn, reads=(), writes=()):
        waits = s._waits(e, reads, writes)
        s.ecnt[e] += 1
        tok = (s.esem[e], s.ecnt[e])
        s.q[e].append((waits, fn, tok[0], 1))
        s._commit(tok, reads, writes)

    def dma(s, e, fn, reads=(), writes=(), inc=16):
        n = s.dcnt[e]
        s.dcnt[e] += 1
        slot = s.dsem[e][n % s.NDS]
        prev = s.last.get(slot, 0)
        waits = s._waits(e, reads, writes, extra=[(slot, prev)] if prev else [])
        tok = (slot, prev + inc)
        s.q[e].append((waits, fn, slot, inc))
        s._commit(tok, reads, writes)

    MAGIC = 1000

    def prologue(s):
        nc = s.nc
        s.gate = {e: nc.alloc_semaphore(name='gate_' + e) for e in s.ENG}
        s.done = nc.alloc_semaphore(name='done')
        gate, sems, done, MAGIC = s.gate, s.sems, s.done, s.MAGIC
        with nc.Block() as block:
            def mk(e):
                def f(eng):
                    if e == 'pool':
                        for h in sems.values():
                            eng.sem_clear(h)
                        eng.sem_clear(done)
                        for g in gate.values():
                            eng.sem_clear(g)
                        for g in gate.values():
                            eng.sem_inc(g, MAGIC)
                    eng.wait_op(gate[e], MAGIC, 'sem-eq')
                    eng.sem_inc(gate[e], 1)
                return f
            block.sync(mk('sp'))
            block.scalar(mk('act'))
            block.vector(mk('dve'))
            block.gpsimd(mk('pool'))
            block.tensor(mk('pe'))

    def epilogue(s):
        nc = s.nc
        gate, sems, done = s.gate, s.sems, s.done
        with nc.Block() as block:
            def mk(e):
                def f(eng):
                    if e == 'pool':
                        eng.wait_ge(done, 4)
                        for h in sems.values():
                            eng.sem_clear(h)
                        for g in gate.values():
                            eng.sem_clear(g)
                        eng.sem_clear(done)
                    else:
                        eng.sem_inc(done, 1)
                return f
            block.sync(mk('sp'))
            block.scalar(mk('act'))
            block.vector(mk('dve'))
            block.gpsimd(mk('pool'))
            block.tensor(mk('pe'))

    def flush(s):
        nc = s.nc
        for e in s.ENG:
            waits = []
            for sk, v in s.last.items():
                if s.waited[e].get(sk, 0) < v:
                    s.waited[e][sk] = v
                    waits.append((sk, v))
            s.q[e].append((waits, None, None, 0))
        q = s.q
        sems = s.sems
        with nc.Block() as block:
            def mk(e):
                def f(eng):
                    for waits, fn, sem, inc in q[e]:
                        for sk, v in waits:
                            eng.wait_ge(sems[sk], v)
                        if fn is not None:
                            ins = fn(eng)
                            ins.then_inc(sems[sem], inc)
                return f
            block.sync(mk('sp'))
            block.scalar(mk('act'))
            block.vector(mk('dve'))
            block.gpsimd(mk('pool'))
            block.tensor(mk('pe'))
        s.q = {e: [] for e in s.ENG}
        s.res = {}


def AP(t, off, dims):
    return bass.AP(t.tensor, off, [list(d) for d in dims])


class Builder:
    def __init__(s, c, debug_out=None, stop=None):
        s.c = c
        s.stop = stop
        s.uid = 0
        s.nc = bass.Bass("TRN2", target_bir_lowering=False)
        s.P = Prog(s.nc)
        s.debug_out = debug_out or []

    def dram(s, name, shape, dt=F32, kind="Internal"):
        if kind == "Internal":
            return s.nc.dram_tensor(name, list(shape), dt).ap()
        return s.nc.dram_tensor(name, list(shape), dt, kind=kind).ap()

    def sb(s, es, name, shape, dt=F32):
        s.uid += 1
        return es.enter_context(s.nc.sbuf_tensor('%s_%d' % (name, s.uid), list(shape), dt))

    def ps(s, es, name, shape, dt=F32):
        s.uid += 1
        return es.enter_context(s.nc.psum_tensor('%s_%d' % (name, s.uid), list(shape), dt))

    def ld(s, out, in_, reads, writes, q='sp'):
        s.P.dma(q, lambda e: e.dma_start(out=out, in_=in_), reads, writes)

    def ldnc(s, out, in_, reads, writes, q='sp'):
        s.P.dma(q, lambda e: e.dma_start(out=out, in_=in_, allow_slow_non_contiguous=True), reads, writes)

    def tt(s, e, out, a, b, op, reads, writes):
        s.P.op(e, lambda g: g.tensor_tensor(out=out, in0=a, in1=b, op=op), reads, writes)

    def ts(s, e, out, a, s1, s2, op0, op1, reads, writes):
        if s2 is None:
            s.P.op(e, lambda g: g.tensor_scalar(out=out, in0=a, scalar1=s1, scalar2=None, op0=op0), reads, writes)
        else:
            s.P.op(e, lambda g: g.tensor_scalar(out=out, in0=a, scalar1=s1, scalar2=s2, op0=op0, op1=op1),
                   reads, writes)

    def stt(s, e, out, a, sc, b, op0, op1, reads, writes):
        s.P.op(e, lambda g: g.scalar_tensor_tensor(out=out, in0=a, scalar=sc, in1=b, op0=op0, op1=op1),
               reads, writes)

    def act(s, out, in_, func, reads, writes, bias=None, scale=None, accum=None):
        kw = {}
        if bias is not None:
            kw['bias'] = bias
        if scale is not None:
            kw['scale'] = scale
        if accum is not None:
            kw['accum_out'] = accum
        s.P.op('act', lambda g: g.activation(out=out, in_=in_, func=func, **kw), reads, writes)

    def rsqrt(s, out, in_, mult, add, reads, writes):
        s.act(out, in_, AF.Sqrt, reads, writes, bias=s.cbias(add), scale=mult)
        s.P.op('dve', lambda g: g.reciprocal(out=out, in_=out), writes, writes)

    def cbias(s, val):
        key = float(val)
        if key not in s.cb:
            i = len(s.cb)
            s.cb[key] = i
            s.P.op('pool', (lambda i, key: lambda g: g.memset(s.cbt[:, i:i + 1], key))(i, key), (), [(s.cbt, i)])
        i = s.cb[key]
        return s.cbt[:, i:i + 1]

    def cp(s, e, out, in_, reads, writes):
        if e == 'act':
            s.act(out, in_, AF.Copy, reads, writes)
        else:
            s.P.op(e, lambda g: g.tensor_copy(out=out, in_=in_), reads, writes)

    def red(s, out, in_, reads, writes, negate=False):
        s.P.op('dve', lambda g: g.tensor_reduce(out=out, in_=in_, axis=AX.X, op=ALU.add, negate=negate),
               reads, writes)

    def mm(s, out, pairs, reads, writes, start=True, stop=True):
        def fn(g):
            ins = None
            n = len(pairs)
            for i, (l, r) in enumerate(pairs):
                ins = g.matmul(out, l, r, start=(start and i == 0), stop=(stop and i == n - 1))
            return ins
        s.P.op('pe', fn, reads, writes)

    def tr(s, outs_ins, ident, reads, writes):
        def fn(g):
            ins = None
            for o, i in outs_ins:
                ins = g.transpose(o, i, ident)
            return ins
        s.P.op('pe', fn, reads, writes)

    def memset(s, e, ap, val, writes):
        s.P.op(e, lambda g: g.memset(ap, val), (), writes)

    def build(s):
        c, nc, P = s.c, s.nc, s.P
        L = c.DEPTH
        s.I = {}
        s.I['x'] = s.dram('x', (c.NS * c.SEQ, c.D), kind="ExternalInput")
        s.I['ctx'] = s.dram('ctx', (c.NS * c.CTX, c.D), kind="ExternalInput")
        s.I['c'] = s.dram('c', (c.NS, c.D), kind="ExternalInput")
        bs = big_shapes(c)
        for n in BIGW:
            r, w = bs[n]
            s.I[n + '_sh'] = s.dram(n, (r // c.NCORES if c.GATHER else r, w), kind="ExternalInput")
        for n, shp in small_shapes(c).items():
            s.I[n] = s.dram(n, (max(shp[0], 1), shp[1]), kind="ExternalInput")
        for n, a in host_consts(c).items():
            s.I[n] = s.dram(n, a.shape, kind="ExternalInput")
        s.out = s.dram('y', (c.NS * c.SEQ, c.D), kind="ExternalOutput")
        s.W = {}
        s.Wsh = {}
        for n in BIGW:
            r, w = bs[n]
            s.Wsh[n] = s.dram(n + '_b16s', (r // c.NCORES, w), BF16)
            s.W[n] = s.dram(n + '_b16', (r, w), BF16)
        s.xres = s.dram('xres', (c.NT, c.D))
        s.hT = s.dram('hT', (c.D, c.NT), BF16)
        s.pT = s.dram('pT', (c.INC, c.NT))
        s.strm = {n: s.dram('st_' + n, (c.NS, c.H, c.TALL, 64)) for n in
                  ['w0', 'w1', 'kd0', 'kd1', 'ka0', 'ka1', 'nkk', 'r', 'v']}
        s.ysc = [s.dram('ysc%d' % d, (c.NS, c.H, c.TALL, 64)) for d in range(2)]
        s.sgdT = s.dram('sgdT', (c.GR, c.NT))
        s.vfT = s.dram('vfT', (c.BW, c.NT))
        s.brT = [s.dram('brT%d' % i, (c.BW, c.NT), BF16) for i in range(3)]
        s.xn = s.dram('xn', (c.NT, c.D))
        s.modrow = s.dram('modrow', (L * (c.NS + 1), 6 * c.D))
        s.gselk = {'l': s.dram('gsel_l', (c.NS, c.CAPL, c.NE)), 'c': s.dram('gsel_c', (c.NS, c.CAPC, c.NE))}
        s.iselk = {'l': s.dram('isel_l', (c.NS, c.CAPL, c.NE), I32), 'c': s.dram('isel_c', (c.NS, c.CAPC, c.NE), I32)}

        with ExitStack() as g:
            g.enter_context(nc.allow_low_precision("bf16 matmul operands, fp32 accumulation"))
            s.ident = s.sb(g, 'ident', (128, 128))
            s.blk = s.sb(g, 'blk', (128, 128))
            s.swp = s.sb(g, 'swp', (64, 64))
            s.masks = s.sb(g, 'masks', (128, 2, 128), BF16)
            s.masks_f = s.sb(g, 'masks_f', (128, 2, 128))
            s.identb = s.sb(g, 'identb', (128, 128), BF16)
            s.onesb = s.sb(g, 'onesb', (128, 64), BF16)
            s.modcol = s.sb(g, 'modcol', (128, 6 * c.KC, c.NS + 1))
            s.ncol = s.sb(g, 'ncol', (128, 2, c.KC))
            s.gcol = s.sb(g, 'gcol', (128, 2, c.KC, c.NS + 1))
            s.cbt = s.sb(g, 'cbt', (128, 8))
            s.cb = {}
            s.dbg = {}
            s.scr = {'pT': s.pT, 'hT': s.hT, 'xres': s.xres, 'modrow': s.modrow, 'sgdT': s.sgdT, 'vfT': s.vfT,
                     'xn': s.xn, 'ysc0': s.ysc[0], 'ysc1': s.ysc[1], 'brT0': s.brT[0], 'brT1': s.brT[1],
                     'brT2': s.brT[2]}
            for n_ in s.strm:
                s.scr['st_' + n_] = s.strm[n_]
            for k_ in 'lc':
                s.scr['gsel_' + k_] = s.gselk[k_]
                s.scr['isel_' + k_] = s.iselk[k_]
            for n_ in s.debug_out:
                a_ = s.scr[n_]
                s.dbg[n_] = s.dram('dbg_' + n_, a_.shape, a_.dtype, kind="ExternalOutput")
            s.P.prologue()
            s.phase_init()
            done = False
            for l in range(L):
                s.l = l
                s.last = (l == L - 1)
                phases = [('mod', s.phase_mod), ('norm1', s.phase_norm1), ('inproj', s.phase_inproj),
                          ('rwkv_pre', s.phase_rwkv_pre), ('scan', s.phase_scan), ('rwkv_post', s.phase_rwkv_post),
                          ('attn', s.phase_attn), ('conv', s.phase_conv), ('merge', s.phase_merge)]
                for kind in (['l'] if s.last else ['l', 'c']):
                    phases.append(('router_' + kind, (lambda k: lambda: s.phase_router(k))(kind)))
                    phases.append(('moe_' + kind, (lambda k: lambda: s.phase_moe(k))(kind)))
                for name, fn in phases:
                    fn()
                    if s.stop == (l, name):
                        done = True
                        break
                if done:
                    break
            if not done:
                s.phase_final()
            for n_ in s.dbg:
                s.ld(s.dbg[n_], s.scr[n_], ['dbgsrc'], [('dbg', n_)])
            s.P.flush()
            s.P.epilogue()
        return nc

    def phase_init(s):
        c, P = s.c, s.P
        I = s.I
        s.ld(s.ident[:, :], I['k_ident'][:, :], (), [s.ident])
        s.ld(s.blk[:, :], I['k_blk'][:, :], (), [s.blk])
        s.ld(s.swp[:, :], I['k_swp'][:, :], (), [s.swp])
        s.ld(s.masks_f[:, :, :], I['k_masks'].rearrange("(m k) q -> k m q", m=2), (), [s.masks_f])
        s.cp('dve', s.masks[:, :, :], s.masks_f[:, :, :], [s.masks_f], [s.masks])
        s.cp('dve', s.identb[:, :], s.ident[:, :], [s.ident], [s.identb])
        s.memset('dve', s.onesb[:, :], 1.0, [s.onesb])
        nct = c.NS * c.CTX
        s.ld(s.xres[0:nct, :], I['ctx'][:, :], (), ['xres'])
        R = c.NS * c.SEQ
        step = max(R // 4, 128)
        for r0 in range(0, R, step):
            s.ld(s.xres[nct + r0:nct + r0 + step, :], I['x'][r0:r0 + step, :], (), [('xres', r0)])
        bs = big_shapes(c)
        for n in BIGW:
            r = bs[n][0] // c.NCORES if c.GATHER else bs[n][0]
            step = max(r // (4 if c.GATHER else 32), 1)
            dstw = s.Wsh[n] if c.GATHER else s.W[n]
            for r0 in range(0, r, step):
                s.P.dma('pool', (lambda dstw, n, r0, step: lambda e: e.dma_start(
                    out=dstw[r0:r0 + step, :], in_=I[n + '_sh'][r0:r0 + step, :]))(dstw, n, r0, step),
                    (), [('wsh', n, r0)])
        P.flush()
        for n in (BIGW if c.GATHER else []):
            s.P.dma('pool', (lambda n: lambda e: e.collective_compute(
                "AllGather", ALU.bypass, replica_groups=[list(range(c.NCORES))],
                ins=[s.Wsh[n].opt()], outs=[s.W[n].opt()]))(n), (), [('wfull', n)], inc=1)
        P.flush()

    def phase_mod(s):
        c, P, l = s.c, s.P, s.l
        NS1 = c.NS + 1
        M6 = 6 * c.D
        with ExitStack() as es:
            cT = s.sb(es, 'cT', (128, c.KC, NS1))
            cTb = s.sb(es, 'cTb', (128, c.KC, NS1), BF16)
            for sc in range(c.NS):
                s.ldnc(cT[:, :, sc], s.I['c'][sc:sc + 1, :].rearrange("s (k p) -> p (s k)", p=128), [cT], [cT])
            s.ldnc(cT[:, :, c.NS], s.I['c_ctx'].rearrange("s (k p) -> p (s k)", p=128), [cT], [cT])
            s.act(cTb[:, :, :], cT[:, :, :], AF.Silu, [cT], [cTb])
            brow = s.sb(es, 'brow', (NS1, M6))
            s.ld(brow[:, :], AP(s.I['b_mod'], l * M6, [[0, NS1], [1, M6]]), (), [brow])
            mrow = s.sb(es, 'mrow', (NS1, M6))
            wb = [s.sb(es, 'wmod%d' % i, (128, c.KC, 512), BF16) for i in range(2)]
            pm = [s.ps(es, 'pmod%d' % i, (NS1, 512)) for i in range(2)]
            wsrc = s.W['w_mod'][l * c.D:(l + 1) * c.D, :].rearrange("(k p) n -> p k n", p=128)
            ng = M6 // 512
            for gi in range(ng):
                w = wb[gi % 2]
                p_ = pm[gi % 2]
                s.ld(w[:, :, :], wsrc[:, :, gi * 512:(gi + 1) * 512], (), [w])
                s.mm(p_[:, :], [(cTb[:, k, :], w[:, k, :]) for k in range(c.KC)], [cTb, w], [p_])
                s.tt('dve', mrow[:, gi * 512:(gi + 1) * 512], p_[:, :], brow[:, gi * 512:(gi + 1) * 512], ALU.add,
                     [p_, brow], [(mrow, gi)])
            allm = [(mrow, gi) for gi in range(ng)]
            s.ld(s.modrow[l * NS1:(l + 1) * NS1, :], mrow[:, :], allm, [('modrow', l)])
            pc = s.ps(es, 'pcol', (128, 6 * c.KC, NS1))
            nch = 6 * c.KC
            s.tr([(pc[:, j, :], mrow[:, j * 128:(j + 1) * 128]) for j in range(nch)], s.ident[0:NS1, 0:NS1],
                 allm + [s.ident], [pc])
            s.cp('dve', s.modcol[:, :, :], pc[:, :, :], [pc], [s.modcol])
            s.ldnc(s.ncol[:, 0, :], s.I['norm_mix'][l:l + 1, :].rearrange("o (k p) -> p (o k)", p=128), (), [s.ncol])
            s.ldnc(s.ncol[:, 1, :], s.I['norm_ffn'][l:l + 1, :].rearrange("o (k p) -> p (o k)", p=128), [s.ncol],
                   [s.ncol])
            KC = c.KC
            for j, mi in ((0, 1), (1, 4)):
                for sc in range(NS1):
                    s.stt('dve', s.gcol[:, j, :, sc], s.modcol[:, mi * KC:(mi + 1) * KC, sc], 1.0, s.ncol[:, j, :],
                          ALU.add, ALU.mult, [s.modcol, s.ncol], [s.gcol])
            P.flush()

    def norm_tile(s, es_t, x_ap, tag):
        pass

    def phase_norm1(s):
        c, P, l = s.c, s.P, s.l
        KC = c.KC
        with ExitStack() as es:
            xt = [s.sb(es, 'n1x%d' % i, (128, c.D)) for i in range(2)]
            junk = s.sb(es, 'n1j', (128, c.D))
            ss = [s.sb(es, 'n1s%d' % i, (128, 2)) for i in range(2)]
            hst = [s.sb(es, 'n1h%d' % i, (128, KC, 128), BF16) for i in range(2)]
            tmp = [s.sb(es, 'n1t%d' % i, (128, 4, 128)) for i in range(2)]
            pt = [s.ps(es, 'n1p%d' % i, (128, 4, 128)) for i in range(4)]
            it = 0
            pi = 0
            for (kind, b, t0s, ln, mc) in c.seqs:
                for t0 in range(t0s, t0s + ln, 128):
                    x, sq, h = xt[it % 2], ss[it % 2], hst[it % 2]
                    s.ld(x[:, :], s.xres[t0:t0 + 128, :], ['xres'], [x])
                    s.act(junk[:, :], x[:, :], AF.Square, [x], [junk, sq], accum=sq[:, 0:1])
                    s.rsqrt(sq[:, 1:2], sq[:, 0:1], 1.0 / c.D, 1e-6, [sq], [sq])
                    s.ts('dve', x[:, :], x[:, :], sq[:, 1:2], None, ALU.mult, None, [x, sq], [x])
                    for q4 in range(KC // 4):
                        p_ = pt[pi % 4]
                        tm = tmp[pi % 2]
                        pi += 1
                        s.tr([(p_[:, j, :], x[:, (q4 * 4 + j) * 128:(q4 * 4 + j + 1) * 128]) for j in range(4)],
                             s.ident[:, :], [x, s.ident], [p_])
                        gb = s.gcol[:, 0, q4 * 4:q4 * 4 + 4, mc]
                        gb = AP(gb, gb.offset, [gb.ap[0], gb.ap[1], [0, 128]])
                        sb_ = s.modcol[:, q4 * 4:q4 * 4 + 4, mc]
                        sb_ = AP(sb_, sb_.offset, [sb_.ap[0], sb_.ap[1], [0, 128]])
                        s.tt('dve', tm[:, :, :], p_[:, :, :], gb, ALU.mult, [p_, s.gcol], [tm])
                        s.tt('pool', h[:, q4 * 4:q4 * 4 + 4, :], tm[:, :, :], sb_, ALU.add, [tm, s.modcol], [h])
                    s.ld(s.hT[:, t0:t0 + 128].rearrange("(k p) t -> p k t", p=128), h[:, :, :], [h], [('hT', t0)])
                    it += 1
            P.flush()

    def phase_inproj(s):
        c, P, l = s.c, s.P, s.l
        KC = c.KC
        G = 512
        nchunk = c.INC // 128
        sig0 = c.CONVEND // 128
        with ExitStack() as es:
            hb = [s.sb(es, 'iph%d' % i, (128, KC, G), BF16) for i in range(2)]
            wb = [s.sb(es, 'ipw%d' % i, (128, KC, 512), BF16) for i in range(3)]
            ob = [s.sb(es, 'ipo%d' % i, (128, 4, G)) for i in range(2)]
            pp = [s.ps(es, 'ipp%d' % i, (128, G)) for i in range(4)]
            wsrc = s.W['w_in'][l * c.D:(l + 1) * c.D, :].rearrange("(k p) n -> p k n", p=128)
            wi = 0
            pi = 0
            oi = 0
            for gi, t0 in enumerate(range(0, c.NT, G)):
                h = hb[gi % 2]
                n = min(G, c.NT - t0)
                s.ld(h[:, :, 0:n], s.hT[:, t0:t0 + n].rearrange("(k p) t -> p k t", p=128), ['hT_all'], [h])
                nch = nchunk
                if s.last and t0 + n <= c.NS * c.CTX:
                    nch = c.CTXC // 128
                for c0 in range(0, nch, 4):
                    n4 = min(4, nch - c0)
                    w = wb[wi % 3]
                    wi += 1
                    s.ld(w[:, :, 0:n4 * 128], wsrc[:, :, c0 * 128:(c0 + n4) * 128], ['wfull_in'], [w])
                    o = ob[oi % 2]
                    oi += 1
                    for j in range(n4):
                        p_ = pp[pi % 4]
                        pi += 1
                        s.mm(p_[:, 0:n], [(w[:, k, j * 128:(j + 1) * 128], h[:, k, 0:n]) for k in range(KC)], [w, h], [p_])
                        if c0 + j >= sig0:
                            s.act(o[:, j, 0:n], p_[:, 0:n], AF.Sigmoid, [p_], [(o, j)])
                        elif (c0 + j) % 2 == 0:
                            s.cp('act', o[:, j, 0:n], p_[:, 0:n], [p_], [(o, j)])
                        else:
                            s.cp('dve', o[:, j, 0:n], p_[:, 0:n], [p_], [(o, j)])
                    s.ld(s.pT[c0 * 128:(c0 + n4) * 128, t0:t0 + n].rearrange("(j p) t -> p j t", p=128),
                         o[:, 0:n4, 0:n], [(o, j) for j in range(n4)], [('pT', c0, t0)] + [(o, j) for j in range(n4)])
            P.flush()

    def load_halo(s, tile, n, row0, t0, seg, s0, s1):
        lo = max(t0 - 1, s0)
        hi = min(t0 + seg + 1, s1)
        a = lo - (t0 - 1)
        if a > 0:
            s.memset('pool', tile[0:n, 0:1], 0.0, [tile])
        if hi < t0 + seg + 1:
            s.memset('pool', tile[0:n, seg + 1:seg + 2], 0.0, [tile])
        s.ld(tile[0:n, a:a + (hi - lo)], s.pT[row0:row0 + n, lo:hi], ['pT_all'], [tile])

    def colvec(s, tile_col, vec_ap_1d_len_n):
        pass

    def phase_rwkv_pre(s):
        c, P, l = s.c, s.P, s.l
        I = s.I
        BC = c.BC
        BW = c.BW
        with ExitStack() as es:
            blocks = []
            for j in range(3 * BC):
                blocks.append((j * 128, 128))
            base = 3 * BW
            for j in range(2):
                blocks.append((base + j * c.DR, c.DR))
            base += 2 * c.DR
            for j in range(2):
                blocks.append((base + j * c.IR, c.IR))
            base += 2 * c.IR
            for j in range(c.GR // 128):
                blocks.append((base + j * 128, 128))
            NB = len(blocks)
            mu = s.sb(es, 'mu', (128, NB))
            omm = s.sb(es, 'omm', (128, NB))
            hmu = s.sb(es, 'hmu', (128, NB))
            s.memset('dve', mu[:, :], 0.0, [mu])
            for j, (r0, n) in enumerate(blocks):
                s.ldnc(mu[0:n, j:j + 1], I['shift_mu'][l:l + 1, r0:r0 + n].rearrange("o n -> n o"), [mu], [mu])
            s.ts('dve', omm[:, :], mu[:, :], -1.0, 1.0, ALU.mult, ALU.add, [mu], [omm])
            s.ts('dve', hmu[:, :], mu[:, :], 0.5, None, ALU.mult, None, [mu], [hmu])
            pc = s.sb(es, 'pcols', (128, 8, BC))
            def colload(idx, src2d_row):
                s.ldnc(pc[:, idx, :], src2d_row.rearrange("o (k p) -> p (o k)", p=128), [pc], [pc])
            s.memset('dve', pc[:, :, :], 0.0, [pc])
            colload(0, I['k_k'][l:l + 1, :])
            colload(1, I['k_a'][l:l + 1, :])
            for d in range(2):
                colload(3 + d, I['decay_bias'][2 * l + d:2 * l + d + 1, :])
                colload(5 + d, I['iclr_bias'][2 * l + d:2 * l + d + 1, :])
            if l > 0:
                colload(7, I['vres_bias'][l - 1:l, :])
            s.ts('dve', pc[:, 2, :], pc[:, 1, :], -1.0, 1.0, ALU.mult, ALU.add, [pc], [pc])
            dup = [s.sb(es, 'dup%d' % d, (c.DR, BW)) for d in range(2)]
            iup = [s.sb(es, 'iup%d' % d, (c.IR, BW)) for d in range(2)]
            for d in range(2):
                s.ld(dup[d][:, :], I['decay_up'][(2 * l + d) * c.DR:(2 * l + d + 1) * c.DR, :], (), [dup[d]])
                s.ld(iup[d][:, :], I['iclr_up'][(2 * l + d) * c.IR:(2 * l + d + 1) * c.IR, :], (), [iup[d]])
            if l > 0:
                vdn = s.sb(es, 'vdn', (128, BC, c.VR))
                vup = s.sb(es, 'vup', (c.VR, BW))
                s.ld(vdn[:, :, :], I['vres_down'][(l - 1) * BW:l * BW, :].rearrange("(k p) r -> p k r", p=128), (), [vdn])
                s.ld(vup[:, :], I['vres_up'][(l - 1) * c.VR:l * c.VR, :], (), [vup])
            SEG = 512
            NTB = 4
            Pin = [s.sb(es, 'rpP%d' % i, (128, SEG + 2)) for i in range(3)]
            tsum = [s.sb(es, 'rpT%d' % i, (128, SEG)) for i in range(2)]
            wdT = [s.sb(es, 'wdT%d' % d, (c.DR, SEG)) for d in range(2)]
            adT = [s.sb(es, 'adT%d' % d, (c.IR, SEG)) for d in range(2)]
            sg = [s.sb(es, 'sg%d' % j, (128, SEG)) for j in range(c.GR // 128)]
            vall = s.sb(es, 'vall', (128, BC, SEG))
            rt = s.sb(es, 'rt', (128, SEG))
            kt = s.sb(es, 'kt', (128, SEG))
            kh = s.sb(es, 'kh', (128, SEG))
            sq = s.sb(es, 'sqk', (128, SEG))
            kk = s.sb(es, 'kk', (128, SEG))
            nkk = s.sb(es, 'nkk', (128, SEG))
            wt = [s.sb(es, 'wt%d' % d, (128, SEG)) for d in range(2)]
            at = [s.sb(es, 'at%d' % d, (128, SEG)) for d in range(2)]
            kdt = [s.sb(es, 'kdt%d' % d, (128, SEG)) for d in range(2)]
            kat = [s.sb(es, 'kat%d' % d, (128, SEG)) for d in range(2)]
            vf = s.sb(es, 'vf', (128, SEG))
            vg = s.sb(es, 'vg', (128, SEG))
            vdT = s.sb(es, 'vdT', (c.VR, SEG))
            stg = [s.sb(es, 'stg%d' % i, (128, NTB, 128)) for i in range(3)]
            pA = [s.ps(es, 'rpA%d' % i, (128, SEG)) for i in range(3)]
            pTr = [s.ps(es, 'rpTr%d' % i, (128, NTB, 128)) for i in range(3)]
            pV = s.ps(es, 'rpV', (c.VR, SEG))
            cnt = {'p': 0, 't': 0, 'a': 0, 'tr': 0}

            def shift(dst, blk, t0, seg, s0, s1, res=None):
                res = res if res is not None else dst
                r0, n = blocks[blk]
                Pt = Pin[cnt['p'] % 3]
                cnt['p'] += 1
                ts_ = tsum[cnt['t'] % 2]
                cnt['t'] += 1
                s.load_halo(Pt, n, r0, t0, seg, s0, s1)
                s.tt('pool', ts_[0:n, 0:seg], Pt[0:n, 0:seg], Pt[0:n, 2:seg + 2], ALU.add, [Pt], [ts_])
                s.act(dst[0:n, 0:seg], Pt[0:n, 1:seg + 1], AF.Copy, [Pt, omm], [res], scale=omm[0:n, blk:blk + 1])
                s.stt('dve', dst[0:n, 0:seg], ts_[0:n, 0:seg], hmu[0:n, blk:blk + 1], dst[0:n, 0:seg],
                      ALU.mult, ALU.add, [ts_, res, hmu], [res])

            def emit_stream(name, tile, b, ch, tall0, seg, res=None):
                res = res if res is not None else tile
                ntb = seg // 128
                p_ = pTr[cnt['tr'] % 3]
                st = stg[cnt['tr'] % 3]
                cnt['tr'] += 1
                s.tr([(p_[:, j, :], tile[:, j * 128:(j + 1) * 128]) for j in range(ntb)], s.ident[:, :],
                     [res, s.ident], [p_])
                s.cp('act' if cnt['tr'] % 2 else 'dve', st[:, 0:ntb, :], p_[:, 0:ntb, :], [p_], [st])
                for tb in range(ntb):
                    dst = s.strm[name][b, 2 * ch:2 * ch + 2, tall0 + tb * 128:tall0 + (tb + 1) * 128, :].rearrange(
                        "h p j -> p h j")
                    src = st[:, tb, :].rearrange("p (h j) -> p h j", j=64)
                    s.ld(dst, src, [st], [('strm', name, b, ch, tall0, tb)])

            for (kind, b, t0s, ln, mc) in c.seqs:
                tallb = 0 if kind == 'c' else c.CTX
                for t0 in range(t0s, t0s + ln, SEG):
                    seg = min(SEG, t0s + ln - t0)
                    tall0 = tallb + (t0 - t0s)
                    a = (t0, seg, t0s, t0s + ln)
                    for d in range(2):
                        shift(wdT[d], 3 * BC + d, *a)
                        s.act(wdT[d][:, 0:seg], wdT[d][:, 0:seg], AF.Tanh, [wdT[d]], [wdT[d]])
                        shift(adT[d], 3 * BC + 2 + d, *a)
                    for j in range(c.GR // 128):
                        shift(sg[j], 3 * BC + 4 + j, *a)
                        s.act(sg[j][:, 0:seg], sg[j][:, 0:seg], AF.Sigmoid, [sg[j]], [sg[j]])
                        s.ld(s.sgdT[j * 128:(j + 1) * 128, t0:t0 + seg], sg[j][:, 0:seg], [sg[j]], [('sgdT', j, t0)])
                    for ch in range(BC):
                        shift(vall[:, ch, :], 2 * BC + ch, *a, res=vall)
                    if l == 0:
                        for ch in range(BC):
                            s.ld(s.vfT[ch * 128:(ch + 1) * 128, t0:t0 + seg], vall[:, ch, 0:seg], [vall],
                                 [('vfT', ch, t0)])
                    else:
                        s.mm(pV[:, 0:seg], [(vdn[:, ch, :], vall[:, ch, 0:seg]) for ch in range(BC)], [vdn, vall], [pV])
                        s.cp('dve', vdT[:, 0:seg], pV[:, 0:seg], [pV], [vdT])
                        for ch in range(BC):
                            p_ = pA[cnt['a'] % 3]
                            cnt['a'] += 1
                            s.mm(p_[:, 0:seg], [(vup[:, ch * 128:(ch + 1) * 128], vdT[:, 0:seg])], [vup, vdT], [p_])
                            s.act(vg[:, 0:seg], p_[:, 0:seg], AF.Sigmoid, [p_, pc], [vg], bias=pc[:, 7, ch:ch + 1])
                            s.ld(vf[:, 0:seg], s.vfT[ch * 128:(ch + 1) * 128, t0:t0 + seg], ['vfT_all'], [vf])
                            s.tt('dve', vf[:, 0:seg], vf[:, 0:seg], vall[:, ch, 0:seg], ALU.subtract, [vf, vall], [vf])
                            s.tt('dve', vf[:, 0:seg], vf[:, 0:seg], vg[:, 0:seg], ALU.mult, [vf, vg], [vf])
                            s.tt('dve', vall[:, ch, 0:seg], vall[:, ch, 0:seg], vf[:, 0:seg], ALU.add, [vf, vall], [vall])
                    for ch in range(BC):
                        shift(rt, ch, *a)
                        shift(kt, BC + ch, *a)
                        s.ts('dve', kh[:, 0:seg], kt[:, 0:seg], pc[:, 0, ch:ch + 1], None, ALU.mult, None, [kt, pc], [kh])
                        s.tt('pool', sq[:, 0:seg], kh[:, 0:seg], kh[:, 0:seg], ALU.mult, [kh], [sq])
                        p_ = pA[cnt['a'] % 3]
                        cnt['a'] += 1
                        s.mm(p_[:, 0:seg], [(s.blk[:, :], sq[:, 0:seg])], [s.blk, sq], [p_])
                        s.ts('dve', sq[:, 0:seg], p_[:, 0:seg], 1e-24, None, ALU.max, None, [p_], [sq])
                        s.rsqrt(sq[:, 0:seg], sq[:, 0:seg], 1.0, 0.0, [sq], [sq])
                        s.tt('dve', kk[:, 0:seg], kh[:, 0:seg], sq[:, 0:seg], ALU.mult, [kh, sq], [kk])
                        s.ts('pool', nkk[:, 0:seg], kk[:, 0:seg], -1.0, None, ALU.mult, None, [kk], [nkk])
                        for d in range(2):
                            p_ = pA[cnt['a'] % 3]
                            cnt['a'] += 1
                            s.mm(p_[:, 0:seg], [(dup[d][:, ch * 128:(ch + 1) * 128], wdT[d][:, 0:seg])],
                                 [dup[d], wdT[d]], [p_])
                            s.act(wt[d][:, 0:seg], p_[:, 0:seg], AF.Sigmoid, [p_, pc], [wt[d]],
                                  bias=pc[:, 3 + d, ch:ch + 1])
                            s.act(wt[d][:, 0:seg], wt[d][:, 0:seg], AF.Exp, [wt[d]], [wt[d]],
                                  scale=-0.6065306597126334)
                            p_ = pA[cnt['a'] % 3]
                            cnt['a'] += 1
                            s.mm(p_[:, 0:seg], [(iup[d][:, ch * 128:(ch + 1) * 128], adT[d][:, 0:seg])],
                                 [iup[d], adT[d]], [p_])
                            s.act(at[d][:, 0:seg], p_[:, 0:seg], AF.Sigmoid, [p_, pc], [at[d]],
                                  bias=pc[:, 5 + d, ch:ch + 1])
                            s.ts('dve', kdt[d][:, 0:seg], at[d][:, 0:seg], pc[:, 1, ch:ch + 1], pc[:, 2, ch:ch + 1],
                                 ALU.mult, ALU.add, [at[d], pc], [kdt[d]])
                            s.tt('dve', kdt[d][:, 0:seg], kdt[d][:, 0:seg], kt[:, 0:seg], ALU.mult, [kdt[d], kt],
                                 [kdt[d]])
                            s.tt('pool', kat[d][:, 0:seg], kk[:, 0:seg], at[d][:, 0:seg], ALU.mult, [kk, at[d]],
                                 [kat[d]])
                            emit_stream('w%d' % d, wt[d], b, ch, tall0, seg)
                            emit_stream('kd%d' % d, kdt[d], b, ch, tall0, seg)
                            emit_stream('ka%d' % d, kat[d], b, ch, tall0, seg)
                        emit_stream('nkk', nkk, b, ch, tall0, seg)
                        emit_stream('r', rt, b, ch, tall0, seg)
                        emit_stream('v', vall[:, ch, :], b, ch, tall0, seg, res=vall)
            P.flush()

    def phase_scan(s):
        c, P = s.c, s.P
        TC = 16
        IQ, IP, H, NS = c.IQ, c.IP, c.H, c.NS
        NBH = NS * H
        names = ['nkk', 'ka', 'w', 'kd', 'r']
        with ExitStack() as es:
            S = [s.sb(es, 'S%d' % i, (128, 2, IP, 64)) for i in range(2)]
            t1 = s.sb(es, 'sc_t1', (128, 2, IP, 64))
            t2 = s.sb(es, 'sc_t2', (128, 2, IP, 64))
            Sw = s.sb(es, 'sc_sw', (128, 2, IP, 64))
            t3 = [s.sb(es, 'sc_t3%d' % i, (128, 2, IP, 64)) for i in range(2)]
            t4 = [s.sb(es, 'sc_t4%d' % i, (128, 2, IP, 64)) for i in range(2)]
            sa = s.sb(es, 'sc_sa', (128, 2, IP))
            st = {n: [s.sb(es, 'sc_%s%d' % (n, i), (128, 2, TC, 64)) for i in range(2)] for n in names}
            vt = [s.sb(es, 'sc_v%d' % i, (128, 2, TC, IP)) for i in range(2)]
            yb = [s.sb(es, 'sc_y%d' % i, (128, 2, TC, IP)) for i in range(2)]
            for hf in range(2):
                s.memset('dve', S[0][:, hf, :, :], 0.0, [(S[0], hf)])
            cur = 0
            step = 0
            ci = 0
            pending = []
            stores = []

            def flush_pending(which):
                for fn in pending:
                    fn(which)

            for (tb0, T) in ((0, c.CTX), (c.CTX, c.SEQ)):
                for ck in range(T // TC):
                    k = ci % 2
                    ci += 1
                    tf = tb0 + ck * TC
                    tr_ = tb0 + T - (ck + 1) * TC
                    for n in names:
                        for d in range(2):
                            tt0 = tf if d == 0 else tr_
                            nm = n if n in ('nkk', 'r') else '%s%d' % (n, d)
                            src = s.strm[nm][:, :, tt0:tt0 + TC, :].rearrange("b h t j -> (b h) t j")
                            for iq in range(IQ):
                                s.ld(st[n][k][iq * NBH:(iq + 1) * NBH, d, :, :], src, ['strm_all'], [(st[n][k], d)])
                    for d in range(2):
                        tt0 = tf if d == 0 else tr_
                        for iq in range(IQ):
                            src = s.strm['v'][:, :, tt0:tt0 + TC, iq * IP:(iq + 1) * IP].rearrange(
                                "b h t j -> (b h) t j")
                            s.ldnc(vt[k][iq * NBH:(iq + 1) * NBH, d, :, :], src, ['strm_all'], [(vt[k], d)])
                    y = yb[k]
                    for sp in range(TC):
                        So, Sn = S[cur], S[1 - cur]
                        T3 = t3[step % 2]
                        T4 = t4[step % 2]

                        def jb(tile, hf):
                            a = tile[:, hf, sp if hf == 0 else TC - 1 - sp, :]
                            return AP(a, a.offset, [a.ap[0], [0, IP], [1, 64]])

                        def ib(tile, hf):
                            a = tile[:, hf, sp if hf == 0 else TC - 1 - sp, :]
                            return AP(a, a.offset, [a.ap[0], [1, IP], [0, 64]])

                        def jb2(tile):
                            a = tile[:, 0, sp, :]
                            return AP(a, a.offset, [a.ap[0], [(2 * TC - 1 - 2 * sp) * 64, 2], [0, IP], [1, 64]])

                        def ib2(tile):
                            a = tile[:, 0, sp, :]
                            return AP(a, a.offset, [a.ap[0], [(2 * TC - 1 - 2 * sp) * IP, 2], [1, IP], [0, 64]])

                        s.tt('pool', T3[:, :, :, :], ib2(vt[k]), jb2(st['kd'][k]), ALU.mult,
                             [(vt[k], 0), (vt[k], 1), (st['kd'][k], 0), (st['kd'][k], 1)], [(T3, 0), (T3, 1)])
                        for hf in range(2):
                            s.tt('pool', Sw[:, hf, :, :], So[:, hf, :, :], jb(st['w'][k], hf), ALU.mult,
                                 [(So, hf), (st['w'][k], hf)], [(Sw, hf)])
                        for hf in range(2):
                            s.tt('dve', t1[:, hf, :, :], So[:, hf, :, :], jb(st['nkk'][k], hf), ALU.mult,
                                 [(So, hf), (st['nkk'][k], hf)], [(t1, hf)])
                        for hf in range(2):
                            s.red(sa[:, hf, :], t1[:, hf, :, :], [(t1, hf)], [(sa, hf)])
                        flush_pending('pool')
                        for hf in range(2):
                            a = sa[:, hf, :]
                            sab = AP(a, a.offset, [a.ap[0], [1, IP], [0, 64]])
                            s.tt('dve', t2[:, hf, :, :], sab, jb(st['ka'][k], hf), ALU.mult,
                                 [(sa, hf), (st['ka'][k], hf)], [(t2, hf)])
                        for hf in range(2):
                            s.tt('dve', Sn[:, hf, :, :], Sw[:, hf, :, :], t2[:, hf, :, :], ALU.add,
                                 [(Sw, hf), (t2, hf)], [(Sn, hf)])
                        for hf in range(2):
                            s.tt('dve', Sn[:, hf, :, :], Sn[:, hf, :, :], T3[:, hf, :, :], ALU.add,
                                 [(Sn, hf), (T3, hf)], [(Sn, hf)])
                        flush_pending('dve')
                        pending.clear()
                        for fn in stores:
                            fn()
                        stores.clear()

                        def mk(Sn, T4, y, k, sp):
                            def fn(which):
                                if which == 'pool':
                                    a = st['r'][k][:, 0, sp, :]
                                    rb = AP(a, a.offset, [a.ap[0], [(2 * TC - 1 - 2 * sp) * 64, 2], [0, IP], [1, 64]])
                                    s.tt('pool', T4[:, :, :, :], Sn[:, :, :, :], rb, ALU.mult,
                                         [(Sn, 0), (Sn, 1), (st['r'][k], 0), (st['r'][k], 1)], [(T4, 0), (T4, 1)])
                                else:
                                    a = y[:, 0, sp, :]
                                    yo = AP(a, a.offset, [a.ap[0], [(2 * TC - 1 - 2 * sp) * IP, 2], [1, IP]])
                                    s.red(yo, T4[:, :, :, :], [(T4, 0), (T4, 1)], [(y, 0), (y, 1)])
                            return fn
                        pending.append(mk(Sn, T4, y, k, sp))
                        cur = 1 - cur
                        step += 1

                    def mkstore(y, tf, tr_):
                        def fn():
                            for d in range(2):
                                tt0 = tf if d == 0 else tr_
                                for iq in range(IQ):
                                    dst = s.ysc[d][:, :, tt0:tt0 + TC, iq * IP:(iq + 1) * IP].rearrange(
                                        "b h t j -> (b h) t j")
                                    s.ldnc(dst, y[iq * NBH:(iq + 1) * NBH, d, :, :], [(y, d)], [('ysc', d, tt0, iq)])
                        return fn
                    stores.append(mkstore(y, tf, tr_))
            flush_pending('pool')
            flush_pending('dve')
            for fn in stores:
                fn()
            P.flush()

    def phase_rwkv_post(s):
        c, P, l = s.c, s.P, s.l
        I = s.I
        BW, H, BC = c.BW, c.H, c.BC
        with ExitStack() as es:
            def rowb(name, src_row):
                t = s.sb(es, name, (128, BW))
                s.ld(t[:, :], AP(src_row, src_row.offset, [[0, 128], [1, BW]]), (), [t])
                return t
            gnw = rowb('gnw', I['gn_w'][l:l + 1, :])
            gnb = rowb('gnb', I['gn_b'][l:l + 1, :])
            rkr = rowb('rkr', I['r_k'][l:l + 1, :])
            gup = s.sb(es, 'gup', (128, c.GR // 128, BW))
            s.ld(gup[:, :, :], I['gate_up'][l * c.GR:(l + 1) * c.GR, :].rearrange("(k p) n -> p k n", p=128), (), [gup])
            nb = 2
            y0 = [s.sb(es, 'po_y0%d' % i, (128, H, 64)) for i in range(nb)]
            y1 = [s.sb(es, 'po_y1%d' % i, (128, H, 64)) for i in range(nb)]
            rr = [s.sb(es, 'po_r%d' % i, (128, H, 64)) for i in range(nb)]
            k0 = [s.sb(es, 'po_k0%d' % i, (128, H, 64)) for i in range(nb)]
            k1 = [s.sb(es, 'po_k1%d' % i, (128, H, 64)) for i in range(nb)]
            vv = [s.sb(es, 'po_v%d' % i, (128, H, 64)) for i in range(nb)]
            sgt = [s.sb(es, 'po_sg%d' % i, (128, c.GR // 128, 128)) for i in range(nb)]
            sq = s.sb(es, 'po_sq', (128, H, 64))
            st = s.sb(es, 'po_st', (128, 4, H))
            ob = [s.sb(es, 'po_o%d' % i, (128, BC, 128), BF16) for i in range(2)]
            pg = [s.ps(es, 'po_pg%d' % i, (128, 512)) for i in range(max(BW // 512, 1))]
            ptr = [s.ps(es, 'po_pt%d' % i, (128, 4, 128)) for i in range(2)]
            it = 0
            for (kind, b, t0s, ln, mc) in c.seqs:
                if s.last and kind == 'c':
                    continue
                tallb = 0 if kind == 'c' else c.CTX
                for t0 in range(t0s, t0s + ln, 128):
                    k = it % nb
                    it += 1
                    ta = tallb + (t0 - t0s)
                    def tok(dr):
                        return dr[b, :, ta:ta + 128, :].rearrange("h t j -> t h j")
                    s.ld(y0[k][:, :, :], tok(s.ysc[0]), ['ysc_all'], [y0[k]])
                    s.ld(y1[k][:, :, :], tok(s.ysc[1]), ['ysc_all'], [y1[k]])
                    s.ld(rr[k][:, :, :], tok(s.strm['r']), ['strm_all'], [rr[k]])
                    s.ld(k0[k][:, :, :], tok(s.strm['kd0']), ['strm_all'], [k0[k]])
                    s.ld(k1[k][:, :, :], tok(s.strm['kd1']), ['strm_all'], [k1[k]])
                    s.ld(vv[k][:, :, :], tok(s.strm['v']), ['strm_all'], [vv[k]])
                    s.ld(sgt[k][:, :, :], s.sgdT[:, t0:t0 + 128].rearrange("(k p) t -> p k t", p=128), ['sgdT_all'],
                         [sgt[k]])
                    Y, Y1, R, K0, K1, V = y0[k], y1[k], rr[k], k0[k], k1[k], vv[k]
                    def hb(ap2):
                        return AP(ap2, ap2.offset, [ap2.ap[0], ap2.ap[1], [0, 64]])
                    s.tt('dve', Y[:, :, :], Y[:, :, :], Y1[:, :, :], ALU.add, [Y, Y1], [Y])
                    s.red(st[:, 0, :], Y[:, :, :], [Y], [st])
                    s.ts('dve', st[:, 0, :], st[:, 0, :], 1.0 / 64, None, ALU.mult, None, [st], [st])
                    s.tt('dve', Y[:, :, :], Y[:, :, :], hb(st[:, 0, :]), ALU.subtract, [Y, st], [Y])
                    s.tt('pool', sq[:, :, :], Y[:, :, :], Y[:, :, :], ALU.mult, [Y], [sq])
                    s.red(st[:, 1, :], sq[:, :, :], [sq], [st])
                    s.rsqrt(st[:, 1, :], st[:, 1, :], 1.0 / 64, 64e-5, [st], [st])
                    s.tt('dve', Y[:, :, :], Y[:, :, :], hb(st[:, 1, :]), ALU.mult, [Y, st], [Y])
                    Yf = Y[:, :, :].rearrange("p h j -> p (h j)")
                    s.tt('pool', Yf, Yf, gnw[:, :], ALU.mult, [Y, gnw], [Y])
                    s.tt('pool', Yf, Yf, gnb[:, :], ALU.add, [Y, gnb], [Y])
                    s.tt('pool', K0[:, :, :], K0[:, :, :], K1[:, :, :], ALU.add, [K0, K1], [K0])
                    s.tt('pool', K0[:, :, :], K0[:, :, :], R[:, :, :], ALU.mult, [K0, R], [K0])
                    K0f = K0[:, :, :].rearrange("p h j -> p (h j)")
                    s.tt('pool', K0f, K0f, rkr[:, :], ALU.mult, [K0, rkr], [K0])
                    s.red(st[:, 2, :], K0[:, :, :], [K0], [st])
                    s.tt('dve', V[:, :, :], V[:, :, :], hb(st[:, 2, :]), ALU.mult, [V, st], [V])
                    s.tt('dve', Y[:, :, :], Y[:, :, :], V[:, :, :], ALU.add, [Y, V], [Y])
                    for gi in range(len(pg)):
                        n0 = gi * 512
                        n1 = min(BW, n0 + 512)
                        s.mm(pg[gi][:, 0:n1 - n0], [(sgt[k][:, kk_, :], gup[:, kk_, n0:n1]) for kk_ in range(c.GR // 128)],
                             [sgt[k], gup], [pg[gi]])
                        s.tt('dve', Yf[:, n0:n1], Yf[:, n0:n1], pg[gi][:, 0:n1 - n0], ALU.mult, [Y, pg[gi]], [Y])
                    o = ob[it % 2]
                    for q4 in range(0, BC, 4):
                        n4 = min(4, BC - q4)
                        p_ = ptr[(q4 // 4) % 2]
                        s.tr([(p_[:, j, :], Yf[:, (q4 + j) * 128:(q4 + j + 1) * 128]) for j in range(n4)], s.ident[:, :],
                             [Y, s.ident], [p_])
                        s.cp('act', o[:, q4:q4 + n4, :], p_[:, 0:n4, :], [p_], [o])
                    s.ld(s.brT[0][:, t0:t0 + 128].rearrange("(k p) t -> p k t", p=128), o[:, :, :], [o], [('br0', t0)])
            P.flush()

    def phase_attn(s):
        c, P, l = s.c, s.P, s.l
        I = s.I
        KVH, H = c.KVH, c.H
        TA = c.TALL
        NBK = TA // 128
        NCB = c.CTX // 128
        G = 4
        with ExitStack() as es:
            rope = s.sb(es, 'rope', (64, 2, c.SEQ))
            s.ld(rope[:, :, :], I['k_rope'].rearrange("(a n) t -> n a t", a=2), (), [rope])
            esk = s.sb(es, 'esk', (64, H))
            s.ld(esk[:, :], AP(I['attn_sink'], l * H, [[0, 64], [1, H]]), (), [esk])
            s.act(esk[:, :], esk[:, :], AF.Exp, [esk], [esk])
            kT = s.sb(es, 'kT', (64, KVH, TA), BF16)
            qT = s.sb(es, 'qT', (64, G, TA), BF16)
            vtk = s.sb(es, 'vtk', (128, NBK, KVH, 64), BF16)
            xin = [s.sb(es, 'at_x%d' % i, (64, 512)) for i in range(3)]
            t1 = [s.sb(es, 'at_t%d' % i, (64, 512)) for i in range(2)]
            eb = [s.sb(es, 'at_e%d' % i, (128, G, 128), BF16) for i in range(3)]
            den = s.sb(es, 'at_den', (64, G, 128))
            ot = [s.sb(es, 'at_o%d' % i, (64, G, 128), BF16) for i in range(2)]
            psw = [s.ps(es, 'at_ps%d' % i, (64, 512)) for i in range(2)]
            pss = [s.ps(es, 'at_s%d' % i, (128, G, 128)) for i in range(2)]
            pso = s.ps(es, 'at_po', (64, G, 128))
            psd = s.ps(es, 'at_pd', (64, G, 128))
            psv = s.ps(es, 'at_pv', (128, 4, 64))
            cnt = {'x': 0, 'e': 0, 'o': 0}

            def load_feat(dst, row0, b, with_q_ctx, res):
                segs = []
                tc0 = b * c.CTX
                for t in range(0, c.CTX, 512):
                    n = min(512, c.CTX - t)
                    segs.append((tc0 + t, t, n, None))
                tl0 = c.NS * c.CTX + b * c.SEQ
                for t in range(0, c.SEQ, 512):
                    n = min(512, c.SEQ - t)
                    segs.append((tl0 + t, c.CTX + t, n, t))
                for (tg, td, n, tp) in segs:
                    if tp is None and not with_q_ctx:
                        continue
                    x = xin[cnt['x'] % 3]
                    cnt['x'] += 1
                    s.ld(x[:, 0:n], s.pT[row0:row0 + 64, tg:tg + n], ['pT_all'], [x])
                    if tp is None:
                        s.cp('act', dst[:, td:td + n], x[:, 0:n], [x], [res])
                    else:
                        p_ = psw[cnt['x'] % 2]
                        ta_, tb_ = t1[0], t1[1]
                        s.mm(p_[:, 0:n], [(s.swp[:, :], x[:, 0:n])], [s.swp, x], [p_])
                        s.tt('pool', ta_[:, 0:n], x[:, 0:n], rope[:, 0, tp:tp + n], ALU.mult, [x, rope], [ta_])
                        s.tt('dve', tb_[:, 0:n], p_[:, 0:n], rope[:, 1, tp:tp + n], ALU.mult, [p_, rope], [tb_])
                        s.tt('dve', dst[:, td:td + n], ta_[:, 0:n], tb_[:, 0:n], ALU.add, [ta_, tb_], [res])

            for b in range(c.NS):
                for kh in range(KVH):
                    load_feat(kT[:, kh, :], c.RC + kh * 64, b, True, kT)
                    tc0 = b * c.CTX
                    tl0 = c.NS * c.CTX + b * c.SEQ
                    for blk0 in range(0, NBK, 4):
                        nb_ = min(4, NBK - blk0)
                        x = xin[cnt['x'] % 3]
                        cnt['x'] += 1
                        for j in range(nb_):
                            kb = blk0 + j
                            tg = tc0 + kb * 128 if kb < NCB else tl0 + (kb - NCB) * 128
                            s.ld(x[:, j * 128:(j + 1) * 128], s.pT[c.RC + c.KVW + kh * 64:c.RC + c.KVW + kh * 64 + 64,
                                                                  tg:tg + 128], ['pT_all'], [x])
                        s.tr([(psv[:, j, :], x[:, j * 128:(j + 1) * 128]) for j in range(nb_)], s.ident[0:64, 0:64],
                             [x, s.ident], [psv])
                        s.cp('act', vtk[:, blk0:blk0 + nb_, kh, :], psv[:, 0:nb_, :], [psv], [vtk])
                for kh in range(KVH):
                    for g in range(G):
                        load_feat(qT[:, g, :], c.CTXC + (kh * G + g) * 64, b, not s.last, qT)
                    qblocks = []
                    if not s.last:
                        for n in range(NCB):
                            qblocks.append(('c', n))
                    for n in range(c.SEQ // 128):
                        qblocks.append(('l', n))
                    for (qk, n) in qblocks:
                        if qk == 'c':
                            q0 = n * 128
                            kbs = [(kb, None) for kb in range(NCB)]
                            tok0 = b * c.CTX + n * 128
                        else:
                            q0 = c.CTX + n * 128
                            kbs = [(kb, None) for kb in range(NCB)]
                            nl = c.SEQ // 128
                            if n > 0:
                                kbs.append((NCB + n - 1, 0))
                            kbs.append((NCB + n, None))
                            if n < nl - 1:
                                kbs.append((NCB + n + 1, 1))
                            tok0 = c.NS * c.CTX + b * c.SEQ + n * 128
                        for i, (kb, mk) in enumerate(kbs):
                            ps_ = pss[cnt['e'] % 2]
                            e = eb[cnt['e'] % 3]
                            cnt['e'] += 1
                            s.mm(ps_[:, :, :], [(kT[:, kh, kb * 128:(kb + 1) * 128], qT[:, :, q0:q0 + 128])],
                                 [kT, qT], [ps_])
                            s.act(e[:, :, :], ps_[:, :, :], AF.Exp, [ps_], [e], scale=0.125)
                            if mk is not None:
                                m = s.masks[:, mk, :]
                                mb = AP(m, m.offset, [m.ap[0], [0, G], m.ap[1]])
                                s.tt('dve', e[:, :, :], e[:, :, :], mb, ALU.mult, [e, s.masks], [e])
                            s.mm(pso[:, :, :], [(vtk[:, kb, kh, :], e[:, :, :])], [vtk, e], [pso],
                                 start=(i == 0), stop=(i == len(kbs) - 1))
                            s.mm(psd[:, :, :], [(s.onesb[:, :], e[:, :, :])], [s.onesb, e], [psd],
                                 start=(i == 0), stop=(i == len(kbs) - 1))
                        a = esk[:, kh * G:(kh + 1) * G]
                        eskb = AP(a, a.offset, [a.ap[0], a.ap[1], [0, 128]])
                        s.tt('dve', den[:, :, :], psd[:, :, :], eskb, ALU.add, [psd, esk], [den])
                        s.P.op('dve', lambda g_: g_.reciprocal(out=den[:, :, :], in_=den[:, :, :]), [den], [den])
                        o = ot[cnt['o'] % 2]
                        cnt['o'] += 1
                        s.tt('dve', o[:, :, :], pso[:, :, :], den[:, :, :], ALU.mult, [pso, den], [o])
                        s.ld(s.brT[2][kh * G * 64:(kh + 1) * G * 64, tok0:tok0 + 128].rearrange("(g p) t -> p g t", p=64),
                             o[:, :, :], [o], [('br2', kh, tok0)])
            P.flush()

    def phase_conv(s):
        c, P, l = s.c, s.P, s.l
        BC, BW = c.BC, c.BW
        SEG = 512
        with ExitStack() as es:
            cw = s.sb(es, 'cw', (128, 3, BC))
            for j in range(3):
                s.ldnc(cw[:, j, :], s.I['conv_w'][3 * l + j:3 * l + j + 1, :].rearrange("o (k p) -> p (o k)", p=128),
                       [cw], [cw])
            bt = [s.sb(es, 'cv_b%d' % i, (128, SEG)) for i in range(2)]
            ct = [s.sb(es, 'cv_c%d' % i, (128, SEG + 2)) for i in range(2)]
            ut = [s.sb(es, 'cv_u%d' % i, (128, SEG + 2)) for i in range(2)]
            o = [s.sb(es, 'cv_o%d' % i, (128, SEG)) for i in range(2)]
            ob = [s.sb(es, 'cv_ob%d' % i, (128, SEG), BF16) for i in range(2)]
            it = 0
            for (kind, b, t0s, ln, mc) in c.seqs:
                if s.last and kind == 'c':
                    continue
                for t0 in range(t0s, t0s + ln, SEG):
                    seg = min(SEG, t0s + ln - t0)
                    for ch in range(BC):
                        k = it % 2
                        it += 1
                        B_, C_, U_, O_, OB = bt[k], ct[k], ut[k], o[k], ob[k]
                        s.ld(B_[:, 0:seg], s.pT[c.QEND + ch * 128:c.QEND + (ch + 1) * 128, t0:t0 + seg], ['pT_all'], [B_])
                        s.load_halo(C_, 128, c.QEND + BW + ch * 128, t0, seg, t0s, t0s + ln)
                        s.load_halo(U_, 128, c.QEND + 2 * BW + ch * 128, t0, seg, t0s, t0s + ln)
                        s.tt('pool', C_[:, 0:seg + 2], C_[:, 0:seg + 2], U_[:, 0:seg + 2], ALU.mult, [C_, U_], [C_])
                        s.ts('dve', O_[:, 0:seg], C_[:, 1:seg + 1], cw[:, 1, ch:ch + 1], None, ALU.mult, None, [C_, cw], [O_])
                        s.stt('dve', O_[:, 0:seg], C_[:, 0:seg], cw[:, 0, ch:ch + 1], O_[:, 0:seg], ALU.mult, ALU.add,
                              [C_, cw, O_], [O_])
                        s.stt('dve', O_[:, 0:seg], C_[:, 2:seg + 2], cw[:, 2, ch:ch + 1], O_[:, 0:seg], ALU.mult, ALU.add,
                              [C_, cw, O_], [O_])
                        s.tt('pool', OB[:, 0:seg], O_[:, 0:seg], B_[:, 0:seg], ALU.mult, [O_, B_], [OB])
                        s.ld(s.brT[1][ch * 128:(ch + 1) * 128, t0:t0 + seg], OB[:, 0:seg], [OB], [('br1', ch, t0)])
            P.flush()

    def phase_merge(s):
        c, P, l = s.c, s.P, s.l
        KC, BC, D, BW = c.KC, c.BC, c.D, c.BW
        G = 512
        NS1 = c.NS + 1
        with ExitStack() as es:
            br = [[s.sb(es, 'mg_br%d_%d' % (i, k), (128, BC, G), BF16) for k in range(1)] for i in range(3)]
            wb = [s.sb(es, 'mg_w%d' % i, (128, BC, 512), BF16) for i in range(2)]
            wo = [s.sb(es, 'mg_wo%d' % i, (128, KC, 512), BF16) for i in range(2)]
            gt = [s.sb(es, 'mg_g%d' % i, (128, 4, G)) for i in range(2)]
            mT = s.sb(es, 'mg_m', (128, KC, G), BF16)
            acc = s.sb(es, 'mg_acc', (128, 4, G))
            tmp = s.sb(es, 'mg_tmp', (128, G))
            xt = [s.sb(es, 'mg_x%d' % i, (128, D)) for i in range(4)]
            g2t = s.sb(es, 'mg_gate', (128, D))
            pp = [s.ps(es, 'mg_p%d' % i, (128, 512)) for i in range(4)]
            M6 = 6 * D
            wbs = s.W['w_branch']
            wos = s.W['w_out'][l * D:(l + 1) * D, :].rearrange("(k p) n -> p k n", p=128)
            wi = 0
            pi = 0
            gi_ = 0
            xi = 0
            groups = []
            for (kind, b, t0s, ln, mc) in c.seqs:
                if s.last and kind == 'c':
                    continue
                for t0 in range(t0s, t0s + ln, G):
                    groups.append((t0, min(G, t0s + ln - t0), mc))
            for gidx, (t0, n, mc) in enumerate(groups):
                k = 0
                s.ld(g2t[:, :], AP(s.modrow, (l * NS1 + mc) * M6 + 2 * D, [[0, 128], [1, D]]), ['modrow_all'], [g2t])
                for i in range(3):
                    s.ld(br[i][k][:, :, 0:n], s.brT[i][:, t0:t0 + n].rearrange("(k p) t -> p k t", p=128), ['br_all'],
                         [br[i][k]])
                for oq in range(D // 512):
                    for i in range(3):
                        w = wb[wi % 2]
                        wi += 1
                        s.ld(w[:, :, :], wbs[(l * 3 + i) * BW:(l * 3 + i + 1) * BW, oq * 512:(oq + 1) * 512].rearrange(
                            "(k p) n -> p k n", p=128), ['wfull_b'], [w])
                        gts = gt[gi_ % 2]
                        gi_ += 1
                        r0 = c.CONVEND + i * D + oq * 512
                        s.ld(gts[:, :, 0:n], s.pT[r0:r0 + 512, t0:t0 + n].rearrange("(j p) t -> p j t", p=128), ['pT_all'],
                             [gts])
                        for j in range(4):
                            p_ = pp[pi % 4]
                            pi += 1
                            s.mm(p_[:, 0:n], [(w[:, kk_, j * 128:(j + 1) * 128], br[i][k][:, kk_, 0:n]) for kk_ in range(BC)],
                                 [w, br[i][k]], [p_])
                            if i == 0:
                                s.tt('dve', acc[:, j, 0:n], p_[:, 0:n], gts[:, j, 0:n], ALU.mult, [p_, gts], [(acc, j)])
                            else:
                                s.tt('dve', tmp[:, 0:n], p_[:, 0:n], gts[:, j, 0:n], ALU.mult, [p_, gts], [tmp])
                                if i == 1:
                                    s.tt('pool', acc[:, j, 0:n], acc[:, j, 0:n], tmp[:, 0:n], ALU.add, [(acc, j), tmp],
                                         [(acc, j)])
                                else:
                                    s.tt('pool', mT[:, oq * 4 + j, 0:n], acc[:, j, 0:n], tmp[:, 0:n], ALU.add,
                                         [(acc, j), tmp], [(mT, oq * 4 + j)])
                allm = [(mT, j) for j in range(KC)]
                wts = []
                for oq in range(D // 512):
                    w = wo[oq % 2]
                    s.ld(w[:, :, :], wos[:, :, oq * 512:(oq + 1) * 512], ['wfull_o'], [w])
                    for tb in range(n // 128):
                        x = xt[tb]
                        if oq == 0:
                            s.ld(x[:, :], s.xres[t0 + tb * 128:t0 + (tb + 1) * 128, :], ['xres'], [x])
                        p_ = pp[pi % 4]
                        pi += 1
                        s.mm(p_[:, :], [(mT[:, kk_, tb * 128:(tb + 1) * 128], w[:, kk_, :]) for kk_ in range(KC)],
                             allm + [w], [p_])
                        s.tt('dve', tmp[:, 0:512], p_[:, :], g2t[:, oq * 512:(oq + 1) * 512], ALU.mult, [p_, g2t], [tmp])
                        s.tt('pool', x[:, oq * 512:(oq + 1) * 512], x[:, oq * 512:(oq + 1) * 512], tmp[:, 0:512], ALU.add,
                             [x, tmp], [x])
                        if oq == D // 512 - 1:
                            s.ld(s.xres[t0 + tb * 128:t0 + (tb + 1) * 128, :], x[:, :], [x], ['xres'])
                xi += n // 128
            P.flush()

    def phase_router(s, kind):
        c, P, l = s.c, s.P, s.l
        KC, D, NE = c.KC, c.D, c.NE
        n_tok = c.SEQ if kind == 'l' else c.CTX
        cap = c.CAPL if kind == 'l' else c.CAPC
        s.cap = cap
        with ExitStack() as es:
            wr = s.sb(es, 'rt_w', (128, KC, NE))
            s.ld(wr[:, :, :], s.I['w_router'][l * D:(l + 1) * D, :].rearrange("(k p) e -> p k e", p=128), (), [wr])
            xt = [s.sb(es, 'rt_x%d' % i, (128, D)) for i in range(2)]
            junk = s.sb(es, 'rt_j', (128, D))
            ss = [s.sb(es, 'rt_s%d' % i, (128, 2)) for i in range(2)]
            hT = [s.sb(es, 'rt_h%d' % i, (128, KC, 128)) for i in range(2)]
            tmp = [s.sb(es, 'rt_t%d' % i, (128, 4, 128)) for i in range(2)]
            lg = [s.sb(es, 'rt_lg%d' % i, (128, NE)) for i in range(2)]
            sm = [s.sb(es, 'rt_sm%d' % i, (128, 2)) for i in range(2)]
            affT = [s.sb(es, 'rt_aff%d' % b, (NE, n_tok)) for b in range(c.NS)]
            work = [s.sb(es, 'rt_wk%d' % b, (NE, n_tok)) for b in range(c.NS)]
            pt = [s.ps(es, 'rt_p%d' % i, (128, 4, 128)) for i in range(2)]
            pl = [s.ps(es, 'rt_pl%d' % i, (128, NE)) for i in range(2)]
            pa = [s.ps(es, 'rt_pa%d' % i, (NE, 128)) for i in range(2)]
            it = 0
            pi = 0
            for (kd, b, t0s, ln, mc) in c.seqs:
                if kd != kind:
                    continue
                for t0 in range(t0s, t0s + ln, 128):
                    k = it % 2
                    it += 1
                    x, sq, h = xt[k], ss[k], hT[k]
                    s.ld(x[:, :], s.xres[t0:t0 + 128, :], ['xres'], [x])
                    s.act(junk[:, :], x[:, :], AF.Square, [x], [junk, sq], accum=sq[:, 0:1])
                    s.rsqrt(sq[:, 1:2], sq[:, 0:1], 1.0 / D, 1e-6, [sq], [sq])
                    s.ts('dve', x[:, :], x[:, :], sq[:, 1:2], None, ALU.mult, None, [x, sq], [x])
                    s.ld(s.xn[t0:t0 + 128, :], x[:, :], [x], [('xn', t0)])
                    for q4 in range(KC // 4):
                        p_ = pt[pi % 2]
                        tm = tmp[pi % 2]
                        pi += 1
                        s.tr([(p_[:, j, :], x[:, (q4 * 4 + j) * 128:(q4 * 4 + j + 1) * 128]) for j in range(4)],
                             s.ident[:, :], [x, s.ident], [p_])
                        gb = s.gcol[:, 1, q4 * 4:q4 * 4 + 4, mc]
                        gb = AP(gb, gb.offset, [gb.ap[0], gb.ap[1], [0, 128]])
                        sb_ = s.modcol[:, 3 * KC + q4 * 4:3 * KC + q4 * 4 + 4, mc]
                        sb_ = AP(sb_, sb_.offset, [sb_.ap[0], sb_.ap[1], [0, 128]])
                        s.tt('dve', tm[:, :, :], p_[:, :, :], gb, ALU.mult, [p_, s.gcol], [tm])
                        s.tt('pool', h[:, q4 * 4:q4 * 4 + 4, :], tm[:, :, :], sb_, ALU.add, [tm, s.modcol], [h])
                    pl_ = pl[k]
                    s.mm(pl_[:, :], [(h[:, kk_, :], wr[:, kk_, :]) for kk_ in range(KC)], [h, wr], [pl_])
                    L_, sm_ = lg[k], sm[k]
                    s.P.op('dve', lambda g_, sm_=sm_, pl_=pl_: g_.tensor_reduce(out=sm_[:, 0:1], in_=pl_[:, :], axis=AX.X,
                                                                              op=ALU.max, negate=True), [pl_], [sm_])
                    s.act(L_[:, :], pl_[:, :], AF.Exp, [pl_, sm_], [L_, sm_], bias=sm_[:, 0:1], accum=sm_[:, 1:2])
                    s.P.op('dve', lambda g_, sm_=sm_: g_.reciprocal(out=sm_[:, 1:2], in_=sm_[:, 1:2]), [sm_], [sm_])
                    s.ts('dve', L_[:, :], L_[:, :], sm_[:, 1:2], None, ALU.mult, None, [L_, sm_], [L_])
                    pa_ = pa[k]
                    s.tr([(pa_[:, :], L_[:, :])], s.ident[:, :], [L_, s.ident], [pa_])
                    s.cp('act', affT[b][:, t0 - t0s:t0 - t0s + 128], pa_[:, :], [pa_], [(affT[b], t0)])
            P.flush()
            s.topk = es
            gk = [s.sb(es, 'rt_gk%d' % b, (NE, cap)) for b in range(c.NS)]
            ik = [s.sb(es, 'rt_ik%d' % b, (NE, cap), U32) for b in range(c.NS)]
            ikf = [s.sb(es, 'rt_ikf%d' % b, (NE, cap)) for b in range(c.NS)]
            for b in range(c.NS):
                cur = affT[b]
                for r in range(cap // 8):
                    g8 = gk[b][:, r * 8:(r + 1) * 8]
                    s.P.op('dve', (lambda g8, cur: lambda g_: g_.max(out=g8, in_=cur[:, :]))(g8, cur), [cur], [gk[b]])
                    s.P.op('dve', (lambda g8, cur, b, r: lambda g_: g_.max_index(out=ik[b][:, r * 8:(r + 1) * 8], in_max=g8,
                                                                           in_values=cur[:, :]))(g8, cur, b, r),
                           [cur, gk[b]], [ik[b]])
                    if r < cap // 8 - 1:
                        s.P.op('dve', (lambda g8, cur, b: lambda g_: g_.match_replace(
                            out=work[b][:, :], in_to_replace=g8, in_values=cur[:, :], imm_value=-1.0))(g8, cur, b),
                            [cur, gk[b]], [work[b]])
                        cur = work[b]
                s.cp('dve', ikf[b][:, :], ik[b][:, :], [ik[b]], [ikf[b]])
                t0s_b = [q for q in c.seqs if q[0] == kind and q[1] == b][0][2]
                s.ts('dve', ikf[b][:, :], ikf[b][:, :], float(t0s_b), None, ALU.add, None, [ikf[b]], [ikf[b]])
            nck = (cap + 127) // 128
            s.gsel = s.gselk[kind]
            s.isel = s.iselk[kind]
            ptk = [s.ps(es, 'rt_ptk%d' % i, (128, NE)) for i in range(2)]
            tg = [s.sb(es, 'rt_tg%d' % i, (128, NE)) for i in range(2)]
            ti = [s.sb(es, 'rt_ti%d' % i, (128, NE), I32) for i in range(2)]
            j = 0
            for b in range(c.NS):
                for ck in range(nck):
                    n = min(128, cap - ck * 128)
                    p_ = ptk[j % 2]
                    s.tr([(p_[0:n, :], gk[b][:, ck * 128:ck * 128 + n])], s.ident[0:NE, 0:NE], [gk[b], s.ident], [p_])
                    s.cp('dve', tg[j % 2][0:n, :], p_[0:n, :], [p_], [tg[j % 2]])
                    s.ld(s.gsel[b, ck * 128:ck * 128 + n, :], tg[j % 2][0:n, :], [tg[j % 2]], [('gsel', b, ck)])
                    j += 1
                    p_ = ptk[j % 2]
                    s.tr([(p_[0:n, :], ikf[b][:, ck * 128:ck * 128 + n])], s.ident[0:NE, 0:NE], [ikf[b], s.ident], [p_])
                    s.cp('dve', ti[j % 2][0:n, :], p_[0:n, :], [p_], [ti[j % 2]])
                    s.ld(s.isel[b, ck * 128:ck * 128 + n, :], ti[j % 2][0:n, :], [ti[j % 2]], [('isel', b, ck)])
                    j += 1
            P.flush()

    def phase_moe(s, kind):
        c, P, l = s.c, s.P, s.l
        KC, D, NE, FF = c.KC, c.D, c.NE, c.FF
        FC = FF // 128
        cap = s.cap
        NS1 = c.NS + 1
        nck = (cap + 127) // 128
        cw = min(cap, 128)
        NTOK = c.NS * cap
        M6 = 6 * D
        seqs = [q for q in c.seqs if q[0] == kind]
        with ExitStack() as es:
            gsel = s.sb(es, 'mo_g', (128, c.NS, nck, NE))
            isel = s.sb(es, 'mo_i', (128, c.NS, nck, NE), I32)
            for b in range(c.NS):
                for ck in range(nck):
                    n = min(128, cap - ck * 128)
                    s.ld(gsel[0:n, b, ck, :], s.gsel[b, ck * 128:ck * 128 + n, :], ['gsel_all'], [gsel])
                    s.ld(isel[0:n, b, ck, :], s.isel[b, ck * 128:ck * 128 + n, :], ['isel_all'], [isel])
            g5 = []
            for (kd, b, t0s, ln, mc) in seqs:
                t = s.sb(es, 'mo_g5%d' % b, (128, D))
                s.ld(t[:, :], AP(s.modrow, (l * NS1 + mc) * M6 + 5 * D, [[0, 128], [1, D]]), ['modrow_all'], [t])
                g5.append(t)
            xs = [s.sb(es, 'mo_xs%d' % i, (128, D)) for i in range(2)]
            xsT = [s.sb(es, 'mo_xT%d' % i, (128, KC, NTOK), BF16) for i in range(2)]
            hid = s.sb(es, 'mo_hid', (128, FC, NTOK), BF16)
            sg = [s.sb(es, 'mo_sg%d' % i, (128, NTOK)) for i in range(2)]
            ys = [s.sb(es, 'mo_ys%d' % i, (128, D)) for i in range(c.NS * nck)]
            tmp = [s.sb(es, 'mo_t%d' % i, (128, 4, 128)) for i in range(2)]
            wb = [s.sb(es, 'mo_w%d' % i, (128, max(KC, FC), 512), BF16) for i in range(3)]
            pt = [s.ps(es, 'mo_pt%d' % i, (128, 4, 128)) for i in range(2)]
            pg = [s.ps(es, 'mo_pg%d' % i, (128, NTOK)) for i in range(2)]
            pu = [s.ps(es, 'mo_pu%d' % i, (128, NTOK)) for i in range(2)]
            pd = [s.ps(es, 'mo_pd%d' % i, (128, 512)) for i in range(2)]
            cnt = {'x': 0, 'p': 0, 'w': 0, 'h': 0, 'y': 0, 'd': 0}
            for e_ in range(NE):
                xT = xsT[e_ % 2]
                for bi, (kd, b, t0s, ln, mc) in enumerate(seqs):
                    for ck in range(nck):
                        n = min(128, cap - ck * 128)
                        x = xs[cnt['x'] % 2]
                        cnt['x'] += 1
                        s.P.dma('pool', (lambda x, n, b, ck, e_, t0s, ln: lambda g_: g_.indirect_dma_start(
                            out=x[0:n, :], out_offset=None, in_=s.xn[:, :],
                            in_offset=bass.IndirectOffsetOnAxis(ap=isel[0:n, b, ck, e_:e_ + 1], axis=0)))(
                            x, n, b, ck, e_, t0s, ln), ['xn_all', isel], [x])
                        c0 = bi * cap + ck * 128
                        for q4 in range(KC // 4):
                            p_ = pt[cnt['p'] % 2]
                            tm = tmp[cnt['p'] % 2]
                            cnt['p'] += 1
                            s.tr([(p_[:, j, 0:n], x[0:n, (q4 * 4 + j) * 128:(q4 * 4 + j + 1) * 128]) for j in range(4)],
                                 s.ident[0:n, 0:n], [x, s.ident], [p_])
                            gb = s.gcol[:, 1, q4 * 4:q4 * 4 + 4, mc]
                            gb = AP(gb, gb.offset, [gb.ap[0], gb.ap[1], [0, n]])
                            sb_ = s.modcol[:, 3 * KC + q4 * 4:3 * KC + q4 * 4 + 4, mc]
                            sb_ = AP(sb_, sb_.offset, [sb_.ap[0], sb_.ap[1], [0, n]])
                            s.tt('dve', tm[:, :, 0:n], p_[:, :, 0:n], gb, ALU.mult, [p_, s.gcol], [tm])
                            s.tt('pool', xT[:, q4 * 4:q4 * 4 + 4, c0:c0 + n], tm[:, :, 0:n], sb_, ALU.add,
                                 [tm, s.modcol], [xT])
                wg = s.W['w_exp_gate'][(l * NE + e_) * D:(l * NE + e_ + 1) * D, :].rearrange("(k p) f -> p k f", p=128)
                wu = s.W['w_exp_up'][(l * NE + e_) * D:(l * NE + e_ + 1) * D, :].rearrange("(k p) f -> p k f", p=128)
                wd = s.W['w_exp_down'][(l * NE + e_) * FF:(l * NE + e_ + 1) * FF, :].rearrange("(k p) d -> p k d", p=128)
                for fq in range(FF // 512):
                    w1 = wb[cnt['w'] % 3]
                    cnt['w'] += 1
                    w2 = wb[cnt['w'] % 3]
                    cnt['w'] += 1
                    s.ld(w1[:, 0:KC, :], wg[:, :, fq * 512:(fq + 1) * 512], ['wfull_e'], [w1])
                    s.ld(w2[:, 0:KC, :], wu[:, :, fq * 512:(fq + 1) * 512], ['wfull_e'], [w2])
                    for j in range(4):
                        pg_ = pg[cnt['h'] % 2]
                        pu_ = pu[cnt['h'] % 2]
                        sg_ = sg[cnt['h'] % 2]
                        cnt['h'] += 1
                        s.mm(pg_[:, :], [(w1[:, kk_, j * 128:(j + 1) * 128], xT[:, kk_, :]) for kk_ in range(KC)], [w1, xT], [pg_])
                        s.mm(pu_[:, :], [(w2[:, kk_, j * 128:(j + 1) * 128], xT[:, kk_, :]) for kk_ in range(KC)], [w2, xT], [pu_])
                        s.act(sg_[:, :], pg_[:, :], AF.Silu, [pg_], [sg_])
                        s.tt('dve', hid[:, fq * 4 + j, :], sg_[:, :], pu_[:, :], ALU.mult, [sg_, pu_], [hid])
                tbs = []
                for bi, (kd, b, t0s, ln, mc) in enumerate(seqs):
                    for ck in range(nck):
                        tbs.append((bi, b, ck, min(128, cap - ck * 128), bi * cap + ck * 128))
                for dq in range(D // 512):
                    w = wb[cnt['w'] % 3]
                    cnt['w'] += 1
                    s.ld(w[:, 0:FC, :], wd[:, :, dq * 512:(dq + 1) * 512], ['wfull_e'], [w])
                    for ti_, (bi, b, ck, n, c0) in enumerate(tbs):
                        y = ys[ti_]
                        p_ = pd[cnt['d'] % 2]
                        cnt['d'] += 1
                        s.mm(p_[0:n, :], [(hid[:, kk_, c0:c0 + n], w[:, kk_, :]) for kk_ in range(FC)], [hid, w], [p_])
                        s.stt('dve', y[0:n, dq * 512:(dq + 1) * 512], p_[0:n, :], gsel[0:n, b, ck, e_:e_ + 1],
                              g5[bi][0:n, dq * 512:(dq + 1) * 512], ALU.mult, ALU.mult, [p_, gsel, g5[bi]], [y])
                for ti_, (bi, b, ck, n, c0) in enumerate(tbs):
                    y = ys[ti_]
                    s.P.dma('pool', (lambda y, n, b, ck, e_: lambda g_: g_.indirect_dma_start(
                        out=s.xres[:, :],
                        out_offset=bass.IndirectOffsetOnAxis(ap=isel[0:n, b, ck, e_:e_ + 1], axis=0),
                        in_=y[0:n, :], in_offset=None, compute_op=ALU.add))(y, n, b, ck, e_),
                        [y, isel], ['xres'])
            P.flush()

    def phase_final(s):
        c, P = s.c, s.P
        D = c.D
        with ExitStack() as es:
            nf = s.sb(es, 'fn_w', (128, D))
            s.ld(nf[:, :], AP(s.I['norm_final'], 0, [[0, 128], [1, D]]), (), [nf])
            xt = [s.sb(es, 'fn_x%d' % i, (128, D)) for i in range(3)]
            junk = s.sb(es, 'fn_j', (128, D))
            ss = [s.sb(es, 'fn_s%d' % i, (128, 2)) for i in range(3)]
            it = 0
            for (kind, b, t0s, ln, mc) in c.seqs:
                if kind != 'l':
                    continue
                for t0 in range(t0s, t0s + ln, 128):
                    x, sq = xt[it % 3], ss[it % 3]
                    it += 1
                    s.ld(x[:, :], s.xres[t0:t0 + 128, :], ['xres'], [x])
                    s.act(junk[:, :], x[:, :], AF.Square, [x], [junk, sq], accum=sq[:, 0:1])
                    s.rsqrt(sq[:, 1:2], sq[:, 0:1], 1.0 / D, 1e-6, [sq], [sq])
                    s.stt('dve', x[:, :], x[:, :], sq[:, 1:2], nf[:, :], ALU.mult, ALU.mult, [x, sq, nf], [x])
                    o0 = t0 - c.NS * c.CTX
                    s.ld(s.out[o0:o0 + 128, :], x[:, :], [x], [('out', o0)])
            P.flush()


def make_in_maps(c, inputs):
    bs = big_shapes(c)
    ss = small_shapes(c)
    consts = host_consts(c)
    maps = []
    big = {n: np.ascontiguousarray(np.asarray(inputs[n], np.float32)).reshape(bs[n]) for n in BIGW}
    small = {}
    for n, shp in ss.items():
        a = np.asarray(inputs[n], np.float32)
        if shp[0] == 0:
            a = np.zeros((1, shp[1]), np.float32)
        small[n] = np.ascontiguousarray(a.reshape(max(shp[0], 1), shp[1]))
    x = np.asarray(inputs['x'], np.float32)
    ctx = np.asarray(inputs['ctx'], np.float32)
    cc = np.asarray(inputs['c'], np.float32)
    for i in range(c.NCORES):
        m = {}
        m['x'] = np.ascontiguousarray(x[i * c.NS:(i + 1) * c.NS]).reshape(c.NS * c.SEQ, c.D)
        m['ctx'] = np.ascontiguousarray(ctx[i * c.NS:(i + 1) * c.NS]).reshape(c.NS * c.CTX, c.D)
        m['c'] = np.ascontiguousarray(cc[i * c.NS:(i + 1) * c.NS])
        for n in BIGW:
            r = bs[n][0] // c.NCORES
            m[n] = big[n][i * r:(i + 1) * r] if c.GATHER else big[n]
        m.update(small)
        m.update(consts)
        maps.append(m)
    return maps


def run(c, inputs, debug_out=None, stop=None):
    b = Builder(c, debug_out, stop)
    nc = b.build()
    maps = make_in_maps(c, inputs)
    res = run_bass_kernel_spmd(nc, maps, core_ids=list(range(c.NCORES)))
    return res


def kernel(**inputs):
    c = Cfg(GATHER=False)
    res = run(c, inputs)
    out = np.stack([r['y'] for r in res.results]).reshape(c.BATCH, c.SEQ, c.D)
    return out.astype(np.float32)
```

```python
import numpy as np
from contextlib import ExitStack
import concourse.bass as bass
import concourse.mybir as mybir
from concourse.bass_utils import run_bass_kernel_spmd

F32 = mybir.dt.float32
BF16 = mybir.dt.bfloat16
U32 = mybir.dt.uint32
I32 = mybir.dt.int32
ALU = mybir.AluOpType
AF = mybir.ActivationFunctionType
AX = mybir.AxisListType


class Cfg:
    def __init__(s, D=2048, SEQ=2048, CTX=256, GRID_W=64, DEPTH=2, NE=16, FF=2048, NCORES=8, BATCH=16, GATHER=True,
                 IPG=1024, IPS=512):
        s.IPG, s.IPS = IPG, IPS
        s.GATHER = GATHER
        s.D, s.SEQ, s.CTX, s.GRID_W, s.DEPTH, s.NE, s.FF = D, SEQ, CTX, GRID_W, DEPTH, NE, FF
        s.NCORES, s.BATCH = NCORES, BATCH
        s.NS = BATCH // NCORES
        s.BW = D // 2
        s.H = s.BW // 64
        s.KVH = s.H // 4
        s.DR, s.IR, s.VR, s.GR = 96, 96, 64, 256
        s.RC = 3 * s.BW + 2 * s.DR + 2 * s.IR + s.GR
        s.KVW = s.KVH * 64
        s.CTXC = s.RC + 2 * s.KVW
        s.QEND = s.CTXC + s.BW
        s.CONVEND = s.QEND + 3 * s.BW
        s.INC = s.CONVEND + 3 * D
        s.NT = s.NS * (s.CTX + s.SEQ)
        s.TALL = s.CTX + s.SEQ
        s.KC = D // 128
        s.BC = s.BW // 128
        s.CAPL = 2 * s.SEQ // NE
        s.CAPC = 2 * s.CTX // NE
        s.IQ = 128 // (s.NS * s.H)
        s.IP = 64 // s.IQ
        s.seqs = [('c', b, b * s.CTX, s.CTX, s.NS) for b in range(s.NS)] + \
                 [('l', b, s.NS * s.CTX + b * s.SEQ, s.SEQ, b) for b in range(s.NS)]


BIGW = ['w_mod', 'w_in', 'w_branch', 'w_out', 'w_exp_gate', 'w_exp_up', 'w_exp_down']
SMALLW = ['b_mod', 'norm_mix', 'norm_ffn', 'shift_mu', 'decay_up', 'decay_bias', 'iclr_up', 'iclr_bias',
          'gate_up', 'vres_down', 'vres_up', 'vres_bias', 'k_k', 'k_a', 'r_k', 'gn_w', 'gn_b', 'conv_w',
          'attn_sink', 'w_router', 'norm_final', 'c_ctx']


def big_shapes(c):
    L = c.DEPTH
    return {'w_mod': (L * c.D, 6 * c.D), 'w_in': (L * c.D, c.INC), 'w_branch': (L * 3 * c.BW, c.D),
            'w_out': (L * c.D, c.D), 'w_exp_gate': (L * c.NE * c.D, c.FF), 'w_exp_up': (L * c.NE * c.D, c.FF),
            'w_exp_down': (L * c.NE * c.FF, c.D)}


def small_shapes(c):
    L = c.DEPTH
    return {'b_mod': (L, 6 * c.D), 'norm_mix': (L, c.D), 'norm_ffn': (L, c.D), 'shift_mu': (L, c.RC),
            'decay_up': (L * 2 * c.DR, c.BW), 'decay_bias': (L * 2, c.BW), 'iclr_up': (L * 2 * c.IR, c.BW),
            'iclr_bias': (L * 2, c.BW), 'gate_up': (L * c.GR, c.BW), 'vres_down': ((L - 1) * c.BW, c.VR),
            'vres_up': ((L - 1) * c.VR, c.BW), 'vres_bias': (L - 1, c.BW), 'k_k': (L, c.BW), 'k_a': (L, c.BW),
            'r_k': (L, c.BW), 'gn_w': (L, c.BW), 'gn_b': (L, c.BW), 'conv_w': (L * 3, c.BW),
            'attn_sink': (L, c.H), 'w_router': (L * c.D, c.NE), 'norm_final': (1, c.D), 'c_ctx': (1, c.D)}


def host_consts(c):
    ident = np.eye(128, dtype=np.float32)
    blk = np.zeros((128, 128), np.float32)
    blk[:64, :64] = 1
    blk[64:, 64:] = 1
    swp = np.zeros((64, 64), np.float32)
    for m in range(64):
        q = m % 32
        swp[m + 16 if q < 16 else m - 16, m] = 1
    k = np.arange(128)[:, None]
    q = np.arange(128)[None, :]
    masks = np.stack([(k >= q), (k <= q)]).astype(np.float32)
    t = np.arange(c.SEQ)
    rows = (t // c.GRID_W).astype(np.float32)
    cols = (t % c.GRID_W).astype(np.float32)
    inv = (10000.0 ** (-np.arange(16, dtype=np.float32) / 16)).astype(np.float32)
    cs = np.zeros((2, 64, c.SEQ), np.float32)
    for n in range(64):
        pos = rows if n < 32 else cols
        m = n % 32
        ang = (pos * inv[m % 16]).astype(np.float32)
        cs[0, n] = np.cos(ang)
        cs[1, n] = -np.sin(ang) if m < 16 else np.sin(ang)
    return {'k_ident': ident, 'k_blk': blk, 'k_swp': swp, 'k_masks': masks.reshape(256, 128),
            'k_rope': cs.reshape(128, c.SEQ)}


class Prog:
    ENG = ('sp', 'act', 'dve', 'pool', 'pe')
    NDS = 8

    def __init__(s, nc):
        s.nc = nc
        s.sems = {}
        s.esem = {e: s._sem('e_' + e) for e in s.ENG}
        s.ecnt = {e: 0 for e in s.ENG}
        s.dsem = {e: [s._sem('d_%s%d' % (e, i)) for i in range(s.NDS)] for e in ('sp', 'act', 'pool')}
        s.dcnt = {e: 0 for e in ('sp', 'act', 'pool')}
        s.waited = {e: {} for e in s.ENG}
        s.last = {}
        s.res = {}
        s.q = {e: [] for e in s.ENG}

    def _sem(s, name):
        s.sems[name] = s.nc.alloc_semaphore(name=name)
        return name

    @staticmethod
    def _key(r):
        if isinstance(r, tuple):
            return (Prog._key(r[0]),) + tuple(r[1:])
        if isinstance(r, str):
            return r
        return id(r)

    def _waits(s, e, reads, writes, extra=()):
        toks = list(extra)
        for r in reads:
            st = s.res.get(s._key(r))
            if st and st['w']:
                toks.append(st['w'])
        for w in writes:
            st = s.res.get(s._key(w))
            if st:
                if st['w']:
                    toks.append(st['w'])
                toks.extend(st['r'].items())
        out = {}
        for sk, v in toks:
            if e == 'pe' and sk == s.esem['pe']:
                continue
            if s.waited[e].get(sk, 0) < v:
                out[sk] = max(out.get(sk, 0), v)
        for sk, v in out.items():
            s.waited[e][sk] = v
        return list(out.items())

    def _commit(s, tok, reads, writes):
        s.last[tok[0]] = tok[1]
        wk = [s._key(w) for w in writes]
        for k in wk:
            s.res[k] = {'w': tok, 'r': {}}
        for r in reads:
            k = s._key(r)
            if k in wk:
                continue
            st = s.res.setdefault(k, {'w': None, 'r': {}})
            st['r'][tok[0]] = max(st['r'].get(tok[0], 0), tok[1])

    def op(s, e, fn, reads=(), writes=()):
        waits = s._waits(e, reads, writes)
        s.ecnt[e] += 1
        tok = (s.esem[e], s.ecnt[e])
        s.q[e].append((waits, fn, tok[0], 1))
        s._commit(tok, reads, writes)

    def dma(s, e, fn, reads=(), writes=(), inc=16):
        n = s.dcnt[e]
        s.dcnt[e] += 1
        slot = s.dsem[e][n % s.NDS]
        prev = s.last.get(slot, 0)
        waits = s._waits(e, reads, writes, extra=[(slot, prev)] if prev else [])
        tok = (slot, prev + inc)
        s.q[e].append((waits, fn, slot, inc))
        s._commit(tok, reads, writes)

    MAGIC = 1000

    def prologue(s):
        nc = s.nc
        s.gate = {e: nc.alloc_semaphore(name='gate_' + e) for e in s.ENG}
        s.done = nc.alloc_semaphore(name='done')
        gate, sems, done, MAGIC = s.gate, s.sems, s.done, s.MAGIC
        with nc.Block() as block:
            def mk(e):
                def f(eng):
                    if e == 'pool':
                        for h in sems.values():
                            eng.sem_clear(h)
                        eng.sem_clear(done)
                        for g in gate.values():
                            eng.sem_clear(g)
                        for g in gate.values():
                            eng.sem_inc(g, MAGIC)
                    eng.wait_op(gate[e], MAGIC, 'sem-eq')
                    eng.sem_inc(gate[e], 1)
                return f
            block.sync(mk('sp'))
            block.scalar(mk('act'))
            block.vector(mk('dve'))
            block.gpsimd(mk('pool'))
            block.tensor(mk('pe'))

    def epilogue(s):
        nc = s.nc
        gate, sems, done = s.gate, s.sems, s.done
        with nc.Block() as block:
            def mk(e):
                def f(eng):
                    if e == 'pool':
                        eng.wait_ge(done, 4)
                        for h in sems.values():
                            eng.sem_clear(h)
                        for g in gate.values():
                            eng.sem_clear(g)
                        eng.sem_clear(done)
                    else:
                        eng.sem_inc(done, 1)
                return f
            block.sync(mk('sp'))
            block.scalar(mk('act'))
            block.vector(mk('dve'))
            block.gpsimd(mk('pool'))
            block.tensor(mk('pe'))

    def flush(s):
        nc = s.nc
        for e in s.ENG:
            waits = []
            for sk, v in s.last.items():
                if s.waited[e].get(sk, 0) < v:
                    s.waited[e][sk] = v
                    waits.append((sk, v))
            s.q[e].append((waits, None, None, 0))
        q = s.q
        sems = s.sems
        with nc.Block() as block:
            def mk(e):
                def f(eng):
                    for waits, fn, sem, inc in q[e]:
                        for sk, v in waits:
                            eng.wait_ge(sems[sk], v)
                        if fn is not None:
                            ins = fn(eng)
                            ins.then_inc(sems[sem], inc)
                return f
            block.sync(mk('sp'))
            block.scalar(mk('act'))
            block.vector(mk('dve'))
            block.gpsimd(mk('pool'))
            block.tensor(mk('pe'))
        s.q = {e: [] for e in s.ENG}
        s.res = {}


def AP(t, off, dims):
    return bass.AP(t.tensor, off, [list(d) for d in dims])


class Builder:
    def __init__(s, c, debug_out=None, stop=None):
        s.c = c
        s.stop = stop
        s.uid = 0
        s.nc = bass.Bass("TRN2", target_bir_lowering=False)
        s.P = Prog(s.nc)
        s.debug_out = debug_out or []

    def dram(s, name, shape, dt=F32, kind="Internal"):
        if kind == "Internal":
            return s.nc.dram_tensor(name, list(shape), dt).ap()
        return s.nc.dram_tensor(name, list(shape), dt, kind=kind).ap()

    def sb(s, es, name, shape, dt=F32):
        s.uid += 1
        return es.enter_context(s.nc.sbuf_tensor('%s_%d' % (name, s.uid), list(shape), dt))

    def ps(s, es, name, shape, dt=F32):
        s.uid += 1
        return es.enter_context(s.nc.psum_tensor('%s_%d' % (name, s.uid), list(shape), dt))

    def ld(s, out, in_, reads, writes, q='sp'):
        s.P.dma(q, lambda e: e.dma_start(out=out, in_=in_), reads, writes)

    def ldnc(s, out, in_, reads, writes, q='sp'):
        s.P.dma(q, lambda e: e.dma_start(out=out, in_=in_, allow_slow_non_contiguous=True), reads, writes)

    def tt(s, e, out, a, b, op, reads, writes):
        s.P.op(e, lambda g: g.tensor_tensor(out=out, in0=a, in1=b, op=op), reads, writes)

    def ts(s, e, out, a, s1, s2, op0, op1, reads, writes):
        if s2 is None:
            s.P.op(e, lambda g: g.tensor_scalar(out=out, in0=a, scalar1=s1, scalar2=None, op0=op0), reads, writes)
        else:
            s.P.op(e, lambda g: g.tensor_scalar(out=out, in0=a, scalar1=s1, scalar2=s2, op0=op0, op1=op1),
                   reads, writes)

    def stt(s, e, out, a, sc, b, op0, op1, reads, writes):
        s.P.op(e, lambda g: g.scalar_tensor_tensor(out=out, in0=a, scalar=sc, in1=b, op0=op0, op1=op1),
               reads, writes)

    def act(s, out, in_, func, reads, writes, bias=None, scale=None, accum=None):
        kw = {}
        if bias is not None:
            kw['bias'] = bias
        if scale is not None:
            kw['scale'] = scale
        if accum is not None:
            kw['accum_out'] = accum
        s.P.op('act', lambda g: g.activation(out=out, in_=in_, func=func, **kw), reads, writes)

    def rsqrt(s, out, in_, mult, add, reads, writes):
        s.act(out, in_, AF.Sqrt, reads, writes, bias=s.cbias(add), scale=mult)
        s.P.op('dve', lambda g: g.reciprocal(out=out, in_=out), writes, writes)

    def cbias(s, val):
        key = float(val)
        if key not in s.cb:
            i = len(s.cb)
            s.cb[key] = i
            s.P.op('pool', (lambda i, key: lambda g: g.memset(s.cbt[:, i:i + 1], key))(i, key), (), [(s.cbt, i)])
        i = s.cb[key]
        return s.cbt[:, i:i + 1]

    def cp(s, e, out, in_, reads, writes):
        if e == 'act':
            s.act(out, in_, AF.Copy, reads, writes)
        else:
            s.P.op(e, lambda g: g.tensor_copy(out=out, in_=in_), reads, writes)

    def red(s, out, in_, reads, writes, negate=False):
        s.P.op('dve', lambda g: g.tensor_reduce(out=out, in_=in_, axis=AX.X, op=ALU.add, negate=negate),
               reads, writes)

    def mm(s, out, pairs, reads, writes, start=True, stop=True):
        def fn(g):
            ins = None
            n = len(pairs)
            for i, (l, r) in enumerate(pairs):
                ins = g.matmul(out, l, r, start=(start and i == 0), stop=(stop and i == n - 1))
            return ins
        s.P.op('pe', fn, reads, writes)

    def tr(s, outs_ins, ident, reads, writes):
        def fn(g):
            ins = None
            for o, i in outs_ins:
                ins = g.transpose(o, i, ident)
            return ins
        s.P.op('pe', fn, reads, writes)

    def memset(s, e, ap, val, writes):
        s.P.op(e, lambda g: g.memset(ap, val), (), writes)

    def build(s):
        c, nc, P = s.c, s.nc, s.P
        L = c.DEPTH
        s.I = {}
        s.I['x'] = s.dram('x', (c.NS * c.SEQ, c.D), kind="ExternalInput")
        s.I['ctx'] = s.dram('ctx', (c.NS * c.CTX, c.D), kind="ExternalInput")
        s.I['c'] = s.dram('c', (c.NS, c.D), kind="ExternalInput")
        bs = big_shapes(c)
        for n in BIGW:
            r, w = bs[n]
            s.I[n + '_sh'] = s.dram(n, (r // c.NCORES if c.GATHER else r, w), kind="ExternalInput")
        for n, shp in small_shapes(c).items():
            s.I[n] = s.dram(n, (max(shp[0], 1), shp[1]), kind="ExternalInput")
        for n, a in host_consts(c).items():
            s.I[n] = s.dram(n, a.shape, kind="ExternalInput")
        s.out = s.dram('y', (c.NS * c.SEQ, c.D), kind="ExternalOutput")
        s.W = {}
        s.Wsh = {}
        for n in BIGW:
            r, w = bs[n]
            s.Wsh[n] = s.dram(n + '_b16s', (r // c.NCORES, w), BF16)
            s.W[n] = s.dram(n + '_b16', (r, w), BF16)
        s.xres = s.dram('xres', (c.NT, c.D))
        s.hT = s.dram('hT', (c.D, c.NT), BF16)
        s.pT = s.dram('pT', (c.INC, c.NT))
        s.strm = {n: s.dram('st_' + n, (c.NS, c.H, c.TALL, 64)) for n in
                  ['w0', 'w1', 'kd0', 'kd1', 'ka0', 'ka1', 'nkk', 'r', 'v']}
        s.ysc = [s.dram('ysc%d' % d, (c.NS, c.H, c.TALL, 64)) for d in range(2)]
        s.sgdT = s.dram('sgdT', (c.GR, c.NT))
        s.vfT = s.dram('vfT', (c.BW, c.NT))
        s.brT = [s.dram('brT%d' % i, (c.BW, c.NT), BF16) for i in range(3)]
        s.xn = s.dram('xn', (c.NT, c.D))
        s.modrow = s.dram('modrow', (L * (c.NS + 1), 6 * c.D))
        s.gselk = {'l': s.dram('gsel_l', (c.NS, c.CAPL, c.NE)), 'c': s.dram('gsel_c', (c.NS, c.CAPC, c.NE))}
        s.iselk = {'l': s.dram('isel_l', (c.NS, c.CAPL, c.NE), I32), 'c': s.dram('isel_c', (c.NS, c.CAPC, c.NE), I32)}

        with ExitStack() as g:
            g.enter_context(nc.allow_low_precision("bf16 matmul operands, fp32 accumulation"))
            s.ident = s.sb(g, 'ident', (128, 128))
            s.blk = s.sb(g, 'blk', (128, 128))
            s.swp = s.sb(g, 'swp', (64, 64))
            s.masks = s.sb(g, 'masks', (128, 2, 128), BF16)
            s.masks_f = s.sb(g, 'masks_f', (128, 2, 128))
            s.identb = s.sb(g, 'identb', (128, 128), BF16)
            s.onesb = s.sb(g, 'onesb', (128, 64), BF16)
            s.modcol = s.sb(g, 'modcol', (128, 6 * c.KC, c.NS + 1))
            s.ncol = s.sb(g, 'ncol', (128, 2, c.KC))
            s.gcol = s.sb(g, 'gcol', (128, 2, c.KC, c.NS + 1))
            s.cbt = s.sb(g, 'cbt', (128, 8))
            s.cb = {}
            s.dbg = {}
            s.scr = {'pT': s.pT, 'hT': s.hT, 'xres': s.xres, 'modrow': s.modrow, 'sgdT': s.sgdT, 'vfT': s.vfT,
                     'xn': s.xn, 'ysc0': s.ysc[0], 'ysc1': s.ysc[1], 'brT0': s.brT[0], 'brT1': s.brT[1],
                     'brT2': s.brT[2]}
            for n_ in s.strm:
                s.scr['st_' + n_] = s.strm[n_]
            for k_ in 'lc':
                s.scr['gsel_' + k_] = s.gselk[k_]
                s.scr['isel_' + k_] = s.iselk[k_]
            for n_ in s.debug_out:
                a_ = s.scr[n_]
                s.dbg[n_] = s.dram('dbg_' + n_, a_.shape, a_.dtype, kind="ExternalOutput")
            s.P.prologue()
            s.phase_init()
            done = False
            for l in range(L):
                s.l = l
                s.last = (l == L - 1)
                phases = [('mod', s.phase_mod), ('norm1', s.phase_norm1), ('inproj', s.phase_inproj),
                          ('rwkv_pre', s.phase_rwkv_pre), ('scan', s.phase_scan), ('rwkv_post', s.phase_rwkv_post),
                          ('attn', s.phase_attn), ('conv', s.phase_conv), ('merge', s.phase_merge)]
                for kind in (['l'] if s.last else ['l', 'c']):
                    phases.append(('router_' + kind, (lambda k: lambda: s.phase_router(k))(kind)))
                    phases.append(('moe_' + kind, (lambda k: lambda: s.phase_moe(k))(kind)))
                for name, fn in phases:
                    fn()
                    if s.stop == (l, name):
                        done = True
                        break
                if done:
                    break
            if not done:
                s.phase_final()
            for n_ in s.dbg:
                s.ld(s.dbg[n_], s.scr[n_], ['dbgsrc'], [('dbg', n_)])
            s.P.flush()
            s.P.epilogue()
        return nc

    def phase_init(s):
        c, P = s.c, s.P
        I = s.I
        s.ld(s.ident[:, :], I['k_ident'][:, :], (), [s.ident])
        s.ld(s.blk[:, :], I['k_blk'][:, :], (), [s.blk])
        s.ld(s.swp[:, :], I['k_swp'][:, :], (), [s.swp])
        s.ld(s.masks_f[:, :, :], I['k_masks'].rearrange("(m k) q -> k m q", m=2), (), [s.masks_f])
        s.cp('dve', s.masks[:, :, :], s.masks_f[:, :, :], [s.masks_f], [s.masks])
        s.cp('dve', s.identb[:, :], s.ident[:, :], [s.ident], [s.identb])
        s.memset('dve', s.onesb[:, :], 1.0, [s.onesb])
        nct = c.NS * c.CTX
        s.ld(s.xres[0:nct, :], I['ctx'][:, :], (), ['xres'])
        R = c.NS * c.SEQ
        step = max(R // 4, 128)
        for r0 in range(0, R, step):
            s.ld(s.xres[nct + r0:nct + r0 + step, :], I['x'][r0:r0 + step, :], (), [('xres', r0)])
        bs = big_shapes(c)
        for n in BIGW:
            r = bs[n][0] // c.NCORES if c.GATHER else bs[n][0]
            step = max(r // (4 if c.GATHER else 32), 1)
            dstw = s.Wsh[n] if c.GATHER else s.W[n]
            for r0 in range(0, r, step):
                s.P.dma('pool', (lambda dstw, n, r0, step: lambda e: e.dma_start(
                    out=dstw[r0:r0 + step, :], in_=I[n + '_sh'][r0:r0 + step, :]))(dstw, n, r0, step),
                    (), [('wsh', n, r0)])
        P.flush()
        for n in (BIGW if c.GATHER else []):
            s.P.dma('pool', (lambda n: lambda e: e.collective_compute(
                "AllGather", ALU.bypass, replica_groups=[list(range(c.NCORES))],
                ins=[s.Wsh[n].opt()], outs=[s.W[n].opt()]))(n), (), [('wfull', n)], inc=1)
        P.flush()

    def phase_mod(s):
        c, P, l = s.c, s.P, s.l
        NS1 = c.NS + 1
        M6 = 6 * c.D
        with ExitStack() as es:
            cT = s.sb(es, 'cT', (128, c.KC, NS1))
            cTb = s.sb(es, 'cTb', (128, c.KC, NS1), BF16)
            for sc in range(c.NS):
                s.ldnc(cT[:, :, sc], s.I['c'][sc:sc + 1, :].rearrange("s (k p) -> p (s k)", p=128), [cT], [cT])
            s.ldnc(cT[:, :, c.NS], s.I['c_ctx'].rearrange("s (k p) -> p (s k)", p=128), [cT], [cT])
            s.act(cTb[:, :, :], cT[:, :, :], AF.Silu, [cT], [cTb])
            brow = s.sb(es, 'brow', (NS1, M6))
            s.ld(brow[:, :], AP(s.I['b_mod'], l * M6, [[0, NS1], [1, M6]]), (), [brow])
            mrow = s.sb(es, 'mrow', (NS1, M6))
            wb = [s.sb(es, 'wmod%d' % i, (128, c.KC, 512), BF16) for i in range(2)]
            pm = [s.ps(es, 'pmod%d' % i, (NS1, 512)) for i in range(2)]
            wsrc = s.W['w_mod'][l * c.D:(l + 1) * c.D, :].rearrange("(k p) n -> p k n", p=128)
            ng = M6 // 512
            for gi in range(ng):
                w = wb[gi % 2]
                p_ = pm[gi % 2]
                s.ld(w[:, :, :], wsrc[:, :, gi * 512:(gi + 1) * 512], (), [w])
                s.mm(p_[:, :], [(cTb[:, k, :], w[:, k, :]) for k in range(c.KC)], [cTb, w], [p_])
                s.tt('dve', mrow[:, gi * 512:(gi + 1) * 512], p_[:, :], brow[:, gi * 512:(gi + 1) * 512], ALU.add,
                     [p_, brow], [(mrow, gi)])
            allm = [(mrow, gi) for gi in range(ng)]
            s.ld(s.modrow[l * NS1:(l + 1) * NS1, :], mrow[:, :], allm, [('modrow', l)])
            pc = s.ps(es, 'pcol', (128, 6 * c.KC, NS1))
            nch = 6 * c.KC
            s.tr([(pc[:, j, :], mrow[:, j * 128:(j + 1) * 128]) for j in range(nch)], s.ident[0:NS1, 0:NS1],
                 allm + [s.ident], [pc])
            s.cp('dve', s.modcol[:, :, :], pc[:, :, :], [pc], [s.modcol])
            s.ldnc(s.ncol[:, 0, :], s.I['norm_mix'][l:l + 1, :].rearrange("o (k p) -> p (o k)", p=128), (), [s.ncol])
            s.ldnc(s.ncol[:, 1, :], s.I['norm_ffn'][l:l + 1, :].rearrange("o (k p) -> p (o k)", p=128), [s.ncol],
                   [s.ncol])
            KC = c.KC
            for j, mi in ((0, 1), (1, 4)):
                for sc in range(NS1):
                    s.stt('dve', s.gcol[:, j, :, sc], s.modcol[:, mi * KC:(mi + 1) * KC, sc], 1.0, s.ncol[:, j, :],
                          ALU.add, ALU.mult, [s.modcol, s.ncol], [s.gcol])
            P.flush()

    def norm_tile(s, es_t, x_ap, tag):
        pass

    def phase_norm1(s):
        c, P, l = s.c, s.P, s.l
        KC = c.KC
        with ExitStack() as es:
            xt = [s.sb(es, 'n1x%d' % i, (128, c.D)) for i in range(2)]
            junk = s.sb(es, 'n1j', (128, c.D))
            ss = [s.sb(es, 'n1s%d' % i, (128, 2)) for i in range(2)]
            hst = [s.sb(es, 'n1h%d' % i, (128, KC, 128), BF16) for i in range(2)]
            tmp = [s.sb(es, 'n1t%d' % i, (128, 4, 128)) for i in range(2)]
            pt = [s.ps(es, 'n1p%d' % i, (128, 4, 128)) for i in range(4)]
            it = 0
            pi = 0
            for (kind, b, t0s, ln, mc) in c.seqs:
                for t0 in range(t0s, t0s + ln, 128):
                    x, sq, h = xt[it % 2], ss[it % 2], hst[it % 2]
                    s.ld(x[:, :], s.xres[t0:t0 + 128, :], ['xres'], [x])
                    s.act(junk[:, :], x[:, :], AF.Square, [x], [junk, sq], accum=sq[:, 0:1])
                    s.rsqrt(sq[:, 1:2], sq[:, 0:1], 1.0 / c.D, 1e-6, [sq], [sq])
                    s.ts('dve', x[:, :], x[:, :], sq[:, 1:2], None, ALU.mult, None, [x, sq], [x])
                    for q4 in range(KC // 4):
                        p_ = pt[pi % 4]
                        tm = tmp[pi % 2]
                        pi += 1
                        s.tr([(p_[:, j, :], x[:, (q4 * 4 + j) * 128:(q4 * 4 + j + 1) * 128]) for j in range(4)],
                             s.ident[:, :], [x, s.ident], [p_])
                        gb = s.gcol[:, 0, q4 * 4:q4 * 4 + 4, mc]
                        gb = AP(gb, gb.offset, [gb.ap[0], gb.ap[1], [0, 128]])
                        sb_ = s.modcol[:, q4 * 4:q4 * 4 + 4, mc]
                        sb_ = AP(sb_, sb_.offset, [sb_.ap[0], sb_.ap[1], [0, 128]])
                        s.tt('dve', tm[:, :, :], p_[:, :, :], gb, ALU.mult, [p_, s.gcol], [tm])
                        s.tt('pool', h[:, q4 * 4:q4 * 4 + 4, :], tm[:, :, :], sb_, ALU.add, [tm, s.modcol], [h])
                    s.ld(s.hT[:, t0:t0 + 128].rearrange("(k p) t -> p k t", p=128), h[:, :, :], [h], [('hT', t0)])
                    it += 1
            P.flush()

    def phase_inproj(s):
        c, P, l = s.c, s.P, s.l
        KC = c.KC
        G, SW = c.IPG, c.IPS
        nchunk = c.INC // 128
        sig0 = c.CONVEND // 128
        with ExitStack() as es:
            hb = [s.sb(es, 'iph%d' % i, (128, KC, G), BF16) for i in range(2)]
            wb = [s.sb(es, 'ipw%d' % i, (128, KC, 512), BF16) for i in range(3)]
            ob = [s.sb(es, 'ipo%d' % i, (128, 4, SW)) for i in range(2)]
            pp = [s.ps(es, 'ipp%d' % i, (128, SW)) for i in range(4)]
            wsrc = s.W['w_in'][l * c.D:(l + 1) * c.D, :].rearrange("(k p) n -> p k n", p=128)
            wi = 0
            pi = 0
            oi = 0
            nctx = c.NS * c.CTX
            groups = [(t, min(G, nctx - t)) for t in range(0, nctx, G)] + \
                     [(t, min(G, c.NT - t)) for t in range(nctx, c.NT, G)]
            for gi, (t0, n) in enumerate(groups):
                h = hb[gi % 2]
                s.ld(h[:, :, 0:n], s.hT[:, t0:t0 + n].rearrange("(k p) t -> p k t", p=128), ['hT_all'], [h])
                nch = nchunk
                if s.last and t0 + n <= nctx:
                    nch = c.CTXC // 128
                for c0 in range(0, nch, 4):
                    n4 = min(4, nch - c0)
                    w = wb[wi % 3]
                    wi += 1
                    s.ld(w[:, :, 0:n4 * 128], wsrc[:, :, c0 * 128:(c0 + n4) * 128], ['wfull_in'], [w])
                    for u0 in range(0, n, SW):
                        un = min(SW, n - u0)
                        o = ob[oi % 2]
                        oi += 1
                        for j in range(n4):
                            p_ = pp[pi % 4]
                            pi += 1
                            s.mm(p_[:, 0:un], [(w[:, k, j * 128:(j + 1) * 128], h[:, k, u0:u0 + un]) for k in range(KC)],
                                 [w, h], [p_])
                            if c0 + j >= sig0:
                                s.act(o[:, j, 0:un], p_[:, 0:un], AF.Sigmoid, [p_], [(o, j)])
                            elif (c0 + j) % 2 == 0:
                                s.cp('act', o[:, j, 0:un], p_[:, 0:un], [p_], [(o, j)])
                            else:
                                s.cp('dve', o[:, j, 0:un], p_[:, 0:un], [p_], [(o, j)])
                        s.ld(s.pT[c0 * 128:(c0 + n4) * 128, t0 + u0:t0 + u0 + un].rearrange("(j p) t -> p j t", p=128),
                             o[:, 0:n4, 0:un], [(o, j) for j in range(n4)],
                             [('pT', c0, t0 + u0)] + [(o, j) for j in range(n4)])
            P.flush()

    def load_halo(s, tile, n, row0, t0, seg, s0, s1):
        lo = max(t0 - 1, s0)
        hi = min(t0 + seg + 1, s1)
        a = lo - (t0 - 1)
        if a > 0:
            s.memset('pool', tile[0:n, 0:1], 0.0, [tile])
        if hi < t0 + seg + 1:
            s.memset('pool', tile[0:n, seg + 1:seg + 2], 0.0, [tile])
        s.ld(tile[0:n, a:a + (hi - lo)], s.pT[row0:row0 + n, lo:hi], ['pT_all'], [tile])

    def colvec(s, tile_col, vec_ap_1d_len_n):
        pass

    def phase_rwkv_pre(s):
        c, P, l = s.c, s.P, s.l
        I = s.I
        BC = c.BC
        BW = c.BW
        with ExitStack() as es:
            blocks = []
            for j in range(3 * BC):
                blocks.append((j * 128, 128))
            base = 3 * BW
            for j in range(2):
                blocks.append((base + j * c.DR, c.DR))
            base += 2 * c.DR
            for j in range(2):
                blocks.append((base + j * c.IR, c.IR))
            base += 2 * c.IR
            for j in range(c.GR // 128):
                blocks.append((base + j * 128, 128))
            NB = len(blocks)
            mu = s.sb(es, 'mu', (128, NB))
            omm = s.sb(es, 'omm', (128, NB))
            hmu = s.sb(es, 'hmu', (128, NB))
            s.memset('dve', mu[:, :], 0.0, [mu])
            for j, (r0, n) in enumerate(blocks):
                s.ldnc(mu[0:n, j:j + 1], I['shift_mu'][l:l + 1, r0:r0 + n].rearrange("o n -> n o"), [mu], [mu])
            s.ts('dve', omm[:, :], mu[:, :], -1.0, 1.0, ALU.mult, ALU.add, [mu], [omm])
            s.ts('dve', hmu[:, :], mu[:, :], 0.5, None, ALU.mult, None, [mu], [hmu])
            pc = s.sb(es, 'pcols', (128, 8, BC))
            def colload(idx, src2d_row):
                s.ldnc(pc[:, idx, :], src2d_row.rearrange("o (k p) -> p (o k)", p=128), [pc], [pc])
            s.memset('dve', pc[:, :, :], 0.0, [pc])
            colload(0, I['k_k'][l:l + 1, :])
            colload(1, I['k_a'][l:l + 1, :])
            for d in range(2):
                colload(3 + d, I['decay_bias'][2 * l + d:2 * l + d + 1, :])
                colload(5 + d, I['iclr_bias'][2 * l + d:2 * l + d + 1, :])
            if l > 0:
                colload(7, I['vres_bias'][l - 1:l, :])
            s.ts('dve', pc[:, 2, :], pc[:, 1, :], -1.0, 1.0, ALU.mult, ALU.add, [pc], [pc])
            dup = [s.sb(es, 'dup%d' % d, (c.DR, BW)) for d in range(2)]
            iup = [s.sb(es, 'iup%d' % d, (c.IR, BW)) for d in range(2)]
            for d in range(2):
                s.ld(dup[d][:, :], I['decay_up'][(2 * l + d) * c.DR:(2 * l + d + 1) * c.DR, :], (), [dup[d]])
                s.ld(iup[d][:, :], I['iclr_up'][(2 * l + d) * c.IR:(2 * l + d + 1) * c.IR, :], (), [iup[d]])
            if l > 0:
                vdn = s.sb(es, 'vdn', (128, BC, c.VR))
                vup = s.sb(es, 'vup', (c.VR, BW))
                s.ld(vdn[:, :, :], I['vres_down'][(l - 1) * BW:l * BW, :].rearrange("(k p) r -> p k r", p=128), (), [vdn])
                s.ld(vup[:, :], I['vres_up'][(l - 1) * c.VR:l * c.VR, :], (), [vup])
            SEG = 512
            NTB = 4
            Pin = [s.sb(es, 'rpP%d' % i, (128, SEG + 2)) for i in range(3)]
            tsum = [s.sb(es, 'rpT%d' % i, (128, SEG)) for i in range(2)]
            wdT = [s.sb(es, 'wdT%d' % d, (c.DR, SEG)) for d in range(2)]
            adT = [s.sb(es, 'adT%d' % d, (c.IR, SEG)) for d in range(2)]
            sg = [s.sb(es, 'sg%d' % j, (128, SEG)) for j in range(c.GR // 128)]
            vall = s.sb(es, 'vall', (128, BC, SEG))
            rt = s.sb(es, 'rt', (128, SEG))
            kt = s.sb(es, 'kt', (128, SEG))
            kh = s.sb(es, 'kh', (128, SEG))
            sq = s.sb(es, 'sqk', (128, SEG))
            kk = s.sb(es, 'kk', (128, SEG))
            nkk = s.sb(es, 'nkk', (128, SEG))
            wt = [s.sb(es, 'wt%d' % d, (128, SEG)) for d in range(2)]
            at = [s.sb(es, 'at%d' % d, (128, SEG)) for d in range(2)]
            kdt = [s.sb(es, 'kdt%d' % d, (128, SEG)) for d in range(2)]
            kat = [s.sb(es, 'kat%d' % d, (128, SEG)) for d in range(2)]
            vf = s.sb(es, 'vf', (128, SEG))
            vg = s.sb(es, 'vg', (128, SEG))
            vdT = s.sb(es, 'vdT', (c.VR, SEG))
            stg = [s.sb(es, 'stg%d' % i, (128, NTB, 128)) for i in range(3)]
            pA = [s.ps(es, 'rpA%d' % i, (128, SEG)) for i in range(3)]
            pTr = [s.ps(es, 'rpTr%d' % i, (128, NTB, 128)) for i in range(3)]
            pV = s.ps(es, 'rpV', (c.VR, SEG))
            cnt = {'p': 0, 't': 0, 'a': 0, 'tr': 0}

            def shift(dst, blk, t0, seg, s0, s1, res=None):
                res = res if res is not None else dst
                r0, n = blocks[blk]
                Pt = Pin[cnt['p'] % 3]
                cnt['p'] += 1
                ts_ = tsum[cnt['t'] % 2]
                cnt['t'] += 1
                s.load_halo(Pt, n, r0, t0, seg, s0, s1)
                s.tt('pool', ts_[0:n, 0:seg], Pt[0:n, 0:seg], Pt[0:n, 2:seg + 2], ALU.add, [Pt], [ts_])
                s.act(dst[0:n, 0:seg], Pt[0:n, 1:seg + 1], AF.Copy, [Pt, omm], [res], scale=omm[0:n, blk:blk + 1])
                s.stt('dve', dst[0:n, 0:seg], ts_[0:n, 0:seg], hmu[0:n, blk:blk + 1], dst[0:n, 0:seg],
                      ALU.mult, ALU.add, [ts_, res, hmu], [res])

            def emit_stream(name, tile, b, ch, tall0, seg, res=None):
                res = res if res is not None else tile
                ntb = seg // 128
                p_ = pTr[cnt['tr'] % 3]
                st = stg[cnt['tr'] % 3]
                cnt['tr'] += 1
                s.tr([(p_[:, j, :], tile[:, j * 128:(j + 1) * 128]) for j in range(ntb)], s.ident[:, :],
                     [res, s.ident], [p_])
                s.cp('act' if cnt['tr'] % 2 else 'dve', st[:, 0:ntb, :], p_[:, 0:ntb, :], [p_], [st])
                for tb in range(ntb):
                    dst = s.strm[name][b, 2 * ch:2 * ch + 2, tall0 + tb * 128:tall0 + (tb + 1) * 128, :].rearrange(
                        "h p j -> p h j")
                    src = st[:, tb, :].rearrange("p (h j) -> p h j", j=64)
                    s.ld(dst, src, [st], [('strm', name, b, ch, tall0, tb)])

            for (kind, b, t0s, ln, mc) in c.seqs:
                tallb = 0 if kind == 'c' else c.CTX
                for t0 in range(t0s, t0s + ln, SEG):
                    seg = min(SEG, t0s + ln - t0)
                    tall0 = tallb + (t0 - t0s)
                    a = (t0, seg, t0s, t0s + ln)
                    for d in range(2):
                        shift(wdT[d], 3 * BC + d, *a)
                        s.act(wdT[d][:, 0:seg], wdT[d][:, 0:seg], AF.Tanh, [wdT[d]], [wdT[d]])
                        shift(adT[d], 3 * BC + 2 + d, *a)
                    for j in range(c.GR // 128):
                        shift(sg[j], 3 * BC + 4 + j, *a)
                        s.act(sg[j][:, 0:seg], sg[j][:, 0:seg], AF.Sigmoid, [sg[j]], [sg[j]])
                        s.ld(s.sgdT[j * 128:(j + 1) * 128, t0:t0 + seg], sg[j][:, 0:seg], [sg[j]], [('sgdT', j, t0)])
                    for ch in range(BC):
                        shift(vall[:, ch, :], 2 * BC + ch, *a, res=vall)
                    if l == 0:
                        for ch in range(BC):
                            s.ld(s.vfT[ch * 128:(ch + 1) * 128, t0:t0 + seg], vall[:, ch, 0:seg], [vall],
                                 [('vfT', ch, t0)])
                    else:
                        s.mm(pV[:, 0:seg], [(vdn[:, ch, :], vall[:, ch, 0:seg]) for ch in range(BC)], [vdn, vall], [pV])
                        s.cp('dve', vdT[:, 0:seg], pV[:, 0:seg], [pV], [vdT])
                        for ch in range(BC):
                            p_ = pA[cnt['a'] % 3]
                            cnt['a'] += 1
                            s.mm(p_[:, 0:seg], [(vup[:, ch * 128:(ch + 1) * 128], vdT[:, 0:seg])], [vup, vdT], [p_])
                            s.act(vg[:, 0:seg], p_[:, 0:seg], AF.Sigmoid, [p_, pc], [vg], bias=pc[:, 7, ch:ch + 1])
                            s.ld(vf[:, 0:seg], s.vfT[ch * 128:(ch + 1) * 128, t0:t0 + seg], ['vfT_all'], [vf])
                            s.tt('dve', vf[:, 0:seg], vf[:, 0:seg], vall[:, ch, 0:seg], ALU.subtract, [vf, vall], [vf])
                            s.tt('dve', vf[:, 0:seg], vf[:, 0:seg], vg[:, 0:seg], ALU.mult, [vf, vg], [vf])
                            s.tt('dve', vall[:, ch, 0:seg], vall[:, ch, 0:seg], vf[:, 0:seg], ALU.add, [vf, vall], [vall])
                    for ch in range(BC):
                        shift(rt, ch, *a)
                        shift(kt, BC + ch, *a)
                        s.ts('dve', kh[:, 0:seg], kt[:, 0:seg], pc[:, 0, ch:ch + 1], None, ALU.mult, None, [kt, pc], [kh])
                        s.tt('pool', sq[:, 0:seg], kh[:, 0:seg], kh[:, 0:seg], ALU.mult, [kh], [sq])
                        p_ = pA[cnt['a'] % 3]
                        cnt['a'] += 1
                        s.mm(p_[:, 0:seg], [(s.blk[:, :], sq[:, 0:seg])], [s.blk, sq], [p_])
                        s.ts('dve', sq[:, 0:seg], p_[:, 0:seg], 1e-24, None, ALU.max, None, [p_], [sq])
                        s.rsqrt(sq[:, 0:seg], sq[:, 0:seg], 1.0, 0.0, [sq], [sq])
                        s.tt('dve', kk[:, 0:seg], kh[:, 0:seg], sq[:, 0:seg], ALU.mult, [kh, sq], [kk])
                        s.ts('pool', nkk[:, 0:seg], kk[:, 0:seg], -1.0, None, ALU.mult, None, [kk], [nkk])
                        for d in range(2):
                            p_ = pA[cnt['a'] % 3]
                            cnt['a'] += 1
                            s.mm(p_[:, 0:seg], [(dup[d][:, ch * 128:(ch + 1) * 128], wdT[d][:, 0:seg])],
                                 [dup[d], wdT[d]], [p_])
                            s.act(wt[d][:, 0:seg], p_[:, 0:seg], AF.Sigmoid, [p_, pc], [wt[d]],
                                  bias=pc[:, 3 + d, ch:ch + 1])
                            s.act(wt[d][:, 0:seg], wt[d][:, 0:seg], AF.Exp, [wt[d]], [wt[d]],
                                  scale=-0.6065306597126334)
                            p_ = pA[cnt['a'] % 3]
                            cnt['a'] += 1
                            s.mm(p_[:, 0:seg], [(iup[d][:, ch * 128:(ch + 1) * 128], adT[d][:, 0:seg])],
                                 [iup[d], adT[d]], [p_])
                            s.act(at[d][:, 0:seg], p_[:, 0:seg], AF.Sigmoid, [p_, pc], [at[d]],
                                  bias=pc[:, 5 + d, ch:ch + 1])
                            s.ts('dve', kdt[d][:, 0:seg], at[d][:, 0:seg], pc[:, 1, ch:ch + 1], pc[:, 2, ch:ch + 1],
                                 ALU.mult, ALU.add, [at[d], pc], [kdt[d]])
                            s.tt('dve', kdt[d][:, 0:seg], kdt[d][:, 0:seg], kt[:, 0:seg], ALU.mult, [kdt[d], kt],
                                 [kdt[d]])
                            s.tt('pool', kat[d][:, 0:seg], kk[:, 0:seg], at[d][:, 0:seg], ALU.mult, [kk, at[d]],
                                 [kat[d]])
                            emit_stream('w%d' % d, wt[d], b, ch, tall0, seg)
                            emit_stream('kd%d' % d, kdt[d], b, ch, tall0, seg)
                            emit_stream('ka%d' % d, kat[d], b, ch, tall0, seg)
                        emit_stream('nkk', nkk, b, ch, tall0, seg)
                        emit_stream('r', rt, b, ch, tall0, seg)
                        emit_stream('v', vall[:, ch, :], b, ch, tall0, seg, res=vall)
            P.flush()

    def phase_scan(s):
        c, P = s.c, s.P
        TC = 16
        IQ, IP, H, NS = c.IQ, c.IP, c.H, c.NS
        NBH = NS * H
        names = ['nkk', 'ka', 'w', 'kd', 'r']
        with ExitStack() as es:
            S = [s.sb(es, 'S%d' % i, (128, 2, IP, 64)) for i in range(2)]
            t1 = s.sb(es, 'sc_t1', (128, 2, IP, 64))
            t2 = s.sb(es, 'sc_t2', (128, 2, IP, 64))
            Sw = s.sb(es, 'sc_sw', (128, 2, IP, 64))
            t3 = [s.sb(es, 'sc_t3%d' % i, (128, 2, IP, 64)) for i in range(2)]
            t4 = [s.sb(es, 'sc_t4%d' % i, (128, 2, IP, 64)) for i in range(2)]
            sa = s.sb(es, 'sc_sa', (128, 2, IP))
            st = {n: [s.sb(es, 'sc_%s%d' % (n, i), (128, 2, TC, 64)) for i in range(2)] for n in names}
            vt = [s.sb(es, 'sc_v%d' % i, (128, 2, TC, IP)) for i in range(2)]
            yb = [s.sb(es, 'sc_y%d' % i, (128, 2, TC, IP)) for i in range(2)]
            for hf in range(2):
                s.memset('dve', S[0][:, hf, :, :], 0.0, [(S[0], hf)])
            cur = 0
            step = 0
            ci = 0
            pending = []
            stores = []

            def flush_pending(which):
                for fn in pending:
                    fn(which)

            for (tb0, T) in ((0, c.CTX), (c.CTX, c.SEQ)):
                for ck in range(T // TC):
                    k = ci % 2
                    ci += 1
                    tf = tb0 + ck * TC
                    tr_ = tb0 + T - (ck + 1) * TC
                    for n in names:
                        for d in range(2):
                            tt0 = tf if d == 0 else tr_
                            nm = n if n in ('nkk', 'r') else '%s%d' % (n, d)
                            src = s.strm[nm][:, :, tt0:tt0 + TC, :].rearrange("b h t j -> (b h) t j")
                            for iq in range(IQ):
                                s.ld(st[n][k][iq * NBH:(iq + 1) * NBH, d, :, :], src, ['strm_all'], [(st[n][k], d)])
                    for d in range(2):
                        tt0 = tf if d == 0 else tr_
                        for iq in range(IQ):
                            src = s.strm['v'][:, :, tt0:tt0 + TC, iq * IP:(iq + 1) * IP].rearrange(
                                "b h t j -> (b h) t j")
                            s.ldnc(vt[k][iq * NBH:(iq + 1) * NBH, d, :, :], src, ['strm_all'], [(vt[k], d)])
                    y = yb[k]
                    for sp in range(TC):
                        So, Sn = S[cur], S[1 - cur]
                        T3 = t3[step % 2]
                        T4 = t4[step % 2]

                        def jb(tile, hf):
                            a = tile[:, hf, sp if hf == 0 else TC - 1 - sp, :]
                            return AP(a, a.offset, [a.ap[0], [0, IP], [1, 64]])

                        def ib(tile, hf):
                            a = tile[:, hf, sp if hf == 0 else TC - 1 - sp, :]
                            return AP(a, a.offset, [a.ap[0], [1, IP], [0, 64]])

                        def jb2(tile):
                            a = tile[:, 0, sp, :]
                            return AP(a, a.offset, [a.ap[0], [(2 * TC - 1 - 2 * sp) * 64, 2], [0, IP], [1, 64]])

                        def ib2(tile):
                            a = tile[:, 0, sp, :]
                            return AP(a, a.offset, [a.ap[0], [(2 * TC - 1 - 2 * sp) * IP, 2], [1, IP], [0, 64]])

                        s.tt('pool', T3[:, :, :, :], ib2(vt[k]), jb2(st['kd'][k]), ALU.mult,
                             [(vt[k], 0), (vt[k], 1), (st['kd'][k], 0), (st['kd'][k], 1)], [(T3, 0), (T3, 1)])
                        for hf in range(2):
                            s.tt('pool', Sw[:, hf, :, :], So[:, hf, :, :], jb(st['w'][k], hf), ALU.mult,
                                 [(So, hf), (st['w'][k], hf)], [(Sw, hf)])
                        for hf in range(2):
                            s.tt('dve', t1[:, hf, :, :], So[:, hf, :, :], jb(st['nkk'][k], hf), ALU.mult,
                                 [(So, hf), (st['nkk'][k], hf)], [(t1, hf)])
                        for hf in range(2):
                            s.red(sa[:, hf, :], t1[:, hf, :, :], [(t1, hf)], [(sa, hf)])
                        flush_pending('pool')
                        for hf in range(2):
                            a = sa[:, hf, :]
                            sab = AP(a, a.offset, [a.ap[0], [1, IP], [0, 64]])
                            s.tt('dve', t2[:, hf, :, :], sab, jb(st['ka'][k], hf), ALU.mult,
                                 [(sa, hf), (st['ka'][k], hf)], [(t2, hf)])
                        for hf in range(2):
                            s.tt('dve', Sn[:, hf, :, :], Sw[:, hf, :, :], t2[:, hf, :, :], ALU.add,
                                 [(Sw, hf), (t2, hf)], [(Sn, hf)])
                        for hf in range(2):
                            s.tt('dve', Sn[:, hf, :, :], Sn[:, hf, :, :], T3[:, hf, :, :], ALU.add,
                                 [(Sn, hf), (T3, hf)], [(Sn, hf)])
                        flush_pending('dve')
                        pending.clear()
                        for fn in stores:
                            fn()
                        stores.clear()

                        def mk(Sn, T4, y, k, sp):
                            def fn(which):
                                if which == 'pool':
                                    a = st['r'][k][:, 0, sp, :]
                                    rb = AP(a, a.offset, [a.ap[0], [(2 * TC - 1 - 2 * sp) * 64, 2], [0, IP], [1, 64]])
                                    s.tt('pool', T4[:, :, :, :], Sn[:, :, :, :], rb, ALU.mult,
                                         [(Sn, 0), (Sn, 1), (st['r'][k], 0), (st['r'][k], 1)], [(T4, 0), (T4, 1)])
                                else:
                                    a = y[:, 0, sp, :]
                                    yo = AP(a, a.offset, [a.ap[0], [(2 * TC - 1 - 2 * sp) * IP, 2], [1, IP]])
                                    s.red(yo, T4[:, :, :, :], [(T4, 0), (T4, 1)], [(y, 0), (y, 1)])
                            return fn
                        pending.append(mk(Sn, T4, y, k, sp))
                        cur = 1 - cur
                        step += 1

                    def mkstore(y, tf, tr_):
                        def fn():
                            for d in range(2):
                                tt0 = tf if d == 0 else tr_
                                for iq in range(IQ):
                                    dst = s.ysc[d][:, :, tt0:tt0 + TC, iq * IP:(iq + 1) * IP].rearrange(
                                        "b h t j -> (b h) t j")
                                    s.ldnc(dst, y[iq * NBH:(iq + 1) * NBH, d, :, :], [(y, d)], [('ysc', d, tt0, iq)])
                        return fn
                    stores.append(mkstore(y, tf, tr_))
            flush_pending('pool')
            flush_pending('dve')
            for fn in stores:
                fn()
            P.flush()

    def phase_rwkv_post(s):
        c, P, l = s.c, s.P, s.l
        I = s.I
        BW, H, BC = c.BW, c.H, c.BC
        with ExitStack() as es:
            def rowb(name, src_row):
                t = s.sb(es, name, (128, BW))
                s.ld(t[:, :], AP(src_row, src_row.offset, [[0, 128], [1, BW]]), (), [t])
                return t
            gnw = rowb('gnw', I['gn_w'][l:l + 1, :])
            gnb = rowb('gnb', I['gn_b'][l:l + 1, :])
            rkr = rowb('rkr', I['r_k'][l:l + 1, :])
            gup = s.sb(es, 'gup', (128, c.GR // 128, BW))
            s.ld(gup[:, :, :], I['gate_up'][l * c.GR:(l + 1) * c.GR, :].rearrange("(k p) n -> p k n", p=128), (), [gup])
            nb = 2
            y0 = [s.sb(es, 'po_y0%d' % i, (128, H, 64)) for i in range(nb)]
            y1 = [s.sb(es, 'po_y1%d' % i, (128, H, 64)) for i in range(nb)]
            rr = [s.sb(es, 'po_r%d' % i, (128, H, 64)) for i in range(nb)]
            k0 = [s.sb(es, 'po_k0%d' % i, (128, H, 64)) for i in range(nb)]
            k1 = [s.sb(es, 'po_k1%d' % i, (128, H, 64)) for i in range(nb)]
            vv = [s.sb(es, 'po_v%d' % i, (128, H, 64)) for i in range(nb)]
            sgt = [s.sb(es, 'po_sg%d' % i, (128, c.GR // 128, 128)) for i in range(nb)]
            sq = s.sb(es, 'po_sq', (128, H, 64))
            st = s.sb(es, 'po_st', (128, 4, H))
            ob = [s.sb(es, 'po_o%d' % i, (128, BC, 128), BF16) for i in range(2)]
            pg = [s.ps(es, 'po_pg%d' % i, (128, 512)) for i in range(max(BW // 512, 1))]
            ptr = [s.ps(es, 'po_pt%d' % i, (128, 4, 128)) for i in range(2)]
            it = 0
            for (kind, b, t0s, ln, mc) in c.seqs:
                if s.last and kind == 'c':
                    continue
                tallb = 0 if kind == 'c' else c.CTX
                for t0 in range(t0s, t0s + ln, 128):
                    k = it % nb
                    it += 1
                    ta = tallb + (t0 - t0s)
                    def tok(dr):
                        return dr[b, :, ta:ta + 128, :].rearrange("h t j -> t h j")
                    s.ld(y0[k][:, :, :], tok(s.ysc[0]), ['ysc_all'], [y0[k]])
                    s.ld(y1[k][:, :, :], tok(s.ysc[1]), ['ysc_all'], [y1[k]])
                    s.ld(rr[k][:, :, :], tok(s.strm['r']), ['strm_all'], [rr[k]])
                    s.ld(k0[k][:, :, :], tok(s.strm['kd0']), ['strm_all'], [k0[k]])
                    s.ld(k1[k][:, :, :], tok(s.strm['kd1']), ['strm_all'], [k1[k]])
                    s.ld(vv[k][:, :, :], tok(s.strm['v']), ['strm_all'], [vv[k]])
                    s.ld(sgt[k][:, :, :], s.sgdT[:, t0:t0 + 128].rearrange("(k p) t -> p k t", p=128), ['sgdT_all'],
                         [sgt[k]])
                    Y, Y1, R, K0, K1, V = y0[k], y1[k], rr[k], k0[k], k1[k], vv[k]
                    def hb(ap2):
                        return AP(ap2, ap2.offset, [ap2.ap[0], ap2.ap[1], [0, 64]])
                    s.tt('dve', Y[:, :, :], Y[:, :, :], Y1[:, :, :], ALU.add, [Y, Y1], [Y])
                    s.red(st[:, 0, :], Y[:, :, :], [Y], [st])
                    s.ts('dve', st[:, 0, :], st[:, 0, :], 1.0 / 64, None, ALU.mult, None, [st], [st])
                    s.tt('dve', Y[:, :, :], Y[:, :, :], hb(st[:, 0, :]), ALU.subtract, [Y, st], [Y])
                    s.tt('pool', sq[:, :, :], Y[:, :, :], Y[:, :, :], ALU.mult, [Y], [sq])
                    s.red(st[:, 1, :], sq[:, :, :], [sq], [st])
                    s.rsqrt(st[:, 1, :], st[:, 1, :], 1.0 / 64, 64e-5, [st], [st])
                    s.tt('dve', Y[:, :, :], Y[:, :, :], hb(st[:, 1, :]), ALU.mult, [Y, st], [Y])
                    Yf = Y[:, :, :].rearrange("p h j -> p (h j)")
                    s.tt('pool', Yf, Yf, gnw[:, :], ALU.mult, [Y, gnw], [Y])
                    s.tt('pool', Yf, Yf, gnb[:, :], ALU.add, [Y, gnb], [Y])
                    s.tt('pool', K0[:, :, :], K0[:, :, :], K1[:, :, :], ALU.add, [K0, K1], [K0])
                    s.tt('pool', K0[:, :, :], K0[:, :, :], R[:, :, :], ALU.mult, [K0, R], [K0])
                    K0f = K0[:, :, :].rearrange("p h j -> p (h j)")
                    s.tt('pool', K0f, K0f, rkr[:, :], ALU.mult, [K0, rkr], [K0])
                    s.red(st[:, 2, :], K0[:, :, :], [K0], [st])
                    s.tt('dve', V[:, :, :], V[:, :, :], hb(st[:, 2, :]), ALU.mult, [V, st], [V])
                    s.tt('dve', Y[:, :, :], Y[:, :, :], V[:, :, :], ALU.add, [Y, V], [Y])
                    for gi in range(len(pg)):
                        n0 = gi * 512
                        n1 = min(BW, n0 + 512)
                        s.mm(pg[gi][:, 0:n1 - n0], [(sgt[k][:, kk_, :], gup[:, kk_, n0:n1]) for kk_ in range(c.GR // 128)],
                             [sgt[k], gup], [pg[gi]])
                        s.tt('dve', Yf[:, n0:n1], Yf[:, n0:n1], pg[gi][:, 0:n1 - n0], ALU.mult, [Y, pg[gi]], [Y])
                    o = ob[it % 2]
                    for q4 in range(0, BC, 4):
                        n4 = min(4, BC - q4)
                        p_ = ptr[(q4 // 4) % 2]
                        s.tr([(p_[:, j, :], Yf[:, (q4 + j) * 128:(q4 + j + 1) * 128]) for j in range(n4)], s.ident[:, :],
                             [Y, s.ident], [p_])
                        s.cp('act', o[:, q4:q4 + n4, :], p_[:, 0:n4, :], [p_], [o])
                    s.ld(s.brT[0][:, t0:t0 + 128].rearrange("(k p) t -> p k t", p=128), o[:, :, :], [o], [('br0', t0)])
            P.flush()

    def phase_attn(s):
        c, P, l = s.c, s.P, s.l
        I = s.I
        KVH, H = c.KVH, c.H
        TA = c.TALL
        NBK = TA // 128
        NCB = c.CTX // 128
        G = 4
        with ExitStack() as es:
            rope = s.sb(es, 'rope', (64, 2, c.SEQ))
            s.ld(rope[:, :, :], I['k_rope'].rearrange("(a n) t -> n a t", a=2), (), [rope])
            esk = s.sb(es, 'esk', (64, H))
            s.ld(esk[:, :], AP(I['attn_sink'], l * H, [[0, 64], [1, H]]), (), [esk])
            s.act(esk[:, :], esk[:, :], AF.Exp, [esk], [esk])
            kT = s.sb(es, 'kT', (64, KVH, TA), BF16)
            qT = s.sb(es, 'qT', (64, G, TA), BF16)
            vtk = s.sb(es, 'vtk', (128, NBK, KVH, 64), BF16)
            xin = [s.sb(es, 'at_x%d' % i, (64, 512)) for i in range(3)]
            t1 = [s.sb(es, 'at_t%d' % i, (64, 512)) for i in range(2)]
            eb = [s.sb(es, 'at_e%d' % i, (128, G, 128), BF16) for i in range(3)]
            den = s.sb(es, 'at_den', (64, G, 128))
            ot = [s.sb(es, 'at_o%d' % i, (64, G, 128), BF16) for i in range(2)]
            psw = [s.ps(es, 'at_ps%d' % i, (64, 512)) for i in range(2)]
            pss = [s.ps(es, 'at_s%d' % i, (128, G, 128)) for i in range(2)]
            pso = s.ps(es, 'at_po', (64, G, 128))
            psd = s.ps(es, 'at_pd', (64, G, 128))
            psv = s.ps(es, 'at_pv', (128, 4, 64))
            cnt = {'x': 0, 'e': 0, 'o': 0}

            def load_feat(dst, row0, b, with_q_ctx, res):
                segs = []
                tc0 = b * c.CTX
                for t in range(0, c.CTX, 512):
                    n = min(512, c.CTX - t)
                    segs.append((tc0 + t, t, n, None))
                tl0 = c.NS * c.CTX + b * c.SEQ
                for t in range(0, c.SEQ, 512):
                    n = min(512, c.SEQ - t)
                    segs.append((tl0 + t, c.CTX + t, n, t))
                for (tg, td, n, tp) in segs:
                    if tp is None and not with_q_ctx:
                        continue
                    x = xin[cnt['x'] % 3]
                    cnt['x'] += 1
                    s.ld(x[:, 0:n], s.pT[row0:row0 + 64, tg:tg + n], ['pT_all'], [x])
                    if tp is None:
                        s.cp('act', dst[:, td:td + n], x[:, 0:n], [x], [res])
                    else:
                        p_ = psw[cnt['x'] % 2]
                        ta_, tb_ = t1[0], t1[1]
                        s.mm(p_[:, 0:n], [(s.swp[:, :], x[:, 0:n])], [s.swp, x], [p_])
                        s.tt('pool', ta_[:, 0:n], x[:, 0:n], rope[:, 0, tp:tp + n], ALU.mult, [x, rope], [ta_])
                        s.tt('dve', tb_[:, 0:n], p_[:, 0:n], rope[:, 1, tp:tp + n], ALU.mult, [p_, rope], [tb_])
                        s.tt('dve', dst[:, td:td + n], ta_[:, 0:n], tb_[:, 0:n], ALU.add, [ta_, tb_], [res])

            for b in range(c.NS):
                for kh in range(KVH):
                    load_feat(kT[:, kh, :], c.RC + kh * 64, b, True, kT)
                    tc0 = b * c.CTX
                    tl0 = c.NS * c.CTX + b * c.SEQ
                    for blk0 in range(0, NBK, 4):
                        nb_ = min(4, NBK - blk0)
                        x = xin[cnt['x'] % 3]
                        cnt['x'] += 1
                        for j in range(nb_):
                            kb = blk0 + j
                            tg = tc0 + kb * 128 if kb < NCB else tl0 + (kb - NCB) * 128
                            s.ld(x[:, j * 128:(j + 1) * 128], s.pT[c.RC + c.KVW + kh * 64:c.RC + c.KVW + kh * 64 + 64,
                                                                  tg:tg + 128], ['pT_all'], [x])
                        s.tr([(psv[:, j, :], x[:, j * 128:(j + 1) * 128]) for j in range(nb_)], s.ident[0:64, 0:64],
                             [x, s.ident], [psv])
                        s.cp('act', vtk[:, blk0:blk0 + nb_, kh, :], psv[:, 0:nb_, :], [psv], [vtk])
                for kh in range(KVH):
                    for g in range(G):
                        load_feat(qT[:, g, :], c.CTXC + (kh * G + g) * 64, b, not s.last, qT)
                    qblocks = []
                    if not s.last:
                        for n in range(NCB):
                            qblocks.append(('c', n))
                    for n in range(c.SEQ // 128):
                        qblocks.append(('l', n))
                    for (qk, n) in qblocks:
                        if qk == 'c':
                            q0 = n * 128
                            kbs = [(kb, None) for kb in range(NCB)]
                            tok0 = b * c.CTX + n * 128
                        else:
                            q0 = c.CTX + n * 128
                            kbs = [(kb, None) for kb in range(NCB)]
                            nl = c.SEQ // 128
                            if n > 0:
                                kbs.append((NCB + n - 1, 0))
                            kbs.append((NCB + n, None))
                            if n < nl - 1:
                                kbs.append((NCB + n + 1, 1))
                            tok0 = c.NS * c.CTX + b * c.SEQ + n * 128
                        for i, (kb, mk) in enumerate(kbs):
                            ps_ = pss[cnt['e'] % 2]
                            e = eb[cnt['e'] % 3]
                            cnt['e'] += 1
                            s.mm(ps_[:, :, :], [(kT[:, kh, kb * 128:(kb + 1) * 128], qT[:, :, q0:q0 + 128])],
                                 [kT, qT], [ps_])
                            s.act(e[:, :, :], ps_[:, :, :], AF.Exp, [ps_], [e], scale=0.125)
                            if mk is not None:
                                m = s.masks[:, mk, :]
                                mb = AP(m, m.offset, [m.ap[0], [0, G], m.ap[1]])
                                s.tt('dve', e[:, :, :], e[:, :, :], mb, ALU.mult, [e, s.masks], [e])
                            s.mm(pso[:, :, :], [(vtk[:, kb, kh, :], e[:, :, :])], [vtk, e], [pso],
                                 start=(i == 0), stop=(i == len(kbs) - 1))
                            s.mm(psd[:, :, :], [(s.onesb[:, :], e[:, :, :])], [s.onesb, e], [psd],
                                 start=(i == 0), stop=(i == len(kbs) - 1))
                        a = esk[:, kh * G:(kh + 1) * G]
                        eskb = AP(a, a.offset, [a.ap[0], a.ap[1], [0, 128]])
                        s.tt('dve', den[:, :, :], psd[:, :, :], eskb, ALU.add, [psd, esk], [den])
                        s.P.op('dve', lambda g_: g_.reciprocal(out=den[:, :, :], in_=den[:, :, :]), [den], [den])
                        o = ot[cnt['o'] % 2]
                        cnt['o'] += 1
                        s.tt('dve', o[:, :, :], pso[:, :, :], den[:, :, :], ALU.mult, [pso, den], [o])
                        s.ld(s.brT[2][kh * G * 64:(kh + 1) * G * 64, tok0:tok0 + 128].rearrange("(g p) t -> p g t", p=64),
                             o[:, :, :], [o], [('br2', kh, tok0)])
            P.flush()

    def phase_conv(s):
        c, P, l = s.c, s.P, s.l
        BC, BW = c.BC, c.BW
        SEG = 512
        with ExitStack() as es:
            cw = s.sb(es, 'cw', (128, 3, BC))
            for j in range(3):
                s.ldnc(cw[:, j, :], s.I['conv_w'][3 * l + j:3 * l + j + 1, :].rearrange("o (k p) -> p (o k)", p=128),
                       [cw], [cw])
            bt = [s.sb(es, 'cv_b%d' % i, (128, SEG)) for i in range(2)]
            ct = [s.sb(es, 'cv_c%d' % i, (128, SEG + 2)) for i in range(2)]
            ut = [s.sb(es, 'cv_u%d' % i, (128, SEG + 2)) for i in range(2)]
            o = [s.sb(es, 'cv_o%d' % i, (128, SEG)) for i in range(2)]
            ob = [s.sb(es, 'cv_ob%d' % i, (128, SEG), BF16) for i in range(2)]
            it = 0
            for (kind, b, t0s, ln, mc) in c.seqs:
                if s.last and kind == 'c':
                    continue
                for t0 in range(t0s, t0s + ln, SEG):
                    seg = min(SEG, t0s + ln - t0)
                    for ch in range(BC):
                        k = it % 2
                        it += 1
                        B_, C_, U_, O_, OB = bt[k], ct[k], ut[k], o[k], ob[k]
                        s.ld(B_[:, 0:seg], s.pT[c.QEND + ch * 128:c.QEND + (ch + 1) * 128, t0:t0 + seg], ['pT_all'], [B_])
                        s.load_halo(C_, 128, c.QEND + BW + ch * 128, t0, seg, t0s, t0s + ln)
                        s.load_halo(U_, 128, c.QEND + 2 * BW + ch * 128, t0, seg, t0s, t0s + ln)
                        s.tt('pool', C_[:, 0:seg + 2], C_[:, 0:seg + 2], U_[:, 0:seg + 2], ALU.mult, [C_, U_], [C_])
                        s.ts('dve', O_[:, 0:seg], C_[:, 1:seg + 1], cw[:, 1, ch:ch + 1], None, ALU.mult, None, [C_, cw], [O_])
                        s.stt('dve', O_[:, 0:seg], C_[:, 0:seg], cw[:, 0, ch:ch + 1], O_[:, 0:seg], ALU.mult, ALU.add,
                              [C_, cw, O_], [O_])
                        s.stt('dve', O_[:, 0:seg], C_[:, 2:seg + 2], cw[:, 2, ch:ch + 1], O_[:, 0:seg], ALU.mult, ALU.add,
                              [C_, cw, O_], [O_])
                        s.tt('pool', OB[:, 0:seg], O_[:, 0:seg], B_[:, 0:seg], ALU.mult, [O_, B_], [OB])
                        s.ld(s.brT[1][ch * 128:(ch + 1) * 128, t0:t0 + seg], OB[:, 0:seg], [OB], [('br1', ch, t0)])
            P.flush()

    def phase_merge(s):
        c, P, l = s.c, s.P, s.l
        KC, BC, D, BW = c.KC, c.BC, c.D, c.BW
        G = 512
        NS1 = c.NS + 1
        with ExitStack() as es:
            br = [[s.sb(es, 'mg_br%d_%d' % (i, k), (128, BC, G), BF16) for k in range(1)] for i in range(3)]
            wb = [s.sb(es, 'mg_w%d' % i, (128, BC, 512), BF16) for i in range(2)]
            wo = [s.sb(es, 'mg_wo%d' % i, (128, KC, 512), BF16) for i in range(2)]
            gt = [s.sb(es, 'mg_g%d' % i, (128, 4, G)) for i in range(2)]
            mT = s.sb(es, 'mg_m', (128, KC, G), BF16)
            acc = s.sb(es, 'mg_acc', (128, 4, G))
            tmp = s.sb(es, 'mg_tmp', (128, G))
            xt = [s.sb(es, 'mg_x%d' % i, (128, D)) for i in range(4)]
            g2t = s.sb(es, 'mg_gate', (128, D))
            pp = [s.ps(es, 'mg_p%d' % i, (128, 512)) for i in range(4)]
            M6 = 6 * D
            wbs = s.W['w_branch']
            wos = s.W['w_out'][l * D:(l + 1) * D, :].rearrange("(k p) n -> p k n", p=128)
            wi = 0
            pi = 0
            gi_ = 0
            xi = 0
            groups = []
            for (kind, b, t0s, ln, mc) in c.seqs:
                if s.last and kind == 'c':
                    continue
                for t0 in range(t0s, t0s + ln, G):
                    groups.append((t0, min(G, t0s + ln - t0), mc))
            for gidx, (t0, n, mc) in enumerate(groups):
                k = 0
                s.ld(g2t[:, :], AP(s.modrow, (l * NS1 + mc) * M6 + 2 * D, [[0, 128], [1, D]]), ['modrow_all'], [g2t])
                for i in range(3):
                    s.ld(br[i][k][:, :, 0:n], s.brT[i][:, t0:t0 + n].rearrange("(k p) t -> p k t", p=128), ['br_all'],
                         [br[i][k]])
                for oq in range(D // 512):
                    for i in range(3):
                        w = wb[wi % 2]
                        wi += 1
                        s.ld(w[:, :, :], wbs[(l * 3 + i) * BW:(l * 3 + i + 1) * BW, oq * 512:(oq + 1) * 512].rearrange(
                            "(k p) n -> p k n", p=128), ['wfull_b'], [w])
                        gts = gt[gi_ % 2]
                        gi_ += 1
                        r0 = c.CONVEND + i * D + oq * 512
                        s.ld(gts[:, :, 0:n], s.pT[r0:r0 + 512, t0:t0 + n].rearrange("(j p) t -> p j t", p=128), ['pT_all'],
                             [gts])
                        for j in range(4):
                            p_ = pp[pi % 4]
                            pi += 1
                            s.mm(p_[:, 0:n], [(w[:, kk_, j * 128:(j + 1) * 128], br[i][k][:, kk_, 0:n]) for kk_ in range(BC)],
                                 [w, br[i][k]], [p_])
                            if i == 0:
                                s.tt('dve', acc[:, j, 0:n], p_[:, 0:n], gts[:, j, 0:n], ALU.mult, [p_, gts], [(acc, j)])
                            else:
                                s.tt('dve', tmp[:, 0:n], p_[:, 0:n], gts[:, j, 0:n], ALU.mult, [p_, gts], [tmp])
                                if i == 1:
                                    s.tt('pool', acc[:, j, 0:n], acc[:, j, 0:n], tmp[:, 0:n], ALU.add, [(acc, j), tmp],
                                         [(acc, j)])
                                else:
                                    s.tt('pool', mT[:, oq * 4 + j, 0:n], acc[:, j, 0:n], tmp[:, 0:n], ALU.add,
                                         [(acc, j), tmp], [(mT, oq * 4 + j)])
                allm = [(mT, j) for j in range(KC)]
                wts = []
                for oq in range(D // 512):
                    w = wo[oq % 2]
                    s.ld(w[:, :, :], wos[:, :, oq * 512:(oq + 1) * 512], ['wfull_o'], [w])
                    for tb in range(n // 128):
                        x = xt[tb]
                        if oq == 0:
                            s.ld(x[:, :], s.xres[t0 + tb * 128:t0 + (tb + 1) * 128, :], ['xres'], [x])
                        p_ = pp[pi % 4]
                        pi += 1
                        s.mm(p_[:, :], [(mT[:, kk_, tb * 128:(tb + 1) * 128], w[:, kk_, :]) for kk_ in range(KC)],
                             allm + [w], [p_])
                        s.tt('dve', tmp[:, 0:512], p_[:, :], g2t[:, oq * 512:(oq + 1) * 512], ALU.mult, [p_, g2t], [tmp])
                        s.tt('pool', x[:, oq * 512:(oq + 1) * 512], x[:, oq * 512:(oq + 1) * 512], tmp[:, 0:512], ALU.add,
                             [x, tmp], [x])
                        if oq == D // 512 - 1:
                            s.ld(s.xres[t0 + tb * 128:t0 + (tb + 1) * 128, :], x[:, :], [x], ['xres'])
                xi += n // 128
            P.flush()

    def phase_router(s, kind):
        c, P, l = s.c, s.P, s.l
        KC, D, NE = c.KC, c.D, c.NE
        n_tok = c.SEQ if kind == 'l' else c.CTX
        cap = c.CAPL if kind == 'l' else c.CAPC
        s.cap = cap
        with ExitStack() as es:
            wr = s.sb(es, 'rt_w', (128, KC, NE))
            s.ld(wr[:, :, :], s.I['w_router'][l * D:(l + 1) * D, :].rearrange("(k p) e -> p k e", p=128), (), [wr])
            xt = [s.sb(es, 'rt_x%d' % i, (128, D)) for i in range(2)]
            junk = s.sb(es, 'rt_j', (128, D))
            ss = [s.sb(es, 'rt_s%d' % i, (128, 2)) for i in range(2)]
            hT = [s.sb(es, 'rt_h%d' % i, (128, KC, 128)) for i in range(2)]
            tmp = [s.sb(es, 'rt_t%d' % i, (128, 4, 128)) for i in range(2)]
            lg = [s.sb(es, 'rt_lg%d' % i, (128, NE)) for i in range(2)]
            sm = [s.sb(es, 'rt_sm%d' % i, (128, 2)) for i in range(2)]
            affT = [s.sb(es, 'rt_aff%d' % b, (NE, n_tok)) for b in range(c.NS)]
            work = [s.sb(es, 'rt_wk%d' % b, (NE, n_tok)) for b in range(c.NS)]
            pt = [s.ps(es, 'rt_p%d' % i, (128, 4, 128)) for i in range(2)]
            pl = [s.ps(es, 'rt_pl%d' % i, (128, NE)) for i in range(2)]
            pa = [s.ps(es, 'rt_pa%d' % i, (NE, 128)) for i in range(2)]
            it = 0
            pi = 0
            for (kd, b, t0s, ln, mc) in c.seqs:
                if kd != kind:
                    continue
                for t0 in range(t0s, t0s + ln, 128):
                    k = it % 2
                    it += 1
                    x, sq, h = xt[k], ss[k], hT[k]
                    s.ld(x[:, :], s.xres[t0:t0 + 128, :], ['xres'], [x])
                    s.act(junk[:, :], x[:, :], AF.Square, [x], [junk, sq], accum=sq[:, 0:1])
                    s.rsqrt(sq[:, 1:2], sq[:, 0:1], 1.0 / D, 1e-6, [sq], [sq])
                    s.ts('dve', x[:, :], x[:, :], sq[:, 1:2], None, ALU.mult, None, [x, sq], [x])
                    s.ld(s.xn[t0:t0 + 128, :], x[:, :], [x], [('xn', t0)])
                    for q4 in range(KC // 4):
                        p_ = pt[pi % 2]
                        tm = tmp[pi % 2]
                        pi += 1
                        s.tr([(p_[:, j, :], x[:, (q4 * 4 + j) * 128:(q4 * 4 + j + 1) * 128]) for j in range(4)],
                             s.ident[:, :], [x, s.ident], [p_])
                        gb = s.gcol[:, 1, q4 * 4:q4 * 4 + 4, mc]
                        gb = AP(gb, gb.offset, [gb.ap[0], gb.ap[1], [0, 128]])
                        sb_ = s.modcol[:, 3 * KC + q4 * 4:3 * KC + q4 * 4 + 4, mc]
                        sb_ = AP(sb_, sb_.offset, [sb_.ap[0], sb_.ap[1], [0, 128]])
                        s.tt('dve', tm[:, :, :], p_[:, :, :], gb, ALU.mult, [p_, s.gcol], [tm])
                        s.tt('pool', h[:, q4 * 4:q4 * 4 + 4, :], tm[:, :, :], sb_, ALU.add, [tm, s.modcol], [h])
                    pl_ = pl[k]
                    s.mm(pl_[:, :], [(h[:, kk_, :], wr[:, kk_, :]) for kk_ in range(KC)], [h, wr], [pl_])
                    L_, sm_ = lg[k], sm[k]
                    s.P.op('dve', lambda g_, sm_=sm_, pl_=pl_: g_.tensor_reduce(out=sm_[:, 0:1], in_=pl_[:, :], axis=AX.X,
                                                                              op=ALU.max, negate=True), [pl_], [sm_])
                    s.act(L_[:, :], pl_[:, :], AF.Exp, [pl_, sm_], [L_, sm_], bias=sm_[:, 0:1], accum=sm_[:, 1:2])
                    s.P.op('dve', lambda g_, sm_=sm_: g_.reciprocal(out=sm_[:, 1:2], in_=sm_[:, 1:2]), [sm_], [sm_])
                    s.ts('dve', L_[:, :], L_[:, :], sm_[:, 1:2], None, ALU.mult, None, [L_, sm_], [L_])
                    pa_ = pa[k]
                    s.tr([(pa_[:, :], L_[:, :])], s.ident[:, :], [L_, s.ident], [pa_])
                    s.cp('act', affT[b][:, t0 - t0s:t0 - t0s + 128], pa_[:, :], [pa_], [(affT[b], t0)])
            P.flush()
            s.topk = es
            gk = [s.sb(es, 'rt_gk%d' % b, (NE, cap)) for b in range(c.NS)]
            ik = [s.sb(es, 'rt_ik%d' % b, (NE, cap), U32) for b in range(c.NS)]
            ikf = [s.sb(es, 'rt_ikf%d' % b, (NE, cap)) for b in range(c.NS)]
            for b in range(c.NS):
                cur = affT[b]
                for r in range(cap // 8):
                    g8 = gk[b][:, r * 8:(r + 1) * 8]
                    s.P.op('dve', (lambda g8, cur: lambda g_: g_.max(out=g8, in_=cur[:, :]))(g8, cur), [cur], [gk[b]])
                    s.P.op('dve', (lambda g8, cur, b, r: lambda g_: g_.max_index(out=ik[b][:, r * 8:(r + 1) * 8], in_max=g8,
                                                                           in_values=cur[:, :]))(g8, cur, b, r),
                           [cur, gk[b]], [ik[b]])
                    if r < cap // 8 - 1:
                        s.P.op('dve', (lambda g8, cur, b: lambda g_: g_.match_replace(
                            out=work[b][:, :], in_to_replace=g8, in_values=cur[:, :], imm_value=-1.0))(g8, cur, b),
                            [cur, gk[b]], [work[b]])
                        cur = work[b]
                s.cp('dve', ikf[b][:, :], ik[b][:, :], [ik[b]], [ikf[b]])
                t0s_b = [q for q in c.seqs if q[0] == kind and q[1] == b][0][2]
                s.ts('dve', ikf[b][:, :], ikf[b][:, :], float(t0s_b), None, ALU.add, None, [ikf[b]], [ikf[b]])
            nck = (cap + 127) // 128
            s.gsel = s.gselk[kind]
            s.isel = s.iselk[kind]
            ptk = [s.ps(es, 'rt_ptk%d' % i, (128, NE)) for i in range(2)]
            tg = [s.sb(es, 'rt_tg%d' % i, (128, NE)) for i in range(2)]
            ti = [s.sb(es, 'rt_ti%d' % i, (128, NE), I32) for i in range(2)]
            j = 0
            for b in range(c.NS):
                for ck in range(nck):
                    n = min(128, cap - ck * 128)
                    p_ = ptk[j % 2]
                    s.tr([(p_[0:n, :], gk[b][:, ck * 128:ck * 128 + n])], s.ident[0:NE, 0:NE], [gk[b], s.ident], [p_])
                    s.cp('dve', tg[j % 2][0:n, :], p_[0:n, :], [p_], [tg[j % 2]])
                    s.ld(s.gsel[b, ck * 128:ck * 128 + n, :], tg[j % 2][0:n, :], [tg[j % 2]], [('gsel', b, ck)])
                    j += 1
                    p_ = ptk[j % 2]
                    s.tr([(p_[0:n, :], ikf[b][:, ck * 128:ck * 128 + n])], s.ident[0:NE, 0:NE], [ikf[b], s.ident], [p_])
                    s.cp('dve', ti[j % 2][0:n, :], p_[0:n, :], [p_], [ti[j % 2]])
                    s.ld(s.isel[b, ck * 128:ck * 128 + n, :], ti[j % 2][0:n, :], [ti[j % 2]], [('isel', b, ck)])
                    j += 1
            P.flush()

    def phase_moe(s, kind):
        c, P, l = s.c, s.P, s.l
        KC, D, NE, FF = c.KC, c.D, c.NE, c.FF
        FC = FF // 128
        cap = s.cap
        NS1 = c.NS + 1
        nck = (cap + 127) // 128
        cw = min(cap, 128)
        NTOK = c.NS * cap
        M6 = 6 * D
        seqs = [q for q in c.seqs if q[0] == kind]
        with ExitStack() as es:
            gsel = s.sb(es, 'mo_g', (128, c.NS, nck, NE))
            isel = s.sb(es, 'mo_i', (128, c.NS, nck, NE), I32)
            for b in range(c.NS):
                for ck in range(nck):
                    n = min(128, cap - ck * 128)
                    s.ld(gsel[0:n, b, ck, :], s.gsel[b, ck * 128:ck * 128 + n, :], ['gsel_all'], [gsel])
                    s.ld(isel[0:n, b, ck, :], s.isel[b, ck * 128:ck * 128 + n, :], ['isel_all'], [isel])
            g5 = []
            for (kd, b, t0s, ln, mc) in seqs:
                t = s.sb(es, 'mo_g5%d' % b, (128, D))
                s.ld(t[:, :], AP(s.modrow, (l * NS1 + mc) * M6 + 5 * D, [[0, 128], [1, D]]), ['modrow_all'], [t])
                g5.append(t)
            xs = [s.sb(es, 'mo_xs%d' % i, (128, D)) for i in range(2)]
            xsT = [s.sb(es, 'mo_xT%d' % i, (128, KC, NTOK), BF16) for i in range(2)]
            hid = s.sb(es, 'mo_hid', (128, FC, NTOK), BF16)
            sg = [s.sb(es, 'mo_sg%d' % i, (128, NTOK)) for i in range(2)]
            ys = [s.sb(es, 'mo_ys%d' % i, (128, D)) for i in range(c.NS * nck)]
            tmp = [s.sb(es, 'mo_t%d' % i, (128, 4, 128)) for i in range(2)]
            wb = [s.sb(es, 'mo_w%d' % i, (128, max(KC, FC), 512), BF16) for i in range(3)]
            pt = [s.ps(es, 'mo_pt%d' % i, (128, 4, 128)) for i in range(2)]
            pg = [s.ps(es, 'mo_pg%d' % i, (128, NTOK)) for i in range(2)]
            pu = [s.ps(es, 'mo_pu%d' % i, (128, NTOK)) for i in range(2)]
            pd = [s.ps(es, 'mo_pd%d' % i, (128, 512)) for i in range(2)]
            cnt = {'x': 0, 'p': 0, 'w': 0, 'h': 0, 'y': 0, 'd': 0}
            for e_ in range(NE):
                xT = xsT[e_ % 2]
                for bi, (kd, b, t0s, ln, mc) in enumerate(seqs):
                    for ck in range(nck):
                        n = min(128, cap - ck * 128)
                        x = xs[cnt['x'] % 2]
                        cnt['x'] += 1
                        s.P.dma('pool', (lambda x, n, b, ck, e_, t0s, ln: lambda g_: g_.indirect_dma_start(
                            out=x[0:n, :], out_offset=None, in_=s.xn[:, :],
                            in_offset=bass.IndirectOffsetOnAxis(ap=isel[0:n, b, ck, e_:e_ + 1], axis=0)))(
                            x, n, b, ck, e_, t0s, ln), ['xn_all', isel], [x])
                        c0 = bi * cap + ck * 128
                        for q4 in range(KC // 4):
                            p_ = pt[cnt['p'] % 2]
                            tm = tmp[cnt['p'] % 2]
                            cnt['p'] += 1
                            s.tr([(p_[:, j, 0:n], x[0:n, (q4 * 4 + j) * 128:(q4 * 4 + j + 1) * 128]) for j in range(4)],
                                 s.ident[0:n, 0:n], [x, s.ident], [p_])
                            gb = s.gcol[:, 1, q4 * 4:q4 * 4 + 4, mc]
                            gb = AP(gb, gb.offset, [gb.ap[0], gb.ap[1], [0, n]])
                            sb_ = s.modcol[:, 3 * KC + q4 * 4:3 * KC + q4 * 4 + 4, mc]
                            sb_ = AP(sb_, sb_.offset, [sb_.ap[0], sb_.ap[1], [0, n]])
                            s.tt('dve', tm[:, :, 0:n], p_[:, :, 0:n], gb, ALU.mult, [p_, s.gcol], [tm])
                            s.tt('pool', xT[:, q4 * 4:q4 * 4 + 4, c0:c0 + n], tm[:, :, 0:n], sb_, ALU.add,
                                 [tm, s.modcol], [xT])
                wg = s.W['w_exp_gate'][(l * NE + e_) * D:(l * NE + e_ + 1) * D, :].rearrange("(k p) f -> p k f", p=128)
                wu = s.W['w_exp_up'][(l * NE + e_) * D:(l * NE + e_ + 1) * D, :].rearrange("(k p) f -> p k f", p=128)
                wd = s.W['w_exp_down'][(l * NE + e_) * FF:(l * NE + e_ + 1) * FF, :].rearrange("(k p) d -> p k d", p=128)
                for fq in range(FF // 512):
                    w1 = wb[cnt['w'] % 3]
                    cnt['w'] += 1
                    w2 = wb[cnt['w'] % 3]
                    cnt['w'] += 1
                    s.ld(w1[:, 0:KC, :], wg[:, :, fq * 512:(fq + 1) * 512], ['wfull_e'], [w1])
                    s.ld(w2[:, 0:KC, :], wu[:, :, fq * 512:(fq + 1) * 512], ['wfull_e'], [w2])
                    for j in range(4):
                        pg_ = pg[cnt['h'] % 2]
                        pu_ = pu[cnt['h'] % 2]
                        sg_ = sg[cnt['h'] % 2]
                        cnt['h'] += 1
                        s.mm(pg_[:, :], [(w1[:, kk_, j * 128:(j + 1) * 128], xT[:, kk_, :]) for kk_ in range(KC)], [w1, xT], [pg_])
                        s.mm(pu_[:, :], [(w2[:, kk_, j * 128:(j + 1) * 128], xT[:, kk_, :]) for kk_ in range(KC)], [w2, xT], [pu_])
                        s.act(sg_[:, :], pg_[:, :], AF.Silu, [pg_], [sg_])
                        s.tt('dve', hid[:, fq * 4 + j, :], sg_[:, :], pu_[:, :], ALU.mult, [sg_, pu_], [hid])
                tbs = []
                for bi, (kd, b, t0s, ln, mc) in enumerate(seqs):
                    for ck in range(nck):
                        tbs.append((bi, b, ck, min(128, cap - ck * 128), bi * cap + ck * 128))
                for dq in range(D // 512):
                    w = wb[cnt['w'] % 3]
                    cnt['w'] += 1
                    s.ld(w[:, 0:FC, :], wd[:, :, dq * 512:(dq + 1) * 512], ['wfull_e'], [w])
                    for ti_, (bi, b, ck, n, c0) in enumerate(tbs):
                        y = ys[ti_]
                        p_ = pd[cnt['d'] % 2]
                        cnt['d'] += 1
                        s.mm(p_[0:n, :], [(hid[:, kk_, c0:c0 + n], w[:, kk_, :]) for kk_ in range(FC)], [hid, w], [p_])
                        s.stt('dve', y[0:n, dq * 512:(dq + 1) * 512], p_[0:n, :], gsel[0:n, b, ck, e_:e_ + 1],
                              g5[bi][0:n, dq * 512:(dq + 1) * 512], ALU.mult, ALU.mult, [p_, gsel, g5[bi]], [y])
                for ti_, (bi, b, ck, n, c0) in enumerate(tbs):
                    y = ys[ti_]
                    s.P.dma('pool', (lambda y, n, b, ck, e_: lambda g_: g_.indirect_dma_start(
                        out=s.xres[:, :],
                        out_offset=bass.IndirectOffsetOnAxis(ap=isel[0:n, b, ck, e_:e_ + 1], axis=0),
                        in_=y[0:n, :], in_offset=None, compute_op=ALU.add))(y, n, b, ck, e_),
                        [y, isel], ['xres'])
            P.flush()

    def phase_final(s):
        c, P = s.c, s.P
        D = c.D
        with ExitStack() as es:
            nf = s.sb(es, 'fn_w', (128, D))
            s.ld(nf[:, :], AP(s.I['norm_final'], 0, [[0, 128], [1, D]]), (), [nf])
            xt = [s.sb(es, 'fn_x%d' % i, (128, D)) for i in range(3)]
            junk = s.sb(es, 'fn_j', (128, D))
            ss = [s.sb(es, 'fn_s%d' % i, (128, 2)) for i in range(3)]
            it = 0
            for (kind, b, t0s, ln, mc) in c.seqs:
                if kind != 'l':
                    continue
                for t0 in range(t0s, t0s + ln, 128):
                    x, sq = xt[it % 3], ss[it % 3]
                    it += 1
                    s.ld(x[:, :], s.xres[t0:t0 + 128, :], ['xres'], [x])
                    s.act(junk[:, :], x[:, :], AF.Square, [x], [junk, sq], accum=sq[:, 0:1])
                    s.rsqrt(sq[:, 1:2], sq[:, 0:1], 1.0 / D, 1e-6, [sq], [sq])
                    s.stt('dve', x[:, :], x[:, :], sq[:, 1:2], nf[:, :], ALU.mult, ALU.mult, [x, sq, nf], [x])
                    o0 = t0 - c.NS * c.CTX
                    s.ld(s.out[o0:o0 + 128, :], x[:, :], [x], [('out', o0)])
            P.flush()


def make_in_maps(c, inputs):
    bs = big_shapes(c)
    ss = small_shapes(c)
    consts = host_consts(c)
    maps = []
    big = {n: np.ascontiguousarray(np.asarray(inputs[n], np.float32)).reshape(bs[n]) for n in BIGW}
    small = {}
    for n, shp in ss.items():
        a = np.asarray(inputs[n], np.float32)
        if shp[0] == 0:
            a = np.zeros((1, shp[1]), np.float32)
        small[n] = np.ascontiguousarray(a.reshape(max(shp[0], 1), shp[1]))
    x = np.asarray(inputs['x'], np.float32)
    ctx = np.asarray(inputs['ctx'], np.float32)
    cc = np.asarray(inputs['c'], np.float32)
    for i in range(c.NCORES):
        m = {}
        m['x'] = np.ascontiguousarray(x[i * c.NS:(i + 1) * c.NS]).reshape(c.NS * c.SEQ, c.D)
        m['ctx'] = np.ascontiguousarray(ctx[i * c.NS:(i + 1) * c.NS]).reshape(c.NS * c.CTX, c.D)
        m['c'] = np.ascontiguousarray(cc[i * c.NS:(i + 1) * c.NS])
        for n in BIGW:
            r = bs[n][0] // c.NCORES
            m[n] = big[n][i * r:(i + 1) * r] if c.GATHER else big[n]
        m.update(small)
        m.update(consts)
        maps.append(m)
    return maps


def run(c, inputs, debug_out=None, stop=None):
    b = Builder(c, debug_out, stop)
    nc = b.build()
    maps = make_in_maps(c, inputs)
    res = run_bass_kernel_spmd(nc, maps, core_ids=list(range(c.NCORES)))
    return res


def kernel(**inputs):
    c = Cfg(GATHER=False)
    res = run(c, inputs)
    out = np.stack([r['y'] for r in res.results]).reshape(c.BATCH, c.SEQ, c.D)
    return out.astype(np.float32)
```

```python
import numpy as np
from contextlib import ExitStack
import concourse.bass as bass
import concourse.mybir as mybir
from concourse.bass_utils import run_bass_kernel_spmd

F32 = mybir.dt.float32
BF16 = mybir.dt.bfloat16
U32 = mybir.dt.uint32
I32 = mybir.dt.int32
ALU = mybir.AluOpType
AF = mybir.ActivationFunctionType
AX = mybir.AxisListType


class Cfg:
    def __init__(s, D=2048, SEQ=2048, CTX=256, GRID_W=64, DEPTH=2, NE=16, FF=2048, NCORES=8, BATCH=16, GATHER=True,
                 IPG=1024, IPS=512):
        s.IPG, s.IPS = IPG, IPS
        s.GATHER = GATHER
        s.D, s.SEQ, s.CTX, s.GRID_W, s.DEPTH, s.NE, s.FF = D, SEQ, CTX, GRID_W, DEPTH, NE, FF
        s.NCORES, s.BATCH = NCORES, BATCH
        s.NS = BATCH // NCORES
        s.BW = D // 2
        s.H = s.BW // 64
        s.KVH = s.H // 4
        s.DR, s.IR, s.VR, s.GR = 96, 96, 64, 256
        s.RC = 3 * s.BW + 2 * s.DR + 2 * s.IR + s.GR
        s.KVW = s.KVH * 64
        s.CTXC = s.RC + 2 * s.KVW
        s.QEND = s.CTXC + s.BW
        s.CONVEND = s.QEND + 3 * s.BW
        s.INC = s.CONVEND + 3 * D
        s.NT = s.NS * (s.CTX + s.SEQ)
        s.TALL = s.CTX + s.SEQ
        s.KC = D // 128
        s.BC = s.BW // 128
        s.CAPL = 2 * s.SEQ // NE
        s.CAPC = 2 * s.CTX // NE
        s.IQ = 128 // (s.NS * s.H)
        s.IP = 64 // s.IQ
        s.seqs = [('c', b, b * s.CTX, s.CTX, s.NS) for b in range(s.NS)] + \
                 [('l', b, s.NS * s.CTX + b * s.SEQ, s.SEQ, b) for b in range(s.NS)]


BIGW = ['w_mod', 'w_in', 'w_branch', 'w_out', 'w_exp_gate', 'w_exp_up', 'w_exp_down']
SMALLW = ['b_mod', 'norm_mix', 'norm_ffn', 'shift_mu', 'decay_up', 'decay_bias', 'iclr_up', 'iclr_bias',
          'gate_up', 'vres_down', 'vres_up', 'vres_bias', 'k_k', 'k_a', 'r_k', 'gn_w', 'gn_b', 'conv_w',
          'attn_sink', 'w_router', 'norm_final', 'c_ctx']


def big_shapes(c):
    L = c.DEPTH
    return {'w_mod': (L * c.D, 6 * c.D), 'w_in': (L * c.D, c.INC), 'w_branch': (L * 3 * c.BW, c.D),
            'w_out': (L * c.D, c.D), 'w_exp_gate': (L * c.NE * c.D, c.FF), 'w_exp_up': (L * c.NE * c.D, c.FF),
            'w_exp_down': (L * c.NE * c.FF, c.D)}


def small_shapes(c):
    L = c.DEPTH
    return {'b_mod': (L, 6 * c.D), 'norm_mix': (L, c.D), 'norm_ffn': (L, c.D), 'shift_mu': (L, c.RC),
            'decay_up': (L * 2 * c.DR, c.BW), 'decay_bias': (L * 2, c.BW), 'iclr_up': (L * 2 * c.IR, c.BW),
            'iclr_bias': (L * 2, c.BW), 'gate_up': (L * c.GR, c.BW), 'vres_down': ((L - 1) * c.BW, c.VR),
            'vres_up': ((L - 1) * c.VR, c.BW), 'vres_bias': (L - 1, c.BW), 'k_k': (L, c.BW), 'k_a': (L, c.BW),
            'r_k': (L, c.BW), 'gn_w': (L, c.BW), 'gn_b': (L, c.BW), 'conv_w': (L * 3, c.BW),
            'attn_sink': (L, c.H), 'w_router': (L * c.D, c.NE), 'norm_final': (1, c.D), 'c_ctx': (1, c.D)}


def host_consts(c):
    ident = np.eye(128, dtype=np.float32)
    blk = np.zeros((128, 128), np.float32)
    blk[:64, :64] = 1
    blk[64:, 64:] = 1
    swp = np.zeros((64, 64), np.float32)
    for m in range(64):
        q = m % 32
        swp[m + 16 if q < 16 else m - 16, m] = 1
    k = np.arange(128)[:, None]
    q = np.arange(128)[None, :]
    masks = np.stack([(k >= q), (k <= q)]).astype(np.float32)
    t = np.arange(c.SEQ)
    rows = (t // c.GRID_W).astype(np.float32)
    cols = (t % c.GRID_W).astype(np.float32)
    inv = (10000.0 ** (-np.arange(16, dtype=np.float32) / 16)).astype(np.float32)
    cs = np.zeros((2, 64, c.SEQ), np.float32)
    for n in range(64):
        pos = rows if n < 32 else cols
        m = n % 32
        ang = (pos * inv[m % 16]).astype(np.float32)
        cs[0, n] = np.cos(ang)
        cs[1, n] = -np.sin(ang) if m < 16 else np.sin(ang)
    return {'k_ident': ident, 'k_blk': blk, 'k_swp': swp, 'k_masks': masks.reshape(256, 128),
            'k_rope': cs.reshape(128, c.SEQ)}


class Prog:
    ENG = ('sp', 'act', 'dve', 'pool', 'pe')
    NDS = 8

    def __init__(s, nc):
        s.nc = nc
        s.sems = {}
        s.esem = {e: s._sem('e_' + e) for e in s.ENG}
        s.ecnt = {e: 0 for e in s.ENG}
        s.dsem = {e: [s._sem('d_%s%d' % (e, i)) for i in range(s.NDS)] for e in ('sp', 'act', 'pool')}
        s.dcnt = {e: 0 for e in ('sp', 'act', 'pool')}
        s.waited = {e: {} for e in s.ENG}
        s.last = {}
        s.res = {}
        s.q = {e: [] for e in s.ENG}

    def _sem(s, name):
        s.sems[name] = s.nc.alloc_semaphore(name=name)
        return name

    @staticmethod
    def _key(r):
        if isinstance(r, tuple):
            return (Prog._key(r[0]),) + tuple(r[1:])
        if isinstance(r, str):
            return r
        return id(r)

    def _waits(s, e, reads, writes, extra=()):
        toks = list(extra)
        for r in reads:
            st = s.res.get(s._key(r))
            if st and st['w']:
                toks.append(st['w'])
        for w in writes:
            st = s.res.get(s._key(w))
            if st:
                if st['w']:
                    toks.append(st['w'])
                toks.extend(st['r'].items())
        out = {}
        for sk, v in toks:
            if e == 'pe' and sk == s.esem['pe']:
                continue
            if s.waited[e].get(sk, 0) < v:
                out[sk] = max(out.get(sk, 0), v)
        for sk, v in out.items():
            s.waited[e][sk] = v
        return list(out.items())

    def _commit(s, tok, reads, writes):
        s.last[tok[0]] = tok[1]
        wk = [s._key(w) for w in writes]
        for k in wk:
            s.res[k] = {'w': tok, 'r': {}}
        for r in reads:
            k = s._key(r)
            if k in wk:
                continue
            st = s.res.setdefault(k, {'w': None, 'r': {}})
            st['r'][tok[0]] = max(st['r'].get(tok[0], 0), tok[1])

    def op(s, e, fn, reads=(), writes=()):
        waits = s._waits(e, reads, writes)
        s.ecnt[e] += 1
        tok = (s.esem[e], s.ecnt[e])
        s.q[e].append((waits, fn, tok[0], 1))
        s._commit(tok, reads, writes)

    def dma(s, e, fn, reads=(), writes=(), inc=16):
        n = s.dcnt[e]
        s.dcnt[e] += 1
        slot = s.dsem[e][n % s.NDS]
        prev = s.last.get(slot, 0)
        waits = s._waits(e, reads, writes, extra=[(slot, prev)] if prev else [])
        tok = (slot, prev + inc)
        s.q[e].append((waits, fn, slot, inc))
        s._commit(tok, reads, writes)

    MAGIC = 1000

    def prologue(s):
        nc = s.nc
        s.gate = {e: nc.alloc_semaphore(name='gate_' + e) for e in s.ENG}
        s.done = nc.alloc_semaphore(name='done')
        gate, sems, done, MAGIC = s.gate, s.sems, s.done, s.MAGIC
        with nc.Block() as block:
            def mk(e):
                def f(eng):
                    if e == 'pool':
                        for h in sems.values():
                            eng.sem_clear(h)
                        eng.sem_clear(done)
                        for g in gate.values():
                            eng.sem_clear(g)
                        for g in gate.values():
                            eng.sem_inc(g, MAGIC)
                    eng.wait_op(gate[e], MAGIC, 'sem-eq')
                    eng.sem_inc(gate[e], 1)
                return f
            block.sync(mk('sp'))
            block.scalar(mk('act'))
            block.vector(mk('dve'))
            block.gpsimd(mk('pool'))
            block.tensor(mk('pe'))

    def epilogue(s):
        nc = s.nc
        gate, sems, done = s.gate, s.sems, s.done
        with nc.Block() as block:
            def mk(e):
                def f(eng):
                    if e == 'pool':
                        eng.wait_ge(done, 4)
                        for h in sems.values():
                            eng.sem_clear(h)
                        for g in gate.values():
                            eng.sem_clear(g)
                        eng.sem_clear(done)
                    else:
                        eng.sem_inc(done, 1)
                return f
            block.sync(mk('sp'))
            block.scalar(mk('act'))
            block.vector(mk('dve'))
            block.gpsimd(mk('pool'))
            block.tensor(mk('pe'))

    def flush(s):
        nc = s.nc
        for e in s.ENG:
            waits = []
            for sk, v in s.last.items():
                if s.waited[e].get(sk, 0) < v:
                    s.waited[e][sk] = v
                    waits.append((sk, v))
            s.q[e].append((waits, None, None, 0))
        q = s.q
        sems = s.sems
        with nc.Block() as block:
            def mk(e):
                def f(eng):
                    for waits, fn, sem, inc in q[e]:
                        for sk, v in waits:
                            eng.wait_ge(sems[sk], v)
                        if fn is not None:
                            ins = fn(eng)
                            ins.then_inc(sems[sem], inc)
                return f
            block.sync(mk('sp'))
            block.scalar(mk('act'))
            block.vector(mk('dve'))
            block.gpsimd(mk('pool'))
            block.tensor(mk('pe'))
        s.q = {e: [] for e in s.ENG}
        s.res = {}


def AP(t, off, dims):
    return bass.AP(t.tensor, off, [list(d) for d in dims])


class Builder:
    def __init__(s, c, debug_out=None, stop=None):
        s.c = c
        s.stop = stop
        s.uid = 0
        s.nc = bass.Bass("TRN2", target_bir_lowering=False)
        s.P = Prog(s.nc)
        s.debug_out = debug_out or []

    def dram(s, name, shape, dt=F32, kind="Internal"):
        if kind == "Internal":
            return s.nc.dram_tensor(name, list(shape), dt).ap()
        return s.nc.dram_tensor(name, list(shape), dt, kind=kind).ap()

    def sb(s, es, name, shape, dt=F32):
        s.uid += 1
        return es.enter_context(s.nc.sbuf_tensor('%s_%d' % (name, s.uid), list(shape), dt))

    def ps(s, es, name, shape, dt=F32):
        s.uid += 1
        return es.enter_context(s.nc.psum_tensor('%s_%d' % (name, s.uid), list(shape), dt))

    def ld(s, out, in_, reads, writes, q='sp'):
        s.P.dma(q, lambda e: e.dma_start(out=out, in_=in_), reads, writes)

    def st(s, out, in_, reads, writes):
        s.P.dma('act', lambda e: e.dma_start(out=out, in_=in_), reads, writes)

    def ldnc(s, out, in_, reads, writes, q='sp'):
        s.P.dma(q, lambda e: e.dma_start(out=out, in_=in_, allow_slow_non_contiguous=True), reads, writes)

    def tt(s, e, out, a, b, op, reads, writes):
        s.P.op(e, lambda g: g.tensor_tensor(out=out, in0=a, in1=b, op=op), reads, writes)

    def ts(s, e, out, a, s1, s2, op0, op1, reads, writes):
        if s2 is None:
            s.P.op(e, lambda g: g.tensor_scalar(out=out, in0=a, scalar1=s1, scalar2=None, op0=op0), reads, writes)
        else:
            s.P.op(e, lambda g: g.tensor_scalar(out=out, in0=a, scalar1=s1, scalar2=s2, op0=op0, op1=op1),
                   reads, writes)

    def stt(s, e, out, a, sc, b, op0, op1, reads, writes):
        s.P.op(e, lambda g: g.scalar_tensor_tensor(out=out, in0=a, scalar=sc, in1=b, op0=op0, op1=op1),
               reads, writes)

    def act(s, out, in_, func, reads, writes, bias=None, scale=None, accum=None):
        kw = {}
        if bias is not None:
            kw['bias'] = bias
        if scale is not None:
            kw['scale'] = scale
        if accum is not None:
            kw['accum_out'] = accum
        s.P.op('act', lambda g: g.activation(out=out, in_=in_, func=func, **kw), reads, writes)

    def rsqrt(s, out, in_, mult, add, reads, writes):
        s.act(out, in_, AF.Sqrt, reads, writes, bias=s.cbias(add), scale=mult)
        s.P.op('dve', lambda g: g.reciprocal(out=out, in_=out), writes, writes)

    def cbias(s, val):
        key = float(val)
        if key not in s.cb:
            i = len(s.cb)
            s.cb[key] = i
            s.P.op('pool', (lambda i, key: lambda g: g.memset(s.cbt[:, i:i + 1], key))(i, key), (), [(s.cbt, i)])
        i = s.cb[key]
        return s.cbt[:, i:i + 1]

    def cp(s, e, out, in_, reads, writes):
        if e == 'act':
            s.act(out, in_, AF.Copy, reads, writes)
        else:
            s.P.op(e, lambda g: g.tensor_copy(out=out, in_=in_), reads, writes)

    def red(s, out, in_, reads, writes, negate=False):
        s.P.op('dve', lambda g: g.tensor_reduce(out=out, in_=in_, axis=AX.X, op=ALU.add, negate=negate),
               reads, writes)

    def mm(s, out, pairs, reads, writes, start=True, stop=True):
        def fn(g):
            ins = None
            n = len(pairs)
            for i, (l, r) in enumerate(pairs):
                ins = g.matmul(out, l, r, start=(start and i == 0), stop=(stop and i == n - 1))
            return ins
        s.P.op('pe', fn, reads, writes)

    def tr(s, outs_ins, ident, reads, writes):
        def fn(g):
            ins = None
            for o, i in outs_ins:
                ins = g.transpose(o, i, ident)
            return ins
        s.P.op('pe', fn, reads, writes)

    def memset(s, e, ap, val, writes):
        s.P.op(e, lambda g: g.memset(ap, val), (), writes)

    def build(s):
        c, nc, P = s.c, s.nc, s.P
        L = c.DEPTH
        s.I = {}
        s.I['x'] = s.dram('x', (c.NS * c.SEQ, c.D), kind="ExternalInput")
        s.I['ctx'] = s.dram('ctx', (c.NS * c.CTX, c.D), kind="ExternalInput")
        s.I['c'] = s.dram('c', (c.NS, c.D), kind="ExternalInput")
        bs = big_shapes(c)
        for n in BIGW:
            r, w = bs[n]
            s.I[n + '_sh'] = s.dram(n, (r // c.NCORES if c.GATHER else r, w), kind="ExternalInput")
        for n, shp in small_shapes(c).items():
            s.I[n] = s.dram(n, (max(shp[0], 1), shp[1]), kind="ExternalInput")
        for n, a in host_consts(c).items():
            s.I[n] = s.dram(n, a.shape, kind="ExternalInput")
        s.out = s.dram('y', (c.NS * c.SEQ, c.D), kind="ExternalOutput")
        s.W = {}
        s.Wsh = {}
        for n in BIGW:
            r, w = bs[n]
            s.Wsh[n] = s.dram(n + '_b16s', (r // c.NCORES, w), BF16)
            s.W[n] = s.dram(n + '_b16', (r, w), BF16)
        s.xres = s.dram('xres', (c.NT, c.D))
        s.hT = s.dram('hT', (c.D, c.NT), BF16)
        s.pT = s.dram('pT', (c.INC, c.NT))
        s.strm = {n: s.dram('st_' + n, (c.NS, c.H, c.TALL, 64)) for n in
                  ['w0', 'w1', 'kd0', 'kd1', 'ka0', 'ka1', 'nkk', 'r', 'v']}
        s.ysc = [s.dram('ysc%d' % d, (c.NS, c.H, c.TALL, 64)) for d in range(2)]
        s.sgdT = s.dram('sgdT', (c.GR, c.NT))
        s.vfT = s.dram('vfT', (c.BW, c.NT))
        s.brT = [s.dram('brT%d' % i, (c.BW, c.NT), BF16) for i in range(3)]
        s.xn = s.dram('xn', (c.NT, c.D))
        s.modrow = s.dram('modrow', (L * (c.NS + 1), 6 * c.D))
        s.gselk = {'l': s.dram('gsel_l', (c.NS, c.CAPL, c.NE)), 'c': s.dram('gsel_c', (c.NS, c.CAPC, c.NE))}
        s.iselk = {'l': s.dram('isel_l', (c.NS, c.CAPL, c.NE), I32), 'c': s.dram('isel_c', (c.NS, c.CAPC, c.NE), I32)}

        with ExitStack() as g:
            g.enter_context(nc.allow_low_precision("bf16 matmul operands, fp32 accumulation"))
            s.ident = s.sb(g, 'ident', (128, 128))
            s.blk = s.sb(g, 'blk', (128, 128))
            s.swp = s.sb(g, 'swp', (64, 64))
            s.masks = s.sb(g, 'masks', (128, 2, 128), BF16)
            s.masks_f = s.sb(g, 'masks_f', (128, 2, 128))
            s.identb = s.sb(g, 'identb', (128, 128), BF16)
            s.onesb = s.sb(g, 'onesb', (128, 64), BF16)
            s.modcol = s.sb(g, 'modcol', (128, 6 * c.KC, c.NS + 1))
            s.ncol = s.sb(g, 'ncol', (128, 2, c.KC))
            s.gcol = s.sb(g, 'gcol', (128, 2, c.KC, c.NS + 1))
            s.cbt = s.sb(g, 'cbt', (128, 8))
            s.cb = {}
            s.dbg = {}
            s.scr = {'pT': s.pT, 'hT': s.hT, 'xres': s.xres, 'modrow': s.modrow, 'sgdT': s.sgdT, 'vfT': s.vfT,
                     'xn': s.xn, 'ysc0': s.ysc[0], 'ysc1': s.ysc[1], 'brT0': s.brT[0], 'brT1': s.brT[1],
                     'brT2': s.brT[2]}
            for n_ in s.strm:
                s.scr['st_' + n_] = s.strm[n_]
            for k_ in 'lc':
                s.scr['gsel_' + k_] = s.gselk[k_]
                s.scr['isel_' + k_] = s.iselk[k_]
            for n_ in s.debug_out:
                a_ = s.scr[n_]
                s.dbg[n_] = s.dram('dbg_' + n_, a_.shape, a_.dtype, kind="ExternalOutput")
            s.P.prologue()
            s.phase_init()
            done = False
            for l in range(L):
                s.l = l
                s.last = (l == L - 1)
                phases = [('mod', s.phase_mod), ('norm1', s.phase_norm1), ('inproj', s.phase_inproj),
                          ('rwkv_pre', s.phase_rwkv_pre), ('scan', s.phase_scan), ('rwkv_post', s.phase_rwkv_post),
                          ('attn', s.phase_attn), ('conv', s.phase_conv), ('merge', s.phase_merge)]
                for kind in (['l'] if s.last else ['l', 'c']):
                    phases.append(('router_' + kind, (lambda k: lambda: s.phase_router(k))(kind)))
                    phases.append(('moe_' + kind, (lambda k: lambda: s.phase_moe(k))(kind)))
                for name, fn in phases:
                    fn()
                    if s.stop == (l, name):
                        done = True
                        break
                if done:
                    break
            if not done:
                s.phase_final()
            for n_ in s.dbg:
                s.ld(s.dbg[n_], s.scr[n_], ['dbgsrc'], [('dbg', n_)])
            s.P.flush()
            s.P.epilogue()
        return nc

    def phase_init(s):
        c, P = s.c, s.P
        I = s.I
        s.ld(s.ident[:, :], I['k_ident'][:, :], (), [s.ident])
        s.ld(s.blk[:, :], I['k_blk'][:, :], (), [s.blk])
        s.ld(s.swp[:, :], I['k_swp'][:, :], (), [s.swp])
        s.ld(s.masks_f[:, :, :], I['k_masks'].rearrange("(m k) q -> k m q", m=2), (), [s.masks_f])
        s.cp('dve', s.masks[:, :, :], s.masks_f[:, :, :], [s.masks_f], [s.masks])
        s.cp('dve', s.identb[:, :], s.ident[:, :], [s.ident], [s.identb])
        s.memset('dve', s.onesb[:, :], 1.0, [s.onesb])
        nct = c.NS * c.CTX
        s.ld(s.xres[0:nct, :], I['ctx'][:, :], (), ['xres'])
        R = c.NS * c.SEQ
        step = max(R // 4, 128)
        for r0 in range(0, R, step):
            s.ld(s.xres[nct + r0:nct + r0 + step, :], I['x'][r0:r0 + step, :], (), [('xres', r0)])
        bs = big_shapes(c)
        for n in BIGW:
            r = bs[n][0] // c.NCORES if c.GATHER else bs[n][0]
            step = max(r // (4 if c.GATHER else 32), 1)
            dstw = s.Wsh[n] if c.GATHER else s.W[n]
            for r0 in range(0, r, step):
                s.P.dma('pool', (lambda dstw, n, r0, step: lambda e: e.dma_start(
                    out=dstw[r0:r0 + step, :], in_=I[n + '_sh'][r0:r0 + step, :]))(dstw, n, r0, step),
                    (), [('wsh', n, r0)])
        P.flush()
        for n in (BIGW if c.GATHER else []):
            s.P.dma('pool', (lambda n: lambda e: e.collective_compute(
                "AllGather", ALU.bypass, replica_groups=[list(range(c.NCORES))],
                ins=[s.Wsh[n].opt()], outs=[s.W[n].opt()]))(n), (), [('wfull', n)], inc=1)
        P.flush()

    def phase_mod(s):
        c, P, l = s.c, s.P, s.l
        NS1 = c.NS + 1
        M6 = 6 * c.D
        with ExitStack() as es:
            cT = s.sb(es, 'cT', (128, c.KC, NS1))
            cTb = s.sb(es, 'cTb', (128, c.KC, NS1), BF16)
            for sc in range(c.NS):
                s.ldnc(cT[:, :, sc], s.I['c'][sc:sc + 1, :].rearrange("s (k p) -> p (s k)", p=128), [cT], [cT])
            s.ldnc(cT[:, :, c.NS], s.I['c_ctx'].rearrange("s (k p) -> p (s k)", p=128), [cT], [cT])
            s.act(cTb[:, :, :], cT[:, :, :], AF.Silu, [cT], [cTb])
            brow = s.sb(es, 'brow', (NS1, M6))
            s.ld(brow[:, :], AP(s.I['b_mod'], l * M6, [[0, NS1], [1, M6]]), (), [brow])
            mrow = s.sb(es, 'mrow', (NS1, M6))
            wb = [s.sb(es, 'wmod%d' % i, (128, c.KC, 512), BF16) for i in range(2)]
            pm = [s.ps(es, 'pmod%d' % i, (NS1, 512)) for i in range(2)]
            wsrc = s.W['w_mod'][l * c.D:(l + 1) * c.D, :].rearrange("(k p) n -> p k n", p=128)
            ng = M6 // 512
            for gi in range(ng):
                w = wb[gi % 2]
                p_ = pm[gi % 2]
                s.ld(w[:, :, :], wsrc[:, :, gi * 512:(gi + 1) * 512], (), [w])
                s.mm(p_[:, :], [(cTb[:, k, :], w[:, k, :]) for k in range(c.KC)], [cTb, w], [p_])
                s.tt('dve', mrow[:, gi * 512:(gi + 1) * 512], p_[:, :], brow[:, gi * 512:(gi + 1) * 512], ALU.add,
                     [p_, brow], [(mrow, gi)])
            allm = [(mrow, gi) for gi in range(ng)]
            s.ld(s.modrow[l * NS1:(l + 1) * NS1, :], mrow[:, :], allm, [('modrow', l)])
            pc = s.ps(es, 'pcol', (128, 6 * c.KC, NS1))
            nch = 6 * c.KC
            s.tr([(pc[:, j, :], mrow[:, j * 128:(j + 1) * 128]) for j in range(nch)], s.ident[0:NS1, 0:NS1],
                 allm + [s.ident], [pc])
            s.cp('dve', s.modcol[:, :, :], pc[:, :, :], [pc], [s.modcol])
            s.ldnc(s.ncol[:, 0, :], s.I['norm_mix'][l:l + 1, :].rearrange("o (k p) -> p (o k)", p=128), (), [s.ncol])
            s.ldnc(s.ncol[:, 1, :], s.I['norm_ffn'][l:l + 1, :].rearrange("o (k p) -> p (o k)", p=128), [s.ncol],
                   [s.ncol])
            KC = c.KC
            for j, mi in ((0, 1), (1, 4)):
                for sc in range(NS1):
                    s.stt('dve', s.gcol[:, j, :, sc], s.modcol[:, mi * KC:(mi + 1) * KC, sc], 1.0, s.ncol[:, j, :],
                          ALU.add, ALU.mult, [s.modcol, s.ncol], [s.gcol])
            P.flush()

    def norm_tile(s, es_t, x_ap, tag):
        pass

    def phase_norm1(s):
        c, P, l = s.c, s.P, s.l
        KC = c.KC
        with ExitStack() as es:
            xt = [s.sb(es, 'n1x%d' % i, (128, c.D)) for i in range(2)]
            junk = s.sb(es, 'n1j', (128, c.D))
            ss = [s.sb(es, 'n1s%d' % i, (128, 2)) for i in range(2)]
            hst = [s.sb(es, 'n1h%d' % i, (128, KC, 128), BF16) for i in range(2)]
            tmp = [s.sb(es, 'n1t%d' % i, (128, 4, 128)) for i in range(2)]
            pt = [s.ps(es, 'n1p%d' % i, (128, 4, 128)) for i in range(4)]
            it = 0
            pi = 0
            for (kind, b, t0s, ln, mc) in c.seqs:
                for t0 in range(t0s, t0s + ln, 128):
                    x, sq, h = xt[it % 2], ss[it % 2], hst[it % 2]
                    s.ld(x[:, :], s.xres[t0:t0 + 128, :], ['xres'], [x])
                    s.act(junk[:, :], x[:, :], AF.Square, [x], [junk, sq], accum=sq[:, 0:1])
                    s.rsqrt(sq[:, 1:2], sq[:, 0:1], 1.0 / c.D, 1e-6, [sq], [sq])
                    s.ts('dve', x[:, :], x[:, :], sq[:, 1:2], None, ALU.mult, None, [x, sq], [x])
                    for q4 in range(KC // 4):
                        p_ = pt[pi % 4]
                        tm = tmp[pi % 2]
                        pi += 1
                        s.tr([(p_[:, j, :], x[:, (q4 * 4 + j) * 128:(q4 * 4 + j + 1) * 128]) for j in range(4)],
                             s.ident[:, :], [x, s.ident], [p_])
                        gb = s.gcol[:, 0, q4 * 4:q4 * 4 + 4, mc]
                        gb = AP(gb, gb.offset, [gb.ap[0], gb.ap[1], [0, 128]])
                        sb_ = s.modcol[:, q4 * 4:q4 * 4 + 4, mc]
                        sb_ = AP(sb_, sb_.offset, [sb_.ap[0], sb_.ap[1], [0, 128]])
                        s.tt('dve', tm[:, :, :], p_[:, :, :], gb, ALU.mult, [p_, s.gcol], [tm])
                        s.tt('pool', h[:, q4 * 4:q4 * 4 + 4, :], tm[:, :, :], sb_, ALU.add, [tm, s.modcol], [h])
                    s.st(s.hT[:, t0:t0 + 128].rearrange("(k p) t -> p k t", p=128), h[:, :, :], [h], [('hT', t0)])
                    it += 1
            P.flush()

    def phase_inproj(s):
        c, P, l = s.c, s.P, s.l
        KC = c.KC
        G, SW = c.IPG, c.IPS
        nchunk = c.INC // 128
        sig0 = c.CONVEND // 128
        with ExitStack() as es:
            hb = [s.sb(es, 'iph%d' % i, (128, KC, G), BF16) for i in range(2)]
            wb = [s.sb(es, 'ipw%d' % i, (128, KC, 512), BF16) for i in range(3)]
            ob = [s.sb(es, 'ipo%d' % i, (128, 4, SW)) for i in range(2)]
            pp = [s.ps(es, 'ipp%d' % i, (128, SW)) for i in range(4)]
            wsrc = s.W['w_in'][l * c.D:(l + 1) * c.D, :].rearrange("(k p) n -> p k n", p=128)
            wi = 0
            pi = 0
            oi = 0
            nctx = c.NS * c.CTX
            groups = [(t, min(G, nctx - t)) for t in range(0, nctx, G)] + \
                     [(t, min(G, c.NT - t)) for t in range(nctx, c.NT, G)]
            for gi, (t0, n) in enumerate(groups):
                h = hb[gi % 2]
                s.ld(h[:, :, 0:n], s.hT[:, t0:t0 + n].rearrange("(k p) t -> p k t", p=128), ['hT_all'], [h])
                nch = nchunk
                if s.last and t0 + n <= nctx:
                    nch = c.CTXC // 128
                for c0 in range(0, nch, 4):
                    n4 = min(4, nch - c0)
                    w = wb[wi % 3]
                    wi += 1
                    s.ld(w[:, :, 0:n4 * 128], wsrc[:, :, c0 * 128:(c0 + n4) * 128], ['wfull_in'], [w])
                    for u0 in range(0, n, SW):
                        un = min(SW, n - u0)
                        o = ob[oi % 2]
                        oi += 1
                        for j in range(n4):
                            p_ = pp[pi % 4]
                            pi += 1
                            s.mm(p_[:, 0:un], [(w[:, k, j * 128:(j + 1) * 128], h[:, k, u0:u0 + un]) for k in range(KC)],
                                 [w, h], [p_])
                            if c0 + j >= sig0:
                                s.act(o[:, j, 0:un], p_[:, 0:un], AF.Sigmoid, [p_], [(o, j)])
                            elif (c0 + j) % 2 == 0:
                                s.cp('act', o[:, j, 0:un], p_[:, 0:un], [p_], [(o, j)])
                            else:
                                s.cp('dve', o[:, j, 0:un], p_[:, 0:un], [p_], [(o, j)])
                        s.st(s.pT[c0 * 128:(c0 + n4) * 128, t0 + u0:t0 + u0 + un].rearrange("(j p) t -> p j t", p=128),
                             o[:, 0:n4, 0:un], [(o, j) for j in range(n4)],
                             [('pT', c0, t0 + u0)] + [(o, j) for j in range(n4)])
            P.flush()

    def load_halo(s, tile, n, row0, t0, seg, s0, s1):
        lo = max(t0 - 1, s0)
        hi = min(t0 + seg + 1, s1)
        a = lo - (t0 - 1)
        if a > 0:
            s.memset('pool', tile[0:n, 0:1], 0.0, [tile])
        if hi < t0 + seg + 1:
            s.memset('pool', tile[0:n, seg + 1:seg + 2], 0.0, [tile])
        s.ld(tile[0:n, a:a + (hi - lo)], s.pT[row0:row0 + n, lo:hi], ['pT_all'], [tile])

    def colvec(s, tile_col, vec_ap_1d_len_n):
        pass

    def phase_rwkv_pre(s):
        c, P, l = s.c, s.P, s.l
        I = s.I
        BC = c.BC
        BW = c.BW
        with ExitStack() as es:
            blocks = []
            for j in range(3 * BC):
                blocks.append((j * 128, 128))
            base = 3 * BW
            for j in range(2):
                blocks.append((base + j * c.DR, c.DR))
            base += 2 * c.DR
            for j in range(2):
                blocks.append((base + j * c.IR, c.IR))
            base += 2 * c.IR
            for j in range(c.GR // 128):
                blocks.append((base + j * 128, 128))
            NB = len(blocks)
            mu = s.sb(es, 'mu', (128, NB))
            omm = s.sb(es, 'omm', (128, NB))
            hmu = s.sb(es, 'hmu', (128, NB))
            s.memset('dve', mu[:, :], 0.0, [mu])
            for j, (r0, n) in enumerate(blocks):
                s.ldnc(mu[0:n, j:j + 1], I['shift_mu'][l:l + 1, r0:r0 + n].rearrange("o n -> n o"), [mu], [mu])
            s.ts('dve', omm[:, :], mu[:, :], -1.0, 1.0, ALU.mult, ALU.add, [mu], [omm])
            s.ts('dve', hmu[:, :], mu[:, :], 0.5, None, ALU.mult, None, [mu], [hmu])
            pc = s.sb(es, 'pcols', (128, 8, BC))
            def colload(idx, src2d_row):
                s.ldnc(pc[:, idx, :], src2d_row.rearrange("o (k p) -> p (o k)", p=128), [pc], [pc])
            s.memset('dve', pc[:, :, :], 0.0, [pc])
            colload(0, I['k_k'][l:l + 1, :])
            colload(1, I['k_a'][l:l + 1, :])
            for d in range(2):
                colload(3 + d, I['decay_bias'][2 * l + d:2 * l + d + 1, :])
                colload(5 + d, I['iclr_bias'][2 * l + d:2 * l + d + 1, :])
            if l > 0:
                colload(7, I['vres_bias'][l - 1:l, :])
            s.ts('dve', pc[:, 2, :], pc[:, 1, :], -1.0, 1.0, ALU.mult, ALU.add, [pc], [pc])
            dup = [s.sb(es, 'dup%d' % d, (c.DR, BW)) for d in range(2)]
            iup = [s.sb(es, 'iup%d' % d, (c.IR, BW)) for d in range(2)]
            for d in range(2):
                s.ld(dup[d][:, :], I['decay_up'][(2 * l + d) * c.DR:(2 * l + d + 1) * c.DR, :], (), [dup[d]])
                s.ld(iup[d][:, :], I['iclr_up'][(2 * l + d) * c.IR:(2 * l + d + 1) * c.IR, :], (), [iup[d]])
            if l > 0:
                vdn = s.sb(es, 'vdn', (128, BC, c.VR))
                vup = s.sb(es, 'vup', (c.VR, BW))
                s.ld(vdn[:, :, :], I['vres_down'][(l - 1) * BW:l * BW, :].rearrange("(k p) r -> p k r", p=128), (), [vdn])
                s.ld(vup[:, :], I['vres_up'][(l - 1) * c.VR:l * c.VR, :], (), [vup])
            SEG = 512
            NTB = 4
            Pin = [s.sb(es, 'rpP%d' % i, (128, SEG + 2)) for i in range(3)]
            tsum = [s.sb(es, 'rpT%d' % i, (128, SEG)) for i in range(2)]
            wdT = [s.sb(es, 'wdT%d' % d, (c.DR, SEG)) for d in range(2)]
            adT = [s.sb(es, 'adT%d' % d, (c.IR, SEG)) for d in range(2)]
            sg = [s.sb(es, 'sg%d' % j, (128, SEG)) for j in range(c.GR // 128)]
            vall = s.sb(es, 'vall', (128, BC, SEG))
            rt = s.sb(es, 'rt', (128, SEG))
            kt = s.sb(es, 'kt', (128, SEG))
            kh = s.sb(es, 'kh', (128, SEG))
            sq = s.sb(es, 'sqk', (128, SEG))
            kk = s.sb(es, 'kk', (128, SEG))
            nkk = s.sb(es, 'nkk', (128, SEG))
            wt = [s.sb(es, 'wt%d' % d, (128, SEG)) for d in range(2)]
            at = [s.sb(es, 'at%d' % d, (128, SEG)) for d in range(2)]
            kdt = [s.sb(es, 'kdt%d' % d, (128, SEG)) for d in range(2)]
            kat = [s.sb(es, 'kat%d' % d, (128, SEG)) for d in range(2)]
            vf = s.sb(es, 'vf', (128, SEG))
            vg = s.sb(es, 'vg', (128, SEG))
            vdT = s.sb(es, 'vdT', (c.VR, SEG))
            stg = [s.sb(es, 'stg%d' % i, (128, NTB, 128)) for i in range(3)]
            pA = [s.ps(es, 'rpA%d' % i, (128, SEG)) for i in range(3)]
            pTr = [s.ps(es, 'rpTr%d' % i, (128, NTB, 128)) for i in range(3)]
            pV = s.ps(es, 'rpV', (c.VR, SEG))
            cnt = {'p': 0, 't': 0, 'a': 0, 'tr': 0}

            def shift(dst, blk, t0, seg, s0, s1, res=None):
                res = res if res is not None else dst
                r0, n = blocks[blk]
                Pt = Pin[cnt['p'] % 3]
                cnt['p'] += 1
                ts_ = tsum[cnt['t'] % 2]
                cnt['t'] += 1
                s.load_halo(Pt, n, r0, t0, seg, s0, s1)
                s.tt('pool', ts_[0:n, 0:seg], Pt[0:n, 0:seg], Pt[0:n, 2:seg + 2], ALU.add, [Pt], [ts_])
                s.act(dst[0:n, 0:seg], Pt[0:n, 1:seg + 1], AF.Copy, [Pt, omm], [res], scale=omm[0:n, blk:blk + 1])
                s.stt('dve', dst[0:n, 0:seg], ts_[0:n, 0:seg], hmu[0:n, blk:blk + 1], dst[0:n, 0:seg],
                      ALU.mult, ALU.add, [ts_, res, hmu], [res])

            def emit_stream(name, tile, b, ch, tall0, seg, res=None):
                res = res if res is not None else tile
                ntb = seg // 128
                p_ = pTr[cnt['tr'] % 3]
                st = stg[cnt['tr'] % 3]
                cnt['tr'] += 1
                s.tr([(p_[:, j, :], tile[:, j * 128:(j + 1) * 128]) for j in range(ntb)], s.ident[:, :],
                     [res, s.ident], [p_])
                s.cp('act' if cnt['tr'] % 2 else 'dve', st[:, 0:ntb, :], p_[:, 0:ntb, :], [p_], [st])
                for tb in range(ntb):
                    dst = s.strm[name][b, 2 * ch:2 * ch + 2, tall0 + tb * 128:tall0 + (tb + 1) * 128, :].rearrange(
                        "h p j -> p h j")
                    src = st[:, tb, :].rearrange("p (h j) -> p h j", j=64)
                    s.st(dst, src, [st], [('strm', name, b, ch, tall0, tb)])

            for (kind, b, t0s, ln, mc) in c.seqs:
                tallb = 0 if kind == 'c' else c.CTX
                for t0 in range(t0s, t0s + ln, SEG):
                    seg = min(SEG, t0s + ln - t0)
                    tall0 = tallb + (t0 - t0s)
                    a = (t0, seg, t0s, t0s + ln)
                    for d in range(2):
                        shift(wdT[d], 3 * BC + d, *a)
                        s.act(wdT[d][:, 0:seg], wdT[d][:, 0:seg], AF.Tanh, [wdT[d]], [wdT[d]])
                        shift(adT[d], 3 * BC + 2 + d, *a)
                    for j in range(c.GR // 128):
                        shift(sg[j], 3 * BC + 4 + j, *a)
                        s.act(sg[j][:, 0:seg], sg[j][:, 0:seg], AF.Sigmoid, [sg[j]], [sg[j]])
                        s.st(s.sgdT[j * 128:(j + 1) * 128, t0:t0 + seg], sg[j][:, 0:seg], [sg[j]], [('sgdT', j, t0)])
                    for ch in range(BC):
                        shift(vall[:, ch, :], 2 * BC + ch, *a, res=vall)
                    if l == 0:
                        for ch in range(BC):
                            s.st(s.vfT[ch * 128:(ch + 1) * 128, t0:t0 + seg], vall[:, ch, 0:seg], [vall],
                                 [('vfT', ch, t0)])
                    else:
                        s.mm(pV[:, 0:seg], [(vdn[:, ch, :], vall[:, ch, 0:seg]) for ch in range(BC)], [vdn, vall], [pV])
                        s.cp('dve', vdT[:, 0:seg], pV[:, 0:seg], [pV], [vdT])
                        for ch in range(BC):
                            p_ = pA[cnt['a'] % 3]
                            cnt['a'] += 1
                            s.mm(p_[:, 0:seg], [(vup[:, ch * 128:(ch + 1) * 128], vdT[:, 0:seg])], [vup, vdT], [p_])
                            s.act(vg[:, 0:seg], p_[:, 0:seg], AF.Sigmoid, [p_, pc], [vg], bias=pc[:, 7, ch:ch + 1])
                            s.ld(vf[:, 0:seg], s.vfT[ch * 128:(ch + 1) * 128, t0:t0 + seg], ['vfT_all'], [vf])
                            s.tt('dve', vf[:, 0:seg], vf[:, 0:seg], vall[:, ch, 0:seg], ALU.subtract, [vf, vall], [vf])
                            s.tt('dve', vf[:, 0:seg], vf[:, 0:seg], vg[:, 0:seg], ALU.mult, [vf, vg], [vf])
                            s.tt('dve', vall[:, ch, 0:seg], vall[:, ch, 0:seg], vf[:, 0:seg], ALU.add, [vf, vall], [vall])
                    for ch in range(BC):
                        shift(rt, ch, *a)
                        shift(kt, BC + ch, *a)
                        s.ts('dve', kh[:, 0:seg], kt[:, 0:seg], pc[:, 0, ch:ch + 1], None, ALU.mult, None, [kt, pc], [kh])
                        s.tt('pool', sq[:, 0:seg], kh[:, 0:seg], kh[:, 0:seg], ALU.mult, [kh], [sq])
                        p_ = pA[cnt['a'] % 3]
                        cnt['a'] += 1
                        s.mm(p_[:, 0:seg], [(s.blk[:, :], sq[:, 0:seg])], [s.blk, sq], [p_])
                        s.ts('dve', sq[:, 0:seg], p_[:, 0:seg], 1e-24, None, ALU.max, None, [p_], [sq])
                        s.rsqrt(sq[:, 0:seg], sq[:, 0:seg], 1.0, 0.0, [sq], [sq])
                        s.tt('dve', kk[:, 0:seg], kh[:, 0:seg], sq[:, 0:seg], ALU.mult, [kh, sq], [kk])
                        s.ts('pool', nkk[:, 0:seg], kk[:, 0:seg], -1.0, None, ALU.mult, None, [kk], [nkk])
                        for d in range(2):
                            p_ = pA[cnt['a'] % 3]
                            cnt['a'] += 1
                            s.mm(p_[:, 0:seg], [(dup[d][:, ch * 128:(ch + 1) * 128], wdT[d][:, 0:seg])],
                                 [dup[d], wdT[d]], [p_])
                            s.act(wt[d][:, 0:seg], p_[:, 0:seg], AF.Sigmoid, [p_, pc], [wt[d]],
                                  bias=pc[:, 3 + d, ch:ch + 1])
                            s.act(wt[d][:, 0:seg], wt[d][:, 0:seg], AF.Exp, [wt[d]], [wt[d]],
                                  scale=-0.6065306597126334)
                            p_ = pA[cnt['a'] % 3]
                            cnt['a'] += 1
                            s.mm(p_[:, 0:seg], [(iup[d][:, ch * 128:(ch + 1) * 128], adT[d][:, 0:seg])],
                                 [iup[d], adT[d]], [p_])
                            s.act(at[d][:, 0:seg], p_[:, 0:seg], AF.Sigmoid, [p_, pc], [at[d]],
                                  bias=pc[:, 5 + d, ch:ch + 1])
                            s.ts('dve', kdt[d][:, 0:seg], at[d][:, 0:seg], pc[:, 1, ch:ch + 1], pc[:, 2, ch:ch + 1],
                                 ALU.mult, ALU.add, [at[d], pc], [kdt[d]])
                            s.tt('dve', kdt[d][:, 0:seg], kdt[d][:, 0:seg], kt[:, 0:seg], ALU.mult, [kdt[d], kt],
                                 [kdt[d]])
                            s.tt('pool', kat[d][:, 0:seg], kk[:, 0:seg], at[d][:, 0:seg], ALU.mult, [kk, at[d]],
                                 [kat[d]])
                            emit_stream('w%d' % d, wt[d], b, ch, tall0, seg)
                            emit_stream('kd%d' % d, kdt[d], b, ch, tall0, seg)
                            emit_stream('ka%d' % d, kat[d], b, ch, tall0, seg)
                        emit_stream('nkk', nkk, b, ch, tall0, seg)
                        emit_stream('r', rt, b, ch, tall0, seg)
                        emit_stream('v', vall[:, ch, :], b, ch, tall0, seg, res=vall)
            P.flush()

    def phase_scan(s):
        c, P = s.c, s.P
        TC = 16
        IQ, IP, H, NS = c.IQ, c.IP, c.H, c.NS
        NBH = NS * H
        names = ['nkk', 'ka', 'w', 'kd', 'r']
        with ExitStack() as es:
            S = [s.sb(es, 'S%d' % i, (128, 2, IP, 64)) for i in range(2)]
            t1 = s.sb(es, 'sc_t1', (128, 2, IP, 64))
            t2 = s.sb(es, 'sc_t2', (128, 2, IP, 64))
            Sw = s.sb(es, 'sc_sw', (128, 2, IP, 64))
            t3 = [s.sb(es, 'sc_t3%d' % i, (128, 2, IP, 64)) for i in range(2)]
            t4 = [s.sb(es, 'sc_t4%d' % i, (128, 2, IP, 64)) for i in range(2)]
            sa = s.sb(es, 'sc_sa', (128, 2, IP))
            st = {n: [s.sb(es, 'sc_%s%d' % (n, i), (128, 2, TC, 64)) for i in range(2)] for n in names}
            vt = [s.sb(es, 'sc_v%d' % i, (128, 2, TC, IP)) for i in range(2)]
            yb = [s.sb(es, 'sc_y%d' % i, (128, 2, TC, IP)) for i in range(2)]
            for hf in range(2):
                s.memset('dve', S[0][:, hf, :, :], 0.0, [(S[0], hf)])
            cur = 0
            step = 0
            ci = 0
            pending = []
            stores = []

            def flush_pending(which):
                for fn in pending:
                    fn(which)

            for (tb0, T) in ((0, c.CTX), (c.CTX, c.SEQ)):
                for ck in range(T // TC):
                    k = ci % 2
                    ci += 1
                    tf = tb0 + ck * TC
                    tr_ = tb0 + T - (ck + 1) * TC
                    for n in names:
                        for d in range(2):
                            tt0 = tf if d == 0 else tr_
                            nm = n if n in ('nkk', 'r') else '%s%d' % (n, d)
                            src = s.strm[nm][:, :, tt0:tt0 + TC, :].rearrange("b h t j -> (b h) t j")
                            for iq in range(IQ):
                                s.ld(st[n][k][iq * NBH:(iq + 1) * NBH, d, :, :], src, ['strm_all'], [(st[n][k], d)])
                    for d in range(2):
                        tt0 = tf if d == 0 else tr_
                        for iq in range(IQ):
                            src = s.strm['v'][:, :, tt0:tt0 + TC, iq * IP:(iq + 1) * IP].rearrange(
                                "b h t j -> (b h) t j")
                            s.ldnc(vt[k][iq * NBH:(iq + 1) * NBH, d, :, :], src, ['strm_all'], [(vt[k], d)])
                    y = yb[k]
                    for sp in range(TC):
                        So, Sn = S[cur], S[1 - cur]
                        T3 = t3[step % 2]
                        T4 = t4[step % 2]

                        def jb(tile, hf):
                            a = tile[:, hf, sp if hf == 0 else TC - 1 - sp, :]
                            return AP(a, a.offset, [a.ap[0], [0, IP], [1, 64]])

                        def ib(tile, hf):
                            a = tile[:, hf, sp if hf == 0 else TC - 1 - sp, :]
                            return AP(a, a.offset, [a.ap[0], [1, IP], [0, 64]])

                        def jb2(tile):
                            a = tile[:, 0, sp, :]
                            return AP(a, a.offset, [a.ap[0], [(2 * TC - 1 - 2 * sp) * 64, 2], [0, IP], [1, 64]])

                        def ib2(tile):
                            a = tile[:, 0, sp, :]
                            return AP(a, a.offset, [a.ap[0], [(2 * TC - 1 - 2 * sp) * IP, 2], [1, IP], [0, 64]])

                        s.tt('pool', T3[:, :, :, :], ib2(vt[k]), jb2(st['kd'][k]), ALU.mult,
                             [(vt[k], 0), (vt[k], 1), (st['kd'][k], 0), (st['kd'][k], 1)], [(T3, 0), (T3, 1)])
                        for hf in range(2):
                            s.tt('pool', Sw[:, hf, :, :], So[:, hf, :, :], jb(st['w'][k], hf), ALU.mult,
                                 [(So, hf), (st['w'][k], hf)], [(Sw, hf)])
                        for hf in range(2):
                            s.tt('dve', t1[:, hf, :, :], So[:, hf, :, :], jb(st['nkk'][k], hf), ALU.mult,
                                 [(So, hf), (st['nkk'][k], hf)], [(t1, hf)])
                        for hf in range(2):
                            s.red(sa[:, hf, :], t1[:, hf, :, :], [(t1, hf)], [(sa, hf)])
                        flush_pending('pool')
                        for hf in range(2):
                            a = sa[:, hf, :]
                            sab = AP(a, a.offset, [a.ap[0], [1, IP], [0, 64]])
                            s.tt('dve', t2[:, hf, :, :], sab, jb(st['ka'][k], hf), ALU.mult,
                                 [(sa, hf), (st['ka'][k], hf)], [(t2, hf)])
                        for hf in range(2):
                            s.tt('dve', Sn[:, hf, :, :], Sw[:, hf, :, :], t2[:, hf, :, :], ALU.add,
                                 [(Sw, hf), (t2, hf)], [(Sn, hf)])
                        for hf in range(2):
                            s.tt('dve', Sn[:, hf, :, :], Sn[:, hf, :, :], T3[:, hf, :, :], ALU.add,
                                 [(Sn, hf), (T3, hf)], [(Sn, hf)])
                        flush_pending('dve')
                        pending.clear()
                        for fn in stores:
                            fn()
                        stores.clear()

                        def mk(Sn, T4, y, k, sp):
                            def fn(which):
                                if which == 'pool':
                                    a = st['r'][k][:, 0, sp, :]
                                    rb = AP(a, a.offset, [a.ap[0], [(2 * TC - 1 - 2 * sp) * 64, 2], [0, IP], [1, 64]])
                                    s.tt('pool', T4[:, :, :, :], Sn[:, :, :, :], rb, ALU.mult,
                                         [(Sn, 0), (Sn, 1), (st['r'][k], 0), (st['r'][k], 1)], [(T4, 0), (T4, 1)])
                                else:
                                    a = y[:, 0, sp, :]
                                    yo = AP(a, a.offset, [a.ap[0], [(2 * TC - 1 - 2 * sp) * IP, 2], [1, IP]])
                                    s.red(yo, T4[:, :, :, :], [(T4, 0), (T4, 1)], [(y, 0), (y, 1)])
                            return fn
                        pending.append(mk(Sn, T4, y, k, sp))
                        cur = 1 - cur
                        step += 1

                    def mkstore(y, tf, tr_):
                        def fn():
                            for d in range(2):
                                tt0 = tf if d == 0 else tr_
                                for iq in range(IQ):
                                    dst = s.ysc[d][:, :, tt0:tt0 + TC, iq * IP:(iq + 1) * IP].rearrange(
                                        "b h t j -> (b h) t j")
                                    s.ldnc(dst, y[iq * NBH:(iq + 1) * NBH, d, :, :], [(y, d)], [('ysc', d, tt0, iq)])
                        return fn
                    stores.append(mkstore(y, tf, tr_))
            flush_pending('pool')
            flush_pending('dve')
            for fn in stores:
                fn()
            P.flush()

    def phase_rwkv_post(s):
        c, P, l = s.c, s.P, s.l
        I = s.I
        BW, H, BC = c.BW, c.H, c.BC
        with ExitStack() as es:
            def rowb(name, src_row):
                t = s.sb(es, name, (128, BW))
                s.ld(t[:, :], AP(src_row, src_row.offset, [[0, 128], [1, BW]]), (), [t])
                return t
            gnw = rowb('gnw', I['gn_w'][l:l + 1, :])
            gnb = rowb('gnb', I['gn_b'][l:l + 1, :])
            rkr = rowb('rkr', I['r_k'][l:l + 1, :])
            gup = s.sb(es, 'gup', (128, c.GR // 128, BW))
            s.ld(gup[:, :, :], I['gate_up'][l * c.GR:(l + 1) * c.GR, :].rearrange("(k p) n -> p k n", p=128), (), [gup])
            nb = 2
            y0 = [s.sb(es, 'po_y0%d' % i, (128, H, 64)) for i in range(nb)]
            y1 = [s.sb(es, 'po_y1%d' % i, (128, H, 64)) for i in range(nb)]
            rr = [s.sb(es, 'po_r%d' % i, (128, H, 64)) for i in range(nb)]
            k0 = [s.sb(es, 'po_k0%d' % i, (128, H, 64)) for i in range(nb)]
            k1 = [s.sb(es, 'po_k1%d' % i, (128, H, 64)) for i in range(nb)]
            vv = [s.sb(es, 'po_v%d' % i, (128, H, 64)) for i in range(nb)]
            sgt = [s.sb(es, 'po_sg%d' % i, (128, c.GR // 128, 128)) for i in range(nb)]
            sq = s.sb(es, 'po_sq', (128, H, 64))
            st = s.sb(es, 'po_st', (128, 4, H))
            ob = [s.sb(es, 'po_o%d' % i, (128, BC, 128), BF16) for i in range(2)]
            pg = [s.ps(es, 'po_pg%d' % i, (128, 512)) for i in range(max(BW // 512, 1))]
            ptr = [s.ps(es, 'po_pt%d' % i, (128, 4, 128)) for i in range(2)]
            it = 0
            for (kind, b, t0s, ln, mc) in c.seqs:
                if s.last and kind == 'c':
                    continue
                tallb = 0 if kind == 'c' else c.CTX
                for t0 in range(t0s, t0s + ln, 128):
                    k = it % nb
                    it += 1
                    ta = tallb + (t0 - t0s)
                    def tok(dr):
                        return dr[b, :, ta:ta + 128, :].rearrange("h t j -> t h j")
                    s.ld(y0[k][:, :, :], tok(s.ysc[0]), ['ysc_all'], [y0[k]])
                    s.ld(y1[k][:, :, :], tok(s.ysc[1]), ['ysc_all'], [y1[k]])
                    s.ld(rr[k][:, :, :], tok(s.strm['r']), ['strm_all'], [rr[k]])
                    s.ld(k0[k][:, :, :], tok(s.strm['kd0']), ['strm_all'], [k0[k]])
                    s.ld(k1[k][:, :, :], tok(s.strm['kd1']), ['strm_all'], [k1[k]])
                    s.ld(vv[k][:, :, :], tok(s.strm['v']), ['strm_all'], [vv[k]])
                    s.ld(sgt[k][:, :, :], s.sgdT[:, t0:t0 + 128].rearrange("(k p) t -> p k t", p=128), ['sgdT_all'],
                         [sgt[k]])
                    Y, Y1, R, K0, K1, V = y0[k], y1[k], rr[k], k0[k], k1[k], vv[k]
                    def hb(ap2):
                        return AP(ap2, ap2.offset, [ap2.ap[0], ap2.ap[1], [0, 64]])
                    s.tt('dve', Y[:, :, :], Y[:, :, :], Y1[:, :, :], ALU.add, [Y, Y1], [Y])
                    s.red(st[:, 0, :], Y[:, :, :], [Y], [st])
                    s.ts('dve', st[:, 0, :], st[:, 0, :], 1.0 / 64, None, ALU.mult, None, [st], [st])
                    s.tt('dve', Y[:, :, :], Y[:, :, :], hb(st[:, 0, :]), ALU.subtract, [Y, st], [Y])
                    s.tt('pool', sq[:, :, :], Y[:, :, :], Y[:, :, :], ALU.mult, [Y], [sq])
                    s.red(st[:, 1, :], sq[:, :, :], [sq], [st])
                    s.rsqrt(st[:, 1, :], st[:, 1, :], 1.0 / 64, 64e-5, [st], [st])
                    s.tt('dve', Y[:, :, :], Y[:, :, :], hb(st[:, 1, :]), ALU.mult, [Y, st], [Y])
                    Yf = Y[:, :, :].rearrange("p h j -> p (h j)")
                    s.tt('pool', Yf, Yf, gnw[:, :], ALU.mult, [Y, gnw], [Y])
                    s.tt('pool', Yf, Yf, gnb[:, :], ALU.add, [Y, gnb], [Y])
                    s.tt('pool', K0[:, :, :], K0[:, :, :], K1[:, :, :], ALU.add, [K0, K1], [K0])
                    s.tt('pool', K0[:, :, :], K0[:, :, :], R[:, :, :], ALU.mult, [K0, R], [K0])
                    K0f = K0[:, :, :].rearrange("p h j -> p (h j)")
                    s.tt('pool', K0f, K0f, rkr[:, :], ALU.mult, [K0, rkr], [K0])
                    s.red(st[:, 2, :], K0[:, :, :], [K0], [st])
                    s.tt('dve', V[:, :, :], V[:, :, :], hb(st[:, 2, :]), ALU.mult, [V, st], [V])
                    s.tt('dve', Y[:, :, :], Y[:, :, :], V[:, :, :], ALU.add, [Y, V], [Y])
                    for gi in range(len(pg)):
                        n0 = gi * 512
                        n1 = min(BW, n0 + 512)
                        s.mm(pg[gi][:, 0:n1 - n0], [(sgt[k][:, kk_, :], gup[:, kk_, n0:n1]) for kk_ in range(c.GR // 128)],
                             [sgt[k], gup], [pg[gi]])
                        s.tt('dve', Yf[:, n0:n1], Yf[:, n0:n1], pg[gi][:, 0:n1 - n0], ALU.mult, [Y, pg[gi]], [Y])
                    o = ob[it % 2]
                    for q4 in range(0, BC, 4):
                        n4 = min(4, BC - q4)
                        p_ = ptr[(q4 // 4) % 2]
                        s.tr([(p_[:, j, :], Yf[:, (q4 + j) * 128:(q4 + j + 1) * 128]) for j in range(n4)], s.ident[:, :],
                             [Y, s.ident], [p_])
                        s.cp('act', o[:, q4:q4 + n4, :], p_[:, 0:n4, :], [p_], [o])
                    s.st(s.brT[0][:, t0:t0 + 128].rearrange("(k p) t -> p k t", p=128), o[:, :, :], [o], [('br0', t0)])
            P.flush()

    def phase_attn(s):
        c, P, l = s.c, s.P, s.l
        I = s.I
        KVH, H = c.KVH, c.H
        TA = c.TALL
        NBK = TA // 128
        NCB = c.CTX // 128
        G = 4
        with ExitStack() as es:
            rope = s.sb(es, 'rope', (64, 2, c.SEQ))
            s.ld(rope[:, :, :], I['k_rope'].rearrange("(a n) t -> n a t", a=2), (), [rope])
            esk = s.sb(es, 'esk', (64, H))
            s.ld(esk[:, :], AP(I['attn_sink'], l * H, [[0, 64], [1, H]]), (), [esk])
            s.act(esk[:, :], esk[:, :], AF.Exp, [esk], [esk])
            kT = s.sb(es, 'kT', (64, KVH, TA), BF16)
            qT = s.sb(es, 'qT', (64, G, TA), BF16)
            vtk = s.sb(es, 'vtk', (128, NBK, KVH, 64), BF16)
            xin = [s.sb(es, 'at_x%d' % i, (64, 512)) for i in range(3)]
            t1 = [s.sb(es, 'at_t%d' % i, (64, 512)) for i in range(2)]
            eb = [s.sb(es, 'at_e%d' % i, (128, G, 128), BF16) for i in range(3)]
            den = s.sb(es, 'at_den', (64, G, 128))
            ot = [s.sb(es, 'at_o%d' % i, (64, G, 128), BF16) for i in range(2)]
            psw = [s.ps(es, 'at_ps%d' % i, (64, 512)) for i in range(2)]
            pss = [s.ps(es, 'at_s%d' % i, (128, G, 128)) for i in range(2)]
            pso = s.ps(es, 'at_po', (64, G, 128))
            psd = s.ps(es, 'at_pd', (64, G, 128))
            psv = s.ps(es, 'at_pv', (128, 4, 64))
            cnt = {'x': 0, 'e': 0, 'o': 0}

            def load_feat(dst, row0, b, with_q_ctx, res):
                segs = []
                tc0 = b * c.CTX
                for t in range(0, c.CTX, 512):
                    n = min(512, c.CTX - t)
                    segs.append((tc0 + t, t, n, None))
                tl0 = c.NS * c.CTX + b * c.SEQ
                for t in range(0, c.SEQ, 512):
                    n = min(512, c.SEQ - t)
                    segs.append((tl0 + t, c.CTX + t, n, t))
                for (tg, td, n, tp) in segs:
                    if tp is None and not with_q_ctx:
                        continue
                    x = xin[cnt['x'] % 3]
                    cnt['x'] += 1
                    s.ld(x[:, 0:n], s.pT[row0:row0 + 64, tg:tg + n], ['pT_all'], [x])
                    if tp is None:
                        s.cp('act', dst[:, td:td + n], x[:, 0:n], [x], [res])
                    else:
                        p_ = psw[cnt['x'] % 2]
                        ta_, tb_ = t1[0], t1[1]
                        s.mm(p_[:, 0:n], [(s.swp[:, :], x[:, 0:n])], [s.swp, x], [p_])
                        s.tt('pool', ta_[:, 0:n], x[:, 0:n], rope[:, 0, tp:tp + n], ALU.mult, [x, rope], [ta_])
                        s.tt('dve', tb_[:, 0:n], p_[:, 0:n], rope[:, 1, tp:tp + n], ALU.mult, [p_, rope], [tb_])
                        s.tt('dve', dst[:, td:td + n], ta_[:, 0:n], tb_[:, 0:n], ALU.add, [ta_, tb_], [res])

            for b in range(c.NS):
                for kh in range(KVH):
                    load_feat(kT[:, kh, :], c.RC + kh * 64, b, True, kT)
                    tc0 = b * c.CTX
                    tl0 = c.NS * c.CTX + b * c.SEQ
                    for blk0 in range(0, NBK, 4):
                        nb_ = min(4, NBK - blk0)
                        x = xin[cnt['x'] % 3]
                        cnt['x'] += 1
                        for j in range(nb_):
                            kb = blk0 + j
                            tg = tc0 + kb * 128 if kb < NCB else tl0 + (kb - NCB) * 128
                            s.ld(x[:, j * 128:(j + 1) * 128], s.pT[c.RC + c.KVW + kh * 64:c.RC + c.KVW + kh * 64 + 64,
                                                                  tg:tg + 128], ['pT_all'], [x])
                        s.tr([(psv[:, j, :], x[:, j * 128:(j + 1) * 128]) for j in range(nb_)], s.ident[0:64, 0:64],
                             [x, s.ident], [psv])
                        s.cp('act', vtk[:, blk0:blk0 + nb_, kh, :], psv[:, 0:nb_, :], [psv], [vtk])
                for kh in range(KVH):
                    for g in range(G):
                        load_feat(qT[:, g, :], c.CTXC + (kh * G + g) * 64, b, not s.last, qT)
                    qblocks = []
                    if not s.last:
                        for n in range(NCB):
                            qblocks.append(('c', n))
                    for n in range(c.SEQ // 128):
                        qblocks.append(('l', n))
                    for (qk, n) in qblocks:
                        if qk == 'c':
                            q0 = n * 128
                            kbs = [(kb, None) for kb in range(NCB)]
                            tok0 = b * c.CTX + n * 128
                        else:
                            q0 = c.CTX + n * 128
                            kbs = [(kb, None) for kb in range(NCB)]
                            nl = c.SEQ // 128
                            if n > 0:
                                kbs.append((NCB + n - 1, 0))
                            kbs.append((NCB + n, None))
                            if n < nl - 1:
                                kbs.append((NCB + n + 1, 1))
                            tok0 = c.NS * c.CTX + b * c.SEQ + n * 128
                        for i, (kb, mk) in enumerate(kbs):
                            ps_ = pss[cnt['e'] % 2]
                            e = eb[cnt['e'] % 3]
                            cnt['e'] += 1
                            s.mm(ps_[:, :, :], [(kT[:, kh, kb * 128:(kb + 1) * 128], qT[:, :, q0:q0 + 128])],
                                 [kT, qT], [ps_])
                            s.act(e[:, :, :], ps_[:, :, :], AF.Exp, [ps_], [e], scale=0.125)
                            if mk is not None:
                                m = s.masks[:, mk, :]
                                mb = AP(m, m.offset, [m.ap[0], [0, G], m.ap[1]])
                                s.tt('dve', e[:, :, :], e[:, :, :], mb, ALU.mult, [e, s.masks], [e])
                            s.mm(pso[:, :, :], [(vtk[:, kb, kh, :], e[:, :, :])], [vtk, e], [pso],
                                 start=(i == 0), stop=(i == len(kbs) - 1))
                            s.mm(psd[:, :, :], [(s.onesb[:, :], e[:, :, :])], [s.onesb, e], [psd],
                                 start=(i == 0), stop=(i == len(kbs) - 1))
                        a = esk[:, kh * G:(kh + 1) * G]
                        eskb = AP(a, a.offset, [a.ap[0], a.ap[1], [0, 128]])
                        s.tt('dve', den[:, :, :], psd[:, :, :], eskb, ALU.add, [psd, esk], [den])
                        s.P.op('dve', lambda g_: g_.reciprocal(out=den[:, :, :], in_=den[:, :, :]), [den], [den])
                        o = ot[cnt['o'] % 2]
                        cnt['o'] += 1
                        s.tt('dve', o[:, :, :], pso[:, :, :], den[:, :, :], ALU.mult, [pso, den], [o])
                        s.st(s.brT[2][kh * G * 64:(kh + 1) * G * 64, tok0:tok0 + 128].rearrange("(g p) t -> p g t", p=64),
                             o[:, :, :], [o], [('br2', kh, tok0)])
            P.flush()

    def phase_conv(s):
        c, P, l = s.c, s.P, s.l
        BC, BW = c.BC, c.BW
        SEG = 512
        with ExitStack() as es:
            cw = s.sb(es, 'cw', (128, 3, BC))
            for j in range(3):
                s.ldnc(cw[:, j, :], s.I['conv_w'][3 * l + j:3 * l + j + 1, :].rearrange("o (k p) -> p (o k)", p=128),
                       [cw], [cw])
            bt = [s.sb(es, 'cv_b%d' % i, (128, SEG)) for i in range(2)]
            ct = [s.sb(es, 'cv_c%d' % i, (128, SEG + 2)) for i in range(2)]
            ut = [s.sb(es, 'cv_u%d' % i, (128, SEG + 2)) for i in range(2)]
            o = [s.sb(es, 'cv_o%d' % i, (128, SEG)) for i in range(2)]
            ob = [s.sb(es, 'cv_ob%d' % i, (128, SEG), BF16) for i in range(2)]
            it = 0
            for (kind, b, t0s, ln, mc) in c.seqs:
                if s.last and kind == 'c':
                    continue
                for t0 in range(t0s, t0s + ln, SEG):
                    seg = min(SEG, t0s + ln - t0)
                    for ch in range(BC):
                        k = it % 2
                        it += 1
                        B_, C_, U_, O_, OB = bt[k], ct[k], ut[k], o[k], ob[k]
                        s.ld(B_[:, 0:seg], s.pT[c.QEND + ch * 128:c.QEND + (ch + 1) * 128, t0:t0 + seg], ['pT_all'], [B_])
                        s.load_halo(C_, 128, c.QEND + BW + ch * 128, t0, seg, t0s, t0s + ln)
                        s.load_halo(U_, 128, c.QEND + 2 * BW + ch * 128, t0, seg, t0s, t0s + ln)
                        s.tt('pool', C_[:, 0:seg + 2], C_[:, 0:seg + 2], U_[:, 0:seg + 2], ALU.mult, [C_, U_], [C_])
                        s.ts('dve', O_[:, 0:seg], C_[:, 1:seg + 1], cw[:, 1, ch:ch + 1], None, ALU.mult, None, [C_, cw], [O_])
                        s.stt('dve', O_[:, 0:seg], C_[:, 0:seg], cw[:, 0, ch:ch + 1], O_[:, 0:seg], ALU.mult, ALU.add,
                              [C_, cw, O_], [O_])
                        s.stt('dve', O_[:, 0:seg], C_[:, 2:seg + 2], cw[:, 2, ch:ch + 1], O_[:, 0:seg], ALU.mult, ALU.add,
                              [C_, cw, O_], [O_])
                        s.tt('pool', OB[:, 0:seg], O_[:, 0:seg], B_[:, 0:seg], ALU.mult, [O_, B_], [OB])
                        s.st(s.brT[1][ch * 128:(ch + 1) * 128, t0:t0 + seg], OB[:, 0:seg], [OB], [('br1', ch, t0)])
            P.flush()

    def phase_merge(s):
        c, P, l = s.c, s.P, s.l
        KC, BC, D, BW = c.KC, c.BC, c.D, c.BW
        G = 512
        NS1 = c.NS + 1
        with ExitStack() as es:
            br = [[s.sb(es, 'mg_br%d_%d' % (i, k), (128, BC, G), BF16) for k in range(1)] for i in range(3)]
            wb = [s.sb(es, 'mg_w%d' % i, (128, BC, 512), BF16) for i in range(2)]
            wo = [s.sb(es, 'mg_wo%d' % i, (128, KC, 512), BF16) for i in range(2)]
            gt = [s.sb(es, 'mg_g%d' % i, (128, 4, G)) for i in range(2)]
            mT = s.sb(es, 'mg_m', (128, KC, G), BF16)
            acc = s.sb(es, 'mg_acc', (128, 4, G))
            tmp = s.sb(es, 'mg_tmp', (128, G))
            xt = [s.sb(es, 'mg_x%d' % i, (128, D)) for i in range(4)]
            g2t = s.sb(es, 'mg_gate', (128, D))
            pp = [s.ps(es, 'mg_p%d' % i, (128, 512)) for i in range(4)]
            M6 = 6 * D
            wbs = s.W['w_branch']
            wos = s.W['w_out'][l * D:(l + 1) * D, :].rearrange("(k p) n -> p k n", p=128)
            wi = 0
            pi = 0
            gi_ = 0
            xi = 0
            groups = []
            for (kind, b, t0s, ln, mc) in c.seqs:
                if s.last and kind == 'c':
                    continue
                for t0 in range(t0s, t0s + ln, G):
                    groups.append((t0, min(G, t0s + ln - t0), mc))
            for gidx, (t0, n, mc) in enumerate(groups):
                k = 0
                s.ld(g2t[:, :], AP(s.modrow, (l * NS1 + mc) * M6 + 2 * D, [[0, 128], [1, D]]), ['modrow_all'], [g2t])
                for i in range(3):
                    s.ld(br[i][k][:, :, 0:n], s.brT[i][:, t0:t0 + n].rearrange("(k p) t -> p k t", p=128), ['br_all'],
                         [br[i][k]])
                for oq in range(D // 512):
                    for i in range(3):
                        w = wb[wi % 2]
                        wi += 1
                        s.ld(w[:, :, :], wbs[(l * 3 + i) * BW:(l * 3 + i + 1) * BW, oq * 512:(oq + 1) * 512].rearrange(
                            "(k p) n -> p k n", p=128), ['wfull_b'], [w])
                        gts = gt[gi_ % 2]
                        gi_ += 1
                        r0 = c.CONVEND + i * D + oq * 512
                        s.ld(gts[:, :, 0:n], s.pT[r0:r0 + 512, t0:t0 + n].rearrange("(j p) t -> p j t", p=128), ['pT_all'],
                             [gts])
                        for j in range(4):
                            p_ = pp[pi % 4]
                            pi += 1
                            s.mm(p_[:, 0:n], [(w[:, kk_, j * 128:(j + 1) * 128], br[i][k][:, kk_, 0:n]) for kk_ in range(BC)],
                                 [w, br[i][k]], [p_])
                            if i == 0:
                                s.tt('dve', acc[:, j, 0:n], p_[:, 0:n], gts[:, j, 0:n], ALU.mult, [p_, gts], [(acc, j)])
                            else:
                                s.tt('dve', tmp[:, 0:n], p_[:, 0:n], gts[:, j, 0:n], ALU.mult, [p_, gts], [tmp])
                                if i == 1:
                                    s.tt('pool', acc[:, j, 0:n], acc[:, j, 0:n], tmp[:, 0:n], ALU.add, [(acc, j), tmp],
                                         [(acc, j)])
                                else:
                                    s.tt('pool', mT[:, oq * 4 + j, 0:n], acc[:, j, 0:n], tmp[:, 0:n], ALU.add,
                                         [(acc, j), tmp], [(mT, oq * 4 + j)])
                allm = [(mT, j) for j in range(KC)]
                wts = []
                for oq in range(D // 512):
                    w = wo[oq % 2]
                    s.ld(w[:, :, :], wos[:, :, oq * 512:(oq + 1) * 512], ['wfull_o'], [w])
                    for tb in range(n // 128):
                        x = xt[tb]
                        if oq == 0:
                            s.ld(x[:, :], s.xres[t0 + tb * 128:t0 + (tb + 1) * 128, :], ['xres'], [x])
                        p_ = pp[pi % 4]
                        pi += 1
                        s.mm(p_[:, :], [(mT[:, kk_, tb * 128:(tb + 1) * 128], w[:, kk_, :]) for kk_ in range(KC)],
                             allm + [w], [p_])
                        s.tt('dve', tmp[:, 0:512], p_[:, :], g2t[:, oq * 512:(oq + 1) * 512], ALU.mult, [p_, g2t], [tmp])
                        s.tt('pool', x[:, oq * 512:(oq + 1) * 512], x[:, oq * 512:(oq + 1) * 512], tmp[:, 0:512], ALU.add,
                             [x, tmp], [x])
                        if oq == D // 512 - 1:
                            s.st(s.xres[t0 + tb * 128:t0 + (tb + 1) * 128, :], x[:, :], [x], ['xres'])
                xi += n // 128
            P.flush()

    def phase_router(s, kind):
        c, P, l = s.c, s.P, s.l
        KC, D, NE = c.KC, c.D, c.NE
        n_tok = c.SEQ if kind == 'l' else c.CTX
        cap = c.CAPL if kind == 'l' else c.CAPC
        s.cap = cap
        with ExitStack() as es:
            wr = s.sb(es, 'rt_w', (128, KC, NE))
            s.ld(wr[:, :, :], s.I['w_router'][l * D:(l + 1) * D, :].rearrange("(k p) e -> p k e", p=128), (), [wr])
            xt = [s.sb(es, 'rt_x%d' % i, (128, D)) for i in range(2)]
            junk = s.sb(es, 'rt_j', (128, D))
            ss = [s.sb(es, 'rt_s%d' % i, (128, 2)) for i in range(2)]
            hT = [s.sb(es, 'rt_h%d' % i, (128, KC, 128)) for i in range(2)]
            tmp = [s.sb(es, 'rt_t%d' % i, (128, 4, 128)) for i in range(2)]
            lg = [s.sb(es, 'rt_lg%d' % i, (128, NE)) for i in range(2)]
            sm = [s.sb(es, 'rt_sm%d' % i, (128, 2)) for i in range(2)]
            affT = [s.sb(es, 'rt_aff%d' % b, (NE, n_tok)) for b in range(c.NS)]
            work = [s.sb(es, 'rt_wk%d' % b, (NE, n_tok)) for b in range(c.NS)]
            pt = [s.ps(es, 'rt_p%d' % i, (128, 4, 128)) for i in range(2)]
            pl = [s.ps(es, 'rt_pl%d' % i, (128, NE)) for i in range(2)]
            pa = [s.ps(es, 'rt_pa%d' % i, (NE, 128)) for i in range(2)]
            it = 0
            pi = 0
            for (kd, b, t0s, ln, mc) in c.seqs:
                if kd != kind:
                    continue
                for t0 in range(t0s, t0s + ln, 128):
                    k = it % 2
                    it += 1
                    x, sq, h = xt[k], ss[k], hT[k]
                    s.ld(x[:, :], s.xres[t0:t0 + 128, :], ['xres'], [x])
                    s.act(junk[:, :], x[:, :], AF.Square, [x], [junk, sq], accum=sq[:, 0:1])
                    s.rsqrt(sq[:, 1:2], sq[:, 0:1], 1.0 / D, 1e-6, [sq], [sq])
                    s.ts('dve', x[:, :], x[:, :], sq[:, 1:2], None, ALU.mult, None, [x, sq], [x])
                    s.st(s.xn[t0:t0 + 128, :], x[:, :], [x], [('xn', t0)])
                    for q4 in range(KC // 4):
                        p_ = pt[pi % 2]
                        tm = tmp[pi % 2]
                        pi += 1
                        s.tr([(p_[:, j, :], x[:, (q4 * 4 + j) * 128:(q4 * 4 + j + 1) * 128]) for j in range(4)],
                             s.ident[:, :], [x, s.ident], [p_])
                        gb = s.gcol[:, 1, q4 * 4:q4 * 4 + 4, mc]
                        gb = AP(gb, gb.offset, [gb.ap[0], gb.ap[1], [0, 128]])
                        sb_ = s.modcol[:, 3 * KC + q4 * 4:3 * KC + q4 * 4 + 4, mc]
                        sb_ = AP(sb_, sb_.offset, [sb_.ap[0], sb_.ap[1], [0, 128]])
                        s.tt('dve', tm[:, :, :], p_[:, :, :], gb, ALU.mult, [p_, s.gcol], [tm])
                        s.tt('pool', h[:, q4 * 4:q4 * 4 + 4, :], tm[:, :, :], sb_, ALU.add, [tm, s.modcol], [h])
                    pl_ = pl[k]
                    s.mm(pl_[:, :], [(h[:, kk_, :], wr[:, kk_, :]) for kk_ in range(KC)], [h, wr], [pl_])
                    L_, sm_ = lg[k], sm[k]
                    s.P.op('dve', lambda g_, sm_=sm_, pl_=pl_: g_.tensor_reduce(out=sm_[:, 0:1], in_=pl_[:, :], axis=AX.X,
                                                                              op=ALU.max, negate=True), [pl_], [sm_])
                    s.act(L_[:, :], pl_[:, :], AF.Exp, [pl_, sm_], [L_, sm_], bias=sm_[:, 0:1], accum=sm_[:, 1:2])
                    s.P.op('dve', lambda g_, sm_=sm_: g_.reciprocal(out=sm_[:, 1:2], in_=sm_[:, 1:2]), [sm_], [sm_])
                    s.ts('dve', L_[:, :], L_[:, :], sm_[:, 1:2], None, ALU.mult, None, [L_, sm_], [L_])
                    pa_ = pa[k]
                    s.tr([(pa_[:, :], L_[:, :])], s.ident[:, :], [L_, s.ident], [pa_])
                    s.cp('act', affT[b][:, t0 - t0s:t0 - t0s + 128], pa_[:, :], [pa_], [(affT[b], t0)])
            P.flush()
            s.topk = es
            gk = [s.sb(es, 'rt_gk%d' % b, (NE, cap)) for b in range(c.NS)]
            ik = [s.sb(es, 'rt_ik%d' % b, (NE, cap), U32) for b in range(c.NS)]
            ikf = [s.sb(es, 'rt_ikf%d' % b, (NE, cap)) for b in range(c.NS)]
            for b in range(c.NS):
                cur = affT[b]
                for r in range(cap // 8):
                    g8 = gk[b][:, r * 8:(r + 1) * 8]
                    s.P.op('dve', (lambda g8, cur: lambda g_: g_.max(out=g8, in_=cur[:, :]))(g8, cur), [cur], [gk[b]])
                    s.P.op('dve', (lambda g8, cur, b, r: lambda g_: g_.max_index(out=ik[b][:, r * 8:(r + 1) * 8], in_max=g8,
                                                                           in_values=cur[:, :]))(g8, cur, b, r),
                           [cur, gk[b]], [ik[b]])
                    if r < cap // 8 - 1:
                        s.P.op('dve', (lambda g8, cur, b: lambda g_: g_.match_replace(
                            out=work[b][:, :], in_to_replace=g8, in_values=cur[:, :], imm_value=-1.0))(g8, cur, b),
                            [cur, gk[b]], [work[b]])
                        cur = work[b]
                s.cp('dve', ikf[b][:, :], ik[b][:, :], [ik[b]], [ikf[b]])
                t0s_b = [q for q in c.seqs if q[0] == kind and q[1] == b][0][2]
                s.ts('dve', ikf[b][:, :], ikf[b][:, :], float(t0s_b), None, ALU.add, None, [ikf[b]], [ikf[b]])
            nck = (cap + 127) // 128
            s.gsel = s.gselk[kind]
            s.isel = s.iselk[kind]
            ptk = [s.ps(es, 'rt_ptk%d' % i, (128, NE)) for i in range(2)]
            tg = [s.sb(es, 'rt_tg%d' % i, (128, NE)) for i in range(2)]
            ti = [s.sb(es, 'rt_ti%d' % i, (128, NE), I32) for i in range(2)]
            j = 0
            for b in range(c.NS):
                for ck in range(nck):
                    n = min(128, cap - ck * 128)
                    p_ = ptk[j % 2]
                    s.tr([(p_[0:n, :], gk[b][:, ck * 128:ck * 128 + n])], s.ident[0:NE, 0:NE], [gk[b], s.ident], [p_])
                    s.cp('dve', tg[j % 2][0:n, :], p_[0:n, :], [p_], [tg[j % 2]])
                    s.ld(s.gsel[b, ck * 128:ck * 128 + n, :], tg[j % 2][0:n, :], [tg[j % 2]], [('gsel', b, ck)])
                    j += 1
                    p_ = ptk[j % 2]
                    s.tr([(p_[0:n, :], ikf[b][:, ck * 128:ck * 128 + n])], s.ident[0:NE, 0:NE], [ikf[b], s.ident], [p_])
                    s.cp('dve', ti[j % 2][0:n, :], p_[0:n, :], [p_], [ti[j % 2]])
                    s.ld(s.isel[b, ck * 128:ck * 128 + n, :], ti[j % 2][0:n, :], [ti[j % 2]], [('isel', b, ck)])
                    j += 1
            P.flush()

    def phase_moe(s, kind):
        c, P, l = s.c, s.P, s.l
        KC, D, NE, FF = c.KC, c.D, c.NE, c.FF
        FC = FF // 128
        cap = s.cap
        NS1 = c.NS + 1
        nck = (cap + 127) // 128
        cw = min(cap, 128)
        NTOK = c.NS * cap
        M6 = 6 * D
        seqs = [q for q in c.seqs if q[0] == kind]
        with ExitStack() as es:
            gsel = s.sb(es, 'mo_g', (128, c.NS, nck, NE))
            isel = s.sb(es, 'mo_i', (128, c.NS, nck, NE), I32)
            for b in range(c.NS):
                for ck in range(nck):
                    n = min(128, cap - ck * 128)
                    s.ld(gsel[0:n, b, ck, :], s.gsel[b, ck * 128:ck * 128 + n, :], ['gsel_all'], [gsel])
                    s.ld(isel[0:n, b, ck, :], s.isel[b, ck * 128:ck * 128 + n, :], ['isel_all'], [isel])
            g5 = []
            for (kd, b, t0s, ln, mc) in seqs:
                t = s.sb(es, 'mo_g5%d' % b, (128, D))
                s.ld(t[:, :], AP(s.modrow, (l * NS1 + mc) * M6 + 5 * D, [[0, 128], [1, D]]), ['modrow_all'], [t])
                g5.append(t)
            xs = [s.sb(es, 'mo_xs%d' % i, (128, D)) for i in range(2)]
            xsT = [s.sb(es, 'mo_xT%d' % i, (128, KC, NTOK), BF16) for i in range(2)]
            hid = s.sb(es, 'mo_hid', (128, FC, NTOK), BF16)
            sg = [s.sb(es, 'mo_sg%d' % i, (128, NTOK)) for i in range(2)]
            ys = [s.sb(es, 'mo_ys%d' % i, (128, D)) for i in range(c.NS * nck)]
            tmp = [s.sb(es, 'mo_t%d' % i, (128, 4, 128)) for i in range(2)]
            wb = [s.sb(es, 'mo_w%d' % i, (128, max(KC, FC), 512), BF16) for i in range(3)]
            pt = [s.ps(es, 'mo_pt%d' % i, (128, 4, 128)) for i in range(2)]
            pg = [s.ps(es, 'mo_pg%d' % i, (128, NTOK)) for i in range(2)]
            pu = [s.ps(es, 'mo_pu%d' % i, (128, NTOK)) for i in range(2)]
            pd = [s.ps(es, 'mo_pd%d' % i, (128, 512)) for i in range(2)]
            cnt = {'x': 0, 'p': 0, 'w': 0, 'h': 0, 'y': 0, 'd': 0}
            for e_ in range(NE):
                xT = xsT[e_ % 2]
                for bi, (kd, b, t0s, ln, mc) in enumerate(seqs):
                    for ck in range(nck):
                        n = min(128, cap - ck * 128)
                        x = xs[cnt['x'] % 2]
                        cnt['x'] += 1
                        s.P.dma('pool', (lambda x, n, b, ck, e_, t0s, ln: lambda g_: g_.indirect_dma_start(
                            out=x[0:n, :], out_offset=None, in_=s.xn[:, :],
                            in_offset=bass.IndirectOffsetOnAxis(ap=isel[0:n, b, ck, e_:e_ + 1], axis=0)))(
                            x, n, b, ck, e_, t0s, ln), ['xn_all', isel], [x])
                        c0 = bi * cap + ck * 128
                        for q4 in range(KC // 4):
                            p_ = pt[cnt['p'] % 2]
                            tm = tmp[cnt['p'] % 2]
                            cnt['p'] += 1
                            s.tr([(p_[:, j, 0:n], x[0:n, (q4 * 4 + j) * 128:(q4 * 4 + j + 1) * 128]) for j in range(4)],
                                 s.ident[0:n, 0:n], [x, s.ident], [p_])
                            gb = s.gcol[:, 1, q4 * 4:q4 * 4 + 4, mc]
                            gb = AP(gb, gb.offset, [gb.ap[0], gb.ap[1], [0, n]])
                            sb_ = s.modcol[:, 3 * KC + q4 * 4:3 * KC + q4 * 4 + 4, mc]
                            sb_ = AP(sb_, sb_.offset, [sb_.ap[0], sb_.ap[1], [0, n]])
                            s.tt('dve', tm[:, :, 0:n], p_[:, :, 0:n], gb, ALU.mult, [p_, s.gcol], [tm])
                            s.tt('pool', xT[:, q4 * 4:q4 * 4 + 4, c0:c0 + n], tm[:, :, 0:n], sb_, ALU.add,
                                 [tm, s.modcol], [xT])
                wg = s.W['w_exp_gate'][(l * NE + e_) * D:(l * NE + e_ + 1) * D, :].rearrange("(k p) f -> p k f", p=128)
                wu = s.W['w_exp_up'][(l * NE + e_) * D:(l * NE + e_ + 1) * D, :].rearrange("(k p) f -> p k f", p=128)
                wd = s.W['w_exp_down'][(l * NE + e_) * FF:(l * NE + e_ + 1) * FF, :].rearrange("(k p) d -> p k d", p=128)
                for fq in range(FF // 512):
                    w1 = wb[cnt['w'] % 3]
                    cnt['w'] += 1
                    w2 = wb[cnt['w'] % 3]
                    cnt['w'] += 1
                    s.ld(w1[:, 0:KC, :], wg[:, :, fq * 512:(fq + 1) * 512], ['wfull_e'], [w1])
                    s.ld(w2[:, 0:KC, :], wu[:, :, fq * 512:(fq + 1) * 512], ['wfull_e'], [w2])
                    for j in range(4):
                        pg_ = pg[cnt['h'] % 2]
                        pu_ = pu[cnt['h'] % 2]
                        sg_ = sg[cnt['h'] % 2]
                        cnt['h'] += 1
                        s.mm(pg_[:, :], [(w1[:, kk_, j * 128:(j + 1) * 128], xT[:, kk_, :]) for kk_ in range(KC)], [w1, xT], [pg_])
                        s.mm(pu_[:, :], [(w2[:, kk_, j * 128:(j + 1) * 128], xT[:, kk_, :]) for kk_ in range(KC)], [w2, xT], [pu_])
                        s.act(sg_[:, :], pg_[:, :], AF.Silu, [pg_], [sg_])
                        s.tt('dve', hid[:, fq * 4 + j, :], sg_[:, :], pu_[:, :], ALU.mult, [sg_, pu_], [hid])
                tbs = []
                for bi, (kd, b, t0s, ln, mc) in enumerate(seqs):
                    for ck in range(nck):
                        tbs.append((bi, b, ck, min(128, cap - ck * 128), bi * cap + ck * 128))
                for dq in range(D // 512):
                    w = wb[cnt['w'] % 3]
                    cnt['w'] += 1
                    s.ld(w[:, 0:FC, :], wd[:, :, dq * 512:(dq + 1) * 512], ['wfull_e'], [w])
                    for ti_, (bi, b, ck, n, c0) in enumerate(tbs):
                        y = ys[ti_]
                        p_ = pd[cnt['d'] % 2]
                        cnt['d'] += 1
                        s.mm(p_[0:n, :], [(hid[:, kk_, c0:c0 + n], w[:, kk_, :]) for kk_ in range(FC)], [hid, w], [p_])
                        s.stt('dve', y[0:n, dq * 512:(dq + 1) * 512], p_[0:n, :], gsel[0:n, b, ck, e_:e_ + 1],
                              g5[bi][0:n, dq * 512:(dq + 1) * 512], ALU.mult, ALU.mult, [p_, gsel, g5[bi]], [y])
                for ti_, (bi, b, ck, n, c0) in enumerate(tbs):
                    y = ys[ti_]
                    s.P.dma('pool', (lambda y, n, b, ck, e_: lambda g_: g_.indirect_dma_start(
                        out=s.xres[:, :],
                        out_offset=bass.IndirectOffsetOnAxis(ap=isel[0:n, b, ck, e_:e_ + 1], axis=0),
                        in_=y[0:n, :], in_offset=None, compute_op=ALU.add))(y, n, b, ck, e_),
                        [y, isel], ['xres'])
            P.flush()

    def phase_final(s):
        c, P = s.c, s.P
        D = c.D
        with ExitStack() as es:
            nf = s.sb(es, 'fn_w', (128, D))
            s.ld(nf[:, :], AP(s.I['norm_final'], 0, [[0, 128], [1, D]]), (), [nf])
            xt = [s.sb(es, 'fn_x%d' % i, (128, D)) for i in range(3)]
            junk = s.sb(es, 'fn_j', (128, D))
            ss = [s.sb(es, 'fn_s%d' % i, (128, 2)) for i in range(3)]
            it = 0
            for (kind, b, t0s, ln, mc) in c.seqs:
                if kind != 'l':
                    continue
                for t0 in range(t0s, t0s + ln, 128):
                    x, sq = xt[it % 3], ss[it % 3]
                    it += 1
                    s.ld(x[:, :], s.xres[t0:t0 + 128, :], ['xres'], [x])
                    s.act(junk[:, :], x[:, :], AF.Square, [x], [junk, sq], accum=sq[:, 0:1])
                    s.rsqrt(sq[:, 1:2], sq[:, 0:1], 1.0 / D, 1e-6, [sq], [sq])
                    s.stt('dve', x[:, :], x[:, :], sq[:, 1:2], nf[:, :], ALU.mult, ALU.mult, [x, sq, nf], [x])
                    o0 = t0 - c.NS * c.CTX
                    s.st(s.out[o0:o0 + 128, :], x[:, :], [x], [('out', o0)])
            P.flush()


def make_in_maps(c, inputs):
    bs = big_shapes(c)
    ss = small_shapes(c)
    consts = host_consts(c)
    maps = []
    big = {n: np.ascontiguousarray(np.asarray(inputs[n], np.float32)).reshape(bs[n]) for n in BIGW}
    small = {}
    for n, shp in ss.items():
        a = np.asarray(inputs[n], np.float32)
        if shp[0] == 0:
            a = np.zeros((1, shp[1]), np.float32)
        small[n] = np.ascontiguousarray(a.reshape(max(shp[0], 1), shp[1]))
    x = np.asarray(inputs['x'], np.float32)
    ctx = np.asarray(inputs['ctx'], np.float32)
    cc = np.asarray(inputs['c'], np.float32)
    for i in range(c.NCORES):
        m = {}
        m['x'] = np.ascontiguousarray(x[i * c.NS:(i + 1) * c.NS]).reshape(c.NS * c.SEQ, c.D)
        m['ctx'] = np.ascontiguousarray(ctx[i * c.NS:(i + 1) * c.NS]).reshape(c.NS * c.CTX, c.D)
        m['c'] = np.ascontiguousarray(cc[i * c.NS:(i + 1) * c.NS])
        for n in BIGW:
            r = bs[n][0] // c.NCORES
            m[n] = big[n][i * r:(i + 1) * r] if c.GATHER else big[n]
        m.update(small)
        m.update(consts)
        maps.append(m)
    return maps


def run(c, inputs, debug_out=None, stop=None):
    b = Builder(c, debug_out, stop)
    nc = b.build()
    maps = make_in_maps(c, inputs)
    res = run_bass_kernel_spmd(nc, maps, core_ids=list(range(c.NCORES)))
    return res


def kernel(**inputs):
    c = Cfg(GATHER=False)
    res = run(c, inputs)
    out = np.stack([r['y'] for r in res.results]).reshape(c.BATCH, c.SEQ, c.D)
    return out.astype(np.float32)
```
